# Optimizing a Trainium2 kernel written in Bass

```python
import math
import jax
import jax.numpy as jnp
from jax import lax
import numpy as np

D_MODEL = 1024
BATCH = 2
SEQ = 16384
DEPTH = 4

GRID_W = 64
CTX_LEN = 256
NA_HEAD_DIM = 64
NA_HEADS = D_MODEL // NA_HEAD_DIM
NA_WIDTH = NA_HEADS * NA_HEAD_DIM
NA_ROWS = 8
NA_COLS = 16
NA_QBLOCK = 128
SSM_HEAD_DIM = 64
SSM_INNER = D_MODEL
SSM_HEADS = SSM_INNER // SSM_HEAD_DIM
SSM_GROUPS = 4
SSM_STATE = 128
SSM_CONV = 5
SSM_CHUNK = 128
SSM_CONV_DIM = SSM_INNER + 2 * SSM_GROUPS * SSM_STATE
EVEN_IN = 3 * NA_WIDTH + SSM_INNER + SSM_CONV_DIM + 2 * SSM_HEADS
EVEN_MIX = NA_WIDTH + SSM_INNER
SC_WIDTH = D_MODEL
SC_CONV = 3
FFN_DIM = 256 * ((8 * D_MODEL // 3 + 255) // 256)
N_EXPERTS = 8
TOP_K = 2
EXPERT_DIM = 7 * D_MODEL // 2
MOE_BLOCK = 128
N_EVEN = (DEPTH + 1) // 2
N_ODD = DEPTH // 2
DEEPNORM_ALPHA = (2 * DEPTH) ** 0.25
DEEPNORM_BETA = (8 * DEPTH) ** -0.25
LN_EPS = 1e-5
RMS_EPS = 1e-5

kernel_name = 'hybrid_natten_ssd_shortconv_moe'


def _layernorm(x, g, b):
    xf = x.astype(jnp.float32)
    mu = jnp.mean(xf, axis=-1, keepdims=True)
    var = jnp.mean(jnp.square(xf - mu), axis=-1, keepdims=True)
    return ((xf - mu) * lax.rsqrt(var + LN_EPS) * g + b).astype(x.dtype)


def _modulate(h, shift, scale):
    return h * (1 + scale) + shift


def _dwconv(x, w):
    k = w.shape[0]
    return lax.conv_general_dilated(
        x, w[:, None, :].astype(x.dtype), window_strides=(1,), padding=[(k // 2, k // 2)],
        dimension_numbers=('NWC', 'WIO', 'NWC'), feature_group_count=x.shape[-1])


def _neighbourhood_tables(n_tok):
    rows = n_tok // GRID_W
    kr = min(NA_ROWS, rows)
    r = np.arange(rows)
    col = np.arange(GRID_W)
    r0 = np.clip(r - kr // 2, 0, rows - kr)
    c0 = np.clip(col - NA_COLS // 2, 0, GRID_W - NA_COLS)
    key_r = r0[:, None] + np.arange(kr)
    key_c = c0[:, None] + np.arange(NA_COLS)
    idx = key_r[:, None, :, None] * GRID_W + key_c[None, :, None, :]
    dr = key_r - r[:, None] + (NA_ROWS - 1)
    dc = key_c - col[:, None] + (NA_COLS - 1)
    bidx = dr[:, None, :, None] * (2 * NA_COLS - 1) + dc[None, :, None, :]
    n_keys = kr * NA_COLS
    return (jnp.asarray(idx.reshape(n_tok, n_keys), jnp.int32),
            jnp.asarray(bidx.reshape(n_tok, n_keys), jnp.int32))


def _neighbourhood_attention(q, k, v, k_ctx, v_ctx, rpb, nbr_idx, bias_idx):
    b, s, h, dh = q.shape
    n_keys = nbr_idx.shape[1]
    nblk = s // NA_QBLOCK
    scale = dh ** -0.5
    rpb_flat = rpb.reshape(h, -1)
    q_blocks = jnp.moveaxis(q.reshape(b, nblk, NA_QBLOCK, h, dh), 1, 0)
    i_blocks = nbr_idx.reshape(nblk, NA_QBLOCK, n_keys)
    b_blocks = bias_idx.reshape(nblk, NA_QBLOCK, n_keys)

    def block(args):
        q_blk, i_blk, b_blk = args
        k_blk = k[:, i_blk]
        v_blk = v[:, i_blk]
        s_loc = jnp.einsum('bqhd,bqkhd->bhqk', q_blk, k_blk) * scale + rpb_flat[:, b_blk]
        s_ctx = jnp.einsum('bqhd,bchd->bhqc', q_blk, k_ctx) * scale
        p = jax.nn.softmax(jnp.concatenate([s_loc, s_ctx], axis=-1).astype(jnp.float32), axis=-1)
        p = p.astype(v.dtype)
        return (jnp.einsum('bhqk,bqkhd->bqhd', p[..., :n_keys], v_blk)
                + jnp.einsum('bhqc,bchd->bqhd', p[..., n_keys:], v_ctx))

    o = lax.map(block, (q_blocks, i_blocks, b_blocks))
    return jnp.moveaxis(o, 0, 1).reshape(b, s, h * dh)


def _context_attention(q, k, v):
    s = jnp.einsum('bqhd,bkhd->bhqk', q, k) * q.shape[-1] ** -0.5
    p = jax.nn.softmax(s.astype(jnp.float32), axis=-1).astype(v.dtype)
    return jnp.einsum('bhqk,bkhd->bqhd', p, v).reshape(q.shape[0], q.shape[1], -1)


def _ssd_scan(x, dt, a, bm, cm, h0):
    b, l, h, p = x.shape
    g, n = bm.shape[2], bm.shape[3]
    hg = h // g
    nc = l // SSM_CHUNK

    def chunks(t):
        return jnp.moveaxis(t.reshape((b, nc, SSM_CHUNK) + t.shape[2:]), 1, 0)

    xs = chunks((x.astype(jnp.float32) * dt[..., None]).reshape(b, l, g, hg, p))
    las = chunks((dt * a).reshape(b, l, g, hg))
    bs = chunks(bm.astype(jnp.float32))
    cs = chunks(cm.astype(jnp.float32))
    tril = jnp.tril(jnp.ones((SSM_CHUNK, SSM_CHUNK), dtype=bool))[None, :, :, None, None]

    def step(state, inp):
        xc, lac, bc, cc = inp
        cum = jnp.cumsum(lac, axis=1)
        seg = cum[:, :, None] - cum[:, None, :]
        decay = jnp.exp(jnp.where(tril, seg, -jnp.inf))
        cb = jnp.einsum('bign,bjgn->bijg', cc, bc)
        y = jnp.einsum('bijg,bijgh,bjghp->bighp', cb, decay, xc)
        y = y + jnp.einsum('bign,bghpn->bighp', cc, state) * jnp.exp(cum)[..., None]
        last = cum[:, -1]
        w = jnp.exp(last[:, None] - cum)
        state = (state * jnp.exp(last)[..., None, None]
                 + jnp.einsum('bjgn,bjgh,bjghp->bghpn', bc, w, xc))
        return state, y

    h_fin, ys = lax.scan(step, h0, (xs, las, bs, cs))
    y = jnp.moveaxis(ys, 0, 1).reshape(b, l, h, p)
    return y.astype(x.dtype), h_fin


def _gated_group_rmsnorm(y, z, w):
    u = (y * jax.nn.silu(z)).astype(jnp.float32)
    ug = u.reshape(u.shape[:-1] + (SSM_GROUPS, -1))
    ug = ug * lax.rsqrt(jnp.mean(jnp.square(ug), axis=-1, keepdims=True) + RMS_EPS)
    return (ug.reshape(u.shape) * w).astype(y.dtype)


def _even_mixer(u_lat, u_ctx, w_in, rpb, conv_w, conv_b, dt_bias, a_log, d_skip, norm_w, w_out,
                nbr_idx, bias_idx, with_ctx_out):
    offs = [NA_WIDTH, 2 * NA_WIDTH, 3 * NA_WIDTH, 3 * NA_WIDTH + SSM_INNER,
            3 * NA_WIDTH + SSM_INNER + SSM_CONV_DIM]

    def split(pr):
        b, l, _ = pr.shape
        q, k, v, z, xbc, dt_raw = jnp.split(pr, offs, axis=-1)
        heads = lambda t: t.reshape(b, l, NA_HEADS, NA_HEAD_DIM)
        return heads(q), heads(k), heads(v), z, xbc, dt_raw

    def ssm_inputs(xbc, dt_raw):
        b, l, _ = xbc.shape
        xbc = jax.nn.silu(_dwconv(xbc, conv_w) + conv_b)
        xs, bm, cm = jnp.split(xbc, [SSM_INNER, SSM_INNER + SSM_GROUPS * SSM_STATE], axis=-1)
        xs = xs.reshape(b, l, SSM_HEADS, SSM_HEAD_DIM)
        bm = bm.reshape(b, l, SSM_GROUPS, SSM_STATE)
        cm = cm.reshape(b, l, SSM_GROUPS, SSM_STATE)
        dt = jax.nn.softplus(dt_raw.astype(jnp.float32).reshape(b, l, 2, SSM_HEADS)
                             + dt_bias.astype(jnp.float32))
        return xs, bm, cm, dt

    flip = lambda t: t[:, ::-1]

    q_l, k_l, v_l, z_l, xbc_l, dtr_l = split(u_lat @ w_in)
    q_c, k_c, v_c, z_c, xbc_c, dtr_c = split(u_ctx @ w_in)

    attn_l = _neighbourhood_attention(q_l, k_l, v_l, k_c, v_c, rpb, nbr_idx, bias_idx)

    a = -jnp.exp(a_log.astype(jnp.float32))
    x_l, b_l, c_l, dt_l = ssm_inputs(xbc_l, dtr_l)
    x_c, b_c, c_c, dt_c = ssm_inputs(xbc_c, dtr_c)
    h0 = jnp.zeros((x_c.shape[0], SSM_GROUPS, SSM_HEADS // SSM_GROUPS, SSM_HEAD_DIM, SSM_STATE),
                   jnp.float32)
    yc_f, hc_f = _ssd_scan(x_c, dt_c[:, :, 0], a[0], b_c, c_c, h0)
    yl_f, _ = _ssd_scan(x_l, dt_l[:, :, 0], a[0], b_l, c_l, hc_f)
    yc_b, hc_b = _ssd_scan(flip(x_c), flip(dt_c[:, :, 1]), a[1], flip(b_c), flip(c_c), h0)
    yl_b, _ = _ssd_scan(flip(x_l), flip(dt_l[:, :, 1]), a[1], flip(b_l), flip(c_l), hc_b)

    def ssm_out(xs, y_f, y_b_rev, z):
        b, l = xs.shape[0], xs.shape[1]
        y = y_f + flip(y_b_rev) + xs * d_skip[:, None]
        return _gated_group_rmsnorm(y.reshape(b, l, SSM_INNER), z, norm_w)

    y_lat = jnp.concatenate([attn_l, ssm_out(x_l, yl_f, yl_b, z_l)], axis=-1) @ w_out
    if not with_ctx_out:
        return y_lat, None
    attn_c = _context_attention(q_c, k_c, v_c)
    y_ctx = jnp.concatenate([attn_c, ssm_out(x_c, yc_f, yc_b, z_c)], axis=-1) @ w_out
    return y_lat, y_ctx


def _short_conv_mixer(u, w_in, conv_w, w_out):
    gate_b, gate_c, h = jnp.split(u @ w_in, 3, axis=-1)
    return (gate_b * _dwconv(gate_c * h, conv_w)) @ w_out


def _swiglu(h, w1, w3, w2):
    return (jax.nn.silu(h @ w1) * (h @ w3)) @ w2


def _moe_swiglu(h, w_router, w1, w3, w2):
    shp = h.shape
    xt = h.reshape(-1, shp[-1])
    t = xt.shape[0]
    logits = (xt @ w_router).astype(jnp.float32)
    top_v, top_e = lax.top_k(logits, TOP_K)
    gates = jax.nn.softmax(top_v, axis=-1)
    n_assign = t * TOP_K
    flat_e = top_e.reshape(-1).astype(jnp.int32)
    order = jnp.argsort(flat_e)
    sorted_e = flat_e[order]
    counts = jnp.bincount(flat_e, length=N_EXPERTS).astype(jnp.int32)
    padded = (counts + MOE_BLOCK - 1) // MOE_BLOCK * MOE_BLOCK
    starts = jnp.cumsum(counts) - counts
    pad_ends = jnp.cumsum(padded)
    pad_starts = pad_ends - padded
    dest_sorted = (pad_starts[sorted_e] + jnp.arange(n_assign, dtype=jnp.int32)
                   - starts[sorted_e]).astype(jnp.int32)
    n_blocks = -(-n_assign // MOE_BLOCK) + N_EXPERTS
    n_rows = n_blocks * MOE_BLOCK
    row_token = jnp.zeros((n_rows,), jnp.int32).at[dest_sorted].set(
        (order // TOP_K).astype(jnp.int32))
    block_expert = jnp.minimum(
        jnp.searchsorted(pad_ends, jnp.arange(n_blocks, dtype=jnp.int32) * MOE_BLOCK, side='right'),
        N_EXPERTS - 1)
    rows_in = xt[row_token].reshape(n_blocks, MOE_BLOCK, shp[-1])

    def expert_block(args):
        xb, e = args
        return (jax.nn.silu(xb @ w1[e]) * (xb @ w3[e])) @ w2[e]

    rows_out = lax.map(expert_block, (rows_in, block_expert)).reshape(n_rows, shp[-1])
    dest = jnp.zeros((n_assign,), jnp.int32).at[order].set(dest_sorted)
    y = jnp.einsum('tkd,tk->td', rows_out[dest].reshape(t, TOP_K, shp[-1]), gates.astype(h.dtype))
    return y.reshape(shp)


def setup_inputs(seed: int = 0) -> dict:
    key = jax.random.key(seed)
    ks = jax.random.split(key, 32)
    f32 = jnp.float32
    d = D_MODEL
    beta = DEEPNORM_BETA

    def nrm(k, shape, scale):
        return jax.random.normal(k, shape, f32) * scale

    dt0 = jnp.exp(jax.random.uniform(ks[12], (N_EVEN, 2, SSM_HEADS), f32,
                                     math.log(1e-3), math.log(1e-1)))
    return {
        'x': nrm(ks[0], (BATCH, SEQ, d), 1.0),
        'c': nrm(ks[1], (BATCH, d), 1.0),
        'ctx': nrm(ks[2], (BATCH, CTX_LEN, d), 1.0),
        'c_ctx': nrm(ks[3], (d,), 1.0),
        'ada_w': nrm(ks[4], (DEPTH, d, 6 * d), 0.5 * d ** -0.5),
        'ada_b': nrm(ks[5], (DEPTH, 6 * d), 0.02),
        'ln_g': 1.0 + nrm(ks[6], (DEPTH, 2, d), 0.02),
        'ln_b': nrm(ks[7], (DEPTH, 2, d), 0.02),
        'even_w_in': nrm(ks[8], (N_EVEN, d, EVEN_IN), d ** -0.5),
        'na_rpb': nrm(ks[9], (N_EVEN, NA_HEADS, 2 * NA_ROWS - 1, 2 * NA_COLS - 1), 0.1),
        'ssm_conv_w': nrm(ks[10], (N_EVEN, SSM_CONV, SSM_CONV_DIM), SSM_CONV ** -0.5),
        'ssm_conv_b': nrm(ks[11], (N_EVEN, SSM_CONV_DIM), 0.02),
        'ssm_dt_bias': dt0 + jnp.log(-jnp.expm1(-dt0)),
        'ssm_a_log': jnp.log(jax.random.uniform(ks[13], (N_EVEN, 2, SSM_HEADS), f32, 1.0, 16.0)),
        'ssm_d': 1.0 + nrm(ks[14], (N_EVEN, SSM_HEADS), 0.1),
        'ssm_norm_w': 1.0 + nrm(ks[15], (N_EVEN, SSM_INNER), 0.02),
        'even_w_out': nrm(ks[16], (N_EVEN, EVEN_MIX, d), beta * EVEN_MIX ** -0.5),
        'ffn_w1': nrm(ks[17], (N_EVEN, d, FFN_DIM), d ** -0.5),
        'ffn_w3': nrm(ks[18], (N_EVEN, d, FFN_DIM), d ** -0.5),
        'ffn_w2': nrm(ks[19], (N_EVEN, FFN_DIM, d), beta * FFN_DIM ** -0.5),
        'sc_w_in': nrm(ks[20], (N_ODD, d, 3 * SC_WIDTH), d ** -0.5),
        'sc_conv_w': nrm(ks[21], (N_ODD, SC_CONV, SC_WIDTH), SC_CONV ** -0.5),
        'sc_w_out': nrm(ks[22], (N_ODD, SC_WIDTH, d), beta * SC_WIDTH ** -0.5),
        'moe_router': nrm(ks[23], (N_ODD, d, N_EXPERTS), d ** -0.5),
        'moe_w1': nrm(ks[24], (N_ODD, N_EXPERTS, d, EXPERT_DIM), d ** -0.5),
        'moe_w3': nrm(ks[25], (N_ODD, N_EXPERTS, d, EXPERT_DIM), d ** -0.5),
        'moe_w2': nrm(ks[26], (N_ODD, N_EXPERTS, EXPERT_DIM, d), beta * EXPERT_DIM ** -0.5),
    }


def reference(x, c, ctx, c_ctx, ada_w, ada_b, ln_g, ln_b,
              even_w_in, na_rpb, ssm_conv_w, ssm_conv_b, ssm_dt_bias, ssm_a_log, ssm_d,
              ssm_norm_w, even_w_out, ffn_w1, ffn_w3, ffn_w2,
              sc_w_in, sc_conv_w, sc_w_out, moe_router, moe_w1, moe_w3, moe_w2):
    nbr_idx, bias_idx = _neighbourhood_tables(x.shape[1])
    silu_c = jax.nn.silu(c)
    silu_cc = jax.nn.silu(c_ctx)
    h_lat, h_ctx = x, ctx
    for i in range(DEPTH):
        j = i // 2
        even = i % 2 == 0
        ctx_live = any(m % 2 == 0 for m in range(i + 1, DEPTH))
        mods_l = jnp.split((silu_c @ ada_w[i] + ada_b[i])[:, None, :], 6, axis=-1)
        mods_c = jnp.split(silu_cc @ ada_w[i] + ada_b[i], 6, axis=-1)

        u_lat = _modulate(h_lat, mods_l[0], mods_l[1])
        u_ctx = _modulate(h_ctx, mods_c[0], mods_c[1])
        if even:
            y_lat, y_ctx = _even_mixer(u_lat, u_ctx, even_w_in[j], na_rpb[j], ssm_conv_w[j],
                                       ssm_conv_b[j], ssm_dt_bias[j], ssm_a_log[j], ssm_d[j],
                                       ssm_norm_w[j], even_w_out[j], nbr_idx, bias_idx, ctx_live)
        else:
            y_lat = _short_conv_mixer(u_lat, sc_w_in[j], sc_conv_w[j], sc_w_out[j])
            y_ctx = (_short_conv_mixer(u_ctx, sc_w_in[j], sc_conv_w[j], sc_w_out[j])
                     if ctx_live else None)
        h_lat = _layernorm(DEEPNORM_ALPHA * h_lat + mods_l[2] * y_lat, ln_g[i, 0], ln_b[i, 0])
        if ctx_live:
            h_ctx = _layernorm(DEEPNORM_ALPHA * h_ctx + mods_c[2] * y_ctx, ln_g[i, 0], ln_b[i, 0])

        def channel(t):
            if even:
                return _swiglu(t, ffn_w1[j], ffn_w3[j], ffn_w2[j])
            return _moe_swiglu(t, moe_router[j], moe_w1[j], moe_w3[j], moe_w2[j])

        f_lat = channel(_modulate(h_lat, mods_l[3], mods_l[4]))
        h_lat = _layernorm(DEEPNORM_ALPHA * h_lat + mods_l[5] * f_lat, ln_g[i, 1], ln_b[i, 1])
        if ctx_live:
            f_ctx = channel(_modulate(h_ctx, mods_c[3], mods_c[4]))
            h_ctx = _layernorm(DEEPNORM_ALPHA * h_ctx + mods_c[5] * f_ctx, ln_g[i, 1], ln_b[i, 1])
    return h_lat
```

```python
import numpy as np
from contextlib import ExitStack
import concourse.bass as bass
import concourse.mybir as mybir
from concourse.bass_utils import run_bass_kernel_spmd

F32 = mybir.dt.float32
BF16 = mybir.dt.bfloat16
I32 = mybir.dt.int32
AF = mybir.ActivationFunctionType
ALU = mybir.AluOpType
AX = mybir.AxisListType

NDMA_SEMS = 12
import os as _os
SAME_ENG_DIST = int(_os.environ.get('SAME_ENG_DIST', '2'))


class Buf:
    __slots__ = ("name", "w", "r")

    def __init__(self, name):
        self.name = name
        self.w = None
        self.r = []


class Op:
    __slots__ = ("eng", "fn", "deps", "idx", "signal", "count", "dma", "slot", "pos", "waits", "cc")


class KB:
    ENGS = ("pe", "act", "dve", "pool", "sp")

    def __init__(self):
        self.nc = bass.Bass("TRN2", target_bir_lowering=False)
        self.ops = []
        self.es = ExitStack()
        self.nbuf = 0
        self.eng_ops = {e: [] for e in self.ENGS}
        self.ndma = {e: 0 for e in self.ENGS}
        self.es_phase = None
        self.nsb = 0
        self.ncc = 0
        self.open_dma = []
        self.bar_d = self.nc.dram_tensor("bar_scr", [1, 2], F32, kind="Internal").ap()
        self.b_bar = Buf("bar")
        self.junk = self.es.enter_context(self.nc.sbuf_tensor("junk", [128, 4], F32))
        self.junk_bf = self.es.enter_context(self.nc.sbuf_tensor("junk_bf", [128, 4], BF16))
        self.junk_ps = None
        self.b_junk = [Buf(f"junk{i}") for i in range(4)]

    def dram(self, name, shape, dtype, kind):
        t = self.nc.dram_tensor(name, list(shape), dtype, kind=kind)
        return t.ap(), Buf(name)

    def sb(self, name, shape, dtype=F32):
        es = self.es_phase if self.es_phase is not None else self.es
        self.nsb += 1
        t = es.enter_context(self.nc.sbuf_tensor(f"{name}_{self.nsb}", list(shape), dtype))
        return t

    def dr(self, io, name, shape, dtype, kind):
        if io is not None and name in io:
            return io[name]
        return self.dram(name, shape, dtype, kind)

    def phase_begin(self):
        self.es_phase = ExitStack()

    def phase_end(self):
        self.barrier()
        self.es_phase.close()
        self.es_phase = None

    def barrier(self):
        deps = set(self.open_dma)
        for e in self.ENGS:
            if self.eng_ops[e]:
                deps.add(self.eng_ops[e][-1].idx)
        bop = self._rec("sp", lambda e: e.dma_start(out=self.bar_d[0:1, 0:1], in_=self.bar_d[0:1, 1:2]), [], [self.b_bar], True,
                        extra=deps)
        self.open_dma = []
        jk = self.junk
        self.op("pool", lambda e: e.memset(jk[:, 0:1], 0.0), reads=[self.b_bar], writes=[self.b_junk[0]])
        self.op("dve", lambda e: e.memset(jk[:, 1:2], 0.0), reads=[self.b_bar], writes=[self.b_junk[1]])
        self.op("act", lambda e: e.mul(jk[:, 2:3], jk[:, 3:4], 1.0), reads=[self.b_bar], writes=[self.b_junk[2]])
        self.op("pe", lambda e: e.matmul(self.junk_ps[0:1, 0:1], lhsT=self.junk_bf[0:1, 0:1], rhs=self.junk_bf[0:1, 0:1],
                                         start=True, stop=True), reads=[self.b_bar], writes=[self.b_junk[3]])

    def cc(self, kind, alu, groups, in_ap, out_ap, reads, writes):
        if _os.environ.get("FUSED_NOCC"):
            return None
        op = self._rec("pool", lambda e: e.collective_compute(kind, alu, replica_groups=groups, ins=[in_ap.opt()],
                                                              outs=[out_ap.opt()]), reads, writes, True)
        op.cc = self.ncc
        self.ncc += 1
        self.ndma["pool"] -= 1
        op.slot = None
        return op

    def ps(self, name, shape, dtype=F32):
        t = self.es.enter_context(self.nc.psum_tensor(name, list(shape), dtype))
        return t

    def buf(self, name=None):
        self.nbuf += 1
        return Buf(name or f"b{self.nbuf}")

    def bufs(self, n, name="b"):
        return [self.buf(f"{name}{i}") for i in range(n)]

    def _rec(self, eng, fn, reads, writes, dma, extra=None):
        op = Op()
        op.cc = None
        op.eng = eng
        op.fn = fn
        op.idx = len(self.ops)
        op.signal = False
        op.count = 0
        op.dma = dma
        op.slot = None
        op.waits = None
        deps = set()
        for b in reads:
            if b.w is not None:
                deps.add(b.w)
        for b in writes:
            if b.w is not None:
                deps.add(b.w)
            for r in b.r:
                deps.add(r)
        if extra:
            deps |= set(extra)
        op.deps = deps
        if dma:
            self.open_dma.append(op.idx)
        for b in reads:
            if not dma:
                b.r = [r for r in b.r if self.ops[r].dma or self.ops[r].eng != eng]
            b.r.append(op.idx)
        for b in writes:
            b.w = op.idx
            b.r = []
        op.pos = len(self.eng_ops[eng])
        self.eng_ops[eng].append(op)
        if dma:
            op.slot = self.ndma[eng]
            self.ndma[eng] += 1
        self.ops.append(op)
        return op

    def op(self, eng, fn, reads=(), writes=()):
        return self._rec(eng, fn, reads, writes, False)

    def dma(self, eng, out, in_, reads=(), writes=(), **kw):
        return self._rec(eng, lambda e: e.dma_start(out=out, in_=in_, **kw), reads, writes, True)

    def finish(self):
        nc = self.nc
        ops = self.ops
        for op in ops:
            for d in op.deps:
                p = ops[d]
                if p.dma:
                    continue
                if p.eng == op.eng and not op.dma:
                    if p.eng == "pe":
                        continue
                    if op.pos - p.pos > SAME_ENG_DIST:
                        continue
                p.signal = True
        cnt = {e: 0 for e in self.ENGS}
        for op in ops:
            if op.dma:
                continue
            if op.signal:
                cnt[op.eng] += 1
                op.count = cnt[op.eng]
        self.sig_counts = dict(cnt)
        seen = {e: {} for e in self.ENGS}
        for op in ops:
            w = {}
            for d in op.deps:
                p = ops[d]
                if p.cc is not None:
                    key = ("cc", p.cc)
                    val = 1
                elif p.dma:
                    key = ("d", p.eng, p.slot % NDMA_SEMS)
                    val = 16 * (p.slot // NDMA_SEMS + 1)
                else:
                    if not p.signal:
                        continue
                    if p.eng == op.eng and not op.dma:
                        if p.eng == "pe" or op.pos - p.pos > SAME_ENG_DIST:
                            continue
                    key = ("c", p.eng)
                    val = p.count
                if val > w.get(key, 0):
                    w[key] = val
            if op.dma and op.cc is None and op.slot >= NDMA_SEMS:
                key = ("d", op.eng, op.slot % NDMA_SEMS)
                val = 16 * (op.slot // NDMA_SEMS)
                if val > w.get(key, 0):
                    w[key] = val
            sw = seen[op.eng]
            op.waits = []
            for key, val in w.items():
                if sw.get(key, 0) >= val:
                    continue
                sw[key] = val
                op.waits.append((key, val))
        sems = {}
        for e in self.ENGS:
            sems[("c", e)] = self.es.enter_context(nc.semaphore(f"c_{e}"))
            if self.ndma[e] > 0:
                for s in range(min(NDMA_SEMS, self.ndma[e])):
                    sems[("d", e, s)] = self.es.enter_context(nc.semaphore(f"d_{e}_{s}"))
        final_waits = []
        for ci in range(self.ncc):
            sems[("cc", ci)] = self.es.enter_context(nc.semaphore(f"cc_{ci}"))
            final_waits.append((("cc", ci), 1))
        for e in self.ENGS:
            n = self.ndma[e]
            for s in range(min(NDMA_SEMS, n)):
                k = (n - 1 - s) // NDMA_SEMS + 1
                final_waits.append((("d", e, s), 16 * k))

        def run(e, engobj):
            for op in self.eng_ops[e]:
                for key, val in op.waits:
                    engobj.wait_ge(sems[key], val)
                ins = op.fn(engobj)
                if op.cc is not None:
                    ins.then_inc(sems[("cc", op.cc)])
                elif op.dma:
                    ins.then_inc(sems[("d", e, op.slot % NDMA_SEMS)], 16)
                elif op.signal:
                    ins.then_inc(sems[("c", e)], 1)
            if e == "sp":
                for key, val in final_waits:
                    engobj.wait_ge(sems[key], val)

        with nc.Block() as block:
            @block.tensor
            def _(eng):
                run("pe", eng)

            @block.scalar
            def _(eng):
                run("act", eng)

            @block.vector
            def _(eng):
                run("dve", eng)

            @block.gpsimd
            def _(eng):
                run("pool", eng)

            @block.sync
            def _(eng):
                run("sp", eng)
        self.es.close()
        return nc


D = 1024
KC = 8
ALPHA = float((2 * 4) ** 0.25)
LN_EPS = 1e-5
NSLOT = 6
WLOOK = 4


def _bc(ap, shape):
    return ap.to_broadcast(list(shape))


class Ctx:
    def __init__(self, k):
        self.k = k
        nc = k.nc
        self.ident = k.sb("ident", [128, 128])
        self.b_ident = k.buf("ident")
        k.op("pool", lambda e: e.memset(self.ident[:], 0.0), writes=[self.b_ident])
        k.op("pool", lambda e: e.affine_select(out=self.ident[:], in_=self.ident[:], pattern=[[-1, 128]],
                                               compare_op=ALU.not_equal, fill=1.0, base=0, channel_multiplier=1),
             reads=[self.b_ident], writes=[self.b_ident])
        self.bank = [k.ps(f"bank{i}", [128, 512]) for i in range(8)]
        self.b_bank = [k.buf(f"bank{i}") for i in range(8)]
        k.junk_ps = self.bank[7]
        k.b_junk[3] = self.b_bank[7]
        k.op("pool", lambda e: e.memset(k.junk[:], 0.0), writes=[k.b_junk[0], k.b_junk[1], k.b_junk[2]])
        k.op("pool", lambda e: e.memset(k.junk_bf[:], 0.0), writes=[k.b_junk[0]])
        self.wslot = [k.sb(f"wslot{i}", [128, 8, 512], BF16) for i in range(NSLOT)]
        self.b_wslot = [k.buf(f"wslot{i}") for i in range(NSLOT)]
        self.steps = []

    def step(self, wspec, fn):
        self.steps.append((wspec, fn))

    def flush(self):
        k = self.k
        steps = self.steps
        widx = [i for i, s in enumerate(steps) if s[0] is not None]
        slot_of = {}
        for j, i in enumerate(widx):
            slot_of[i] = j % NSLOT
        loaded = 0

        def load(j):
            i = widx[j]
            ap, dbuf, n = steps[i][0]
            ncols = ap.shape[1]
            s = slot_of[i]
            k.dma("pool", self.wslot[s][:, 0:n, 0:ncols], ap.rearrange("(c p) n -> p c n", p=128),
                  reads=[dbuf], writes=[self.b_wslot[s]])

        nw_done = 0
        for i, (wspec, fn) in enumerate(steps):
            if wspec is not None:
                while loaded < len(widx) and loaded <= nw_done + WLOOK:
                    load(loaded)
                    loaded += 1
                s = slot_of[i]
                fn(self.wslot[s], self.b_wslot[s])
                nw_done += 1
            else:
                fn(None, None)
        self.steps = []


def emit_transposes(cx, src, b_src, ntok, nchunk, evac):
    k = cx.k
    for g in range(0, nchunk, 4):
        n = min(4, nchunk - g)
        bi = (g // 4) % 2
        bank, bb = cx.bank[bi], cx.b_bank[bi]
        for c in range(n):
            k.op("pe", lambda e, c=c, g=g, bank=bank: e.transpose(
                bank[:, c * 128:c * 128 + ntok], src[0:ntok, (g + c) * 128:(g + c + 1) * 128],
                cx.ident[0:ntok, 0:ntok]), reads=[b_src, cx.b_ident], writes=[bb])
        evac(bank, bb, g, n)


def emit_modT(cx, src, b_src, ntok, dst, b_dst, col0, scale1p, shift, b_mod, dst32=None):
    k = cx.k

    def evac(bank, bb, g, n):
        for c in range(n):
            cc = g + c
            if dst32 is None:
                k.op("act", lambda e, c=c, cc=cc, bank=bank: e.activation(
                    out=dst[:, cc, col0:col0 + ntok], in_=bank[:, c * 128:c * 128 + ntok], func=AF.Identity,
                    scale=scale1p[:, cc:cc + 1], bias=shift[:, cc:cc + 1]),
                    reads=[bb, b_mod], writes=[b_dst])
            else:
                d32, b_d32 = dst32
                k.op("act", lambda e, c=c, cc=cc, bank=bank: e.activation(
                    out=d32[:, cc, col0:col0 + ntok], in_=bank[:, c * 128:c * 128 + ntok], func=AF.Identity,
                    scale=scale1p[:, cc:cc + 1], bias=shift[:, cc:cc + 1]),
                    reads=[bb, b_mod], writes=[b_d32])
        if dst32 is not None:
            d32, b_d32 = dst32
            k.op("pool", lambda e, g=g, n=n: e.tensor_copy(dst[:, g:g + n, col0:col0 + ntok],
                                                           d32[:, g:g + n, col0:col0 + ntok]),
                 reads=[b_d32], writes=[b_dst])

    emit_transposes(cx, src, b_src, ntok, KC, evac)


def emit_copyT(cx, src, b_src, ntok, nchunk, dst, b_dst, col0):
    k = cx.k

    def evac(bank, bb, g, n):
        k.op("act", lambda e, bank=bank, g=g, n=n: e.activation(
            out=dst[:, g:g + n, col0:col0 + ntok],
            in_=bank[:, 0:n * 128].rearrange("p (a b) -> p a b", a=n)[:, :, 0:ntok], func=AF.Copy),
            reads=[bb], writes=[b_dst])

    emit_transposes(cx, src, b_src, ntok, nchunk, evac)


def emit_ln(cx, r, b_r, lng, lnb, b_ln, out, b_out, tmp, b_tmp):
    k = cx.k
    st, mv = tmp
    for hf in range(2):
        k.op("dve", lambda e, hf=hf: e.bn_stats(st[:, hf, :], r[:, hf * 512:(hf + 1) * 512]),
             reads=[b_r], writes=[b_tmp])
    k.op("dve", lambda e: e.bn_aggr(mv[:, 0:2], st[:, :, :]), reads=[b_tmp], writes=[b_tmp])
    k.op("dve", lambda e: e.tensor_scalar_add(mv[:, 1:2], mv[:, 1:2], LN_EPS), reads=[b_tmp], writes=[b_tmp])
    k.op("act", lambda e: e.activation(out=mv[:, 2:3], in_=mv[:, 1:2], func=AF.Ln), reads=[b_tmp], writes=[b_tmp])
    k.op("act", lambda e: e.activation(out=mv[:, 2:3], in_=mv[:, 2:3], func=AF.Exp, scale=-0.5),
         reads=[b_tmp], writes=[b_tmp])
    k.op("dve", lambda e: e.scalar_tensor_tensor(out=mv[:, 3:4], in0=mv[:, 0:1], scalar=-1.0, in1=mv[:, 2:3],
                                                 op0=ALU.mult, op1=ALU.mult), reads=[b_tmp], writes=[b_tmp])
    k.op("act", lambda e: e.activation(out=r, in_=r, func=AF.Identity, scale=mv[:, 2:3], bias=mv[:, 3:4]),
         reads=[b_tmp, b_r], writes=[b_r])
    k.op("pool", lambda e: e.tensor_tensor(out=r, in0=r, in1=lng, op=ALU.mult), reads=[b_r, b_ln], writes=[b_r])
    k.op("pool", lambda e: e.tensor_tensor(out=out, in0=r, in1=lnb, op=ALU.add), reads=[b_r, b_ln],
         writes=[b_out] if b_out is not b_r else [b_r])


def build_tail(cfg, k=None, cx=None, io=None):
    own = k is None
    if own:
        k = KB()
        cx = Ctx(k)
    mixer = cfg["mixer"]
    NE = cfg["ne"]
    F = cfg.get("f", 0)
    FC = F // 128
    groups = cfg["groups"]
    NTOK = cfg["ntok"]
    nms = cfg["nmod"]

    h_in, b_hin = k.dr(io, "h_in", [NTOK, D], F32, "ExternalInput")
    h_out, b_hout = k.dr(io, "h_out", [NTOK, D], F32, "ExternalOutput")
    ncb = 4 + 2 * nms
    cbc_d, b_cbcd = k.dr(io, "cbc", [128, ncb, D], F32, "ExternalInput")
    if "mods_src" not in cfg:
        cT_d, b_cTd = k.dr(io, "cT", [128, 4 * nms, KC], F32, "ExternalInput")
    cbc = k.sb("cbc_sb", [128, ncb, D])
    b_cbc = k.buf("cbc")
    cT = k.sb("cT_sb", [128, 4 * nms, KC])
    b_cT = k.buf("cT")
    if "mods_src" in cfg:
        mods_ap, b_mods, li = cfg["mods_src"]
        k.dma("sp", cbc[:, 0:4, :], cbc_d, reads=[b_cbcd], writes=[b_cbc])
        for m in range(nms):
            for gi, blk in ((0, 2), (1, 5)):
                k.dma("sp", cbc[:, 4 + 2 * m + gi, :], mods_ap[li, m, blk * D:(blk + 1) * D].partition_broadcast(128),
                      reads=[b_mods], writes=[b_cbc])
            for ci, blk in ((0, 0), (1, 1), (2, 3), (3, 4)):
                k.dma("sp", cT[:, 4 * m + ci, :], mods_ap[li, m, blk * D:(blk + 1) * D].rearrange("(c p) -> p c", p=128),
                      reads=[b_mods], writes=[b_cT], allow_slow_non_contiguous=True)
    else:
        k.dma("sp", cbc[:], cbc_d, reads=[b_cbcd], writes=[b_cbc])
        k.dma("sp", cT[:], cT_d, reads=[b_cTd], writes=[b_cT])
    for m in range(nms):
        for i in (1, 3):
            k.op("dve", lambda e, m=m, i=i: e.tensor_scalar_add(cT[:, 4 * m + i, :], cT[:, 4 * m + i, :], 1.0),
                 reads=[b_cT], writes=[b_cT])

    hb = k.sb("hb", [128, 4, D])
    b_hb = k.bufs(4, "hb")
    lnst = k.sb("lnst", [128, 2, 6])
    lnmv = k.sb("lnmv", [128, 4])
    b_lnt = k.buf("lnt")
    tt = k.sb("tt", [128, 2, 512])
    b_tt = k.bufs(2, "tt")
    mT = None
    if mixer == "pre":
        ylat_d, b_ylat = io["y_lat"]
        yctx_d, b_yctx = io["y_ctx"]
        ypre = k.sb("ypre", [128, D])
        b_ypre = k.buf("ypre")
    if mixer == "even":
        mix_d, b_mixd = k.dr(io, "mix", [NTOK, 2 * D], F32, "ExternalInput")
        wout_d, b_woutd = k.dr(io, "w_out", [2 * D, D], F32, "ExternalInput")
        mixb = k.sb("mixb", [128, 2 * D])
        b_mixb = k.buf("mixb")
        mT = k.sb("mT", [128, 16, 512], BF16)
        b_mT = k.buf("mT")
    if mixer == "sc":
        NV = cfg["nv"]
        NH = cfg.get("nhalo", 2)
        hal_d, b_hald = k.dr(io, "hal", [NH, D], F32, "ExternalInput")
        if NH == 2:
            halv_d, b_halvd = k.dr(io, "halv", [128, 2], F32, "ExternalInput")
        else:
            halsel_d, b_halseld = k.dr(io, "halsel", [128, 2, KC, NH], F32, "ExternalInput")
            halsel = k.sb("halsel_sb", [128, 2, KC, NH])
            b_halsel = k.buf("halsel")
            k.dma("sp", halsel[:], halsel_d, reads=[b_halseld], writes=[b_halsel])
            hsel = k.sb("hsel", [128, KC, NH])
            b_hsel = k.buf("hsel")
            hv = k.sb("hv", [128, 2, KC, 1])
            b_hv = k.buf("hv")
        scwin_d, b_scwind = k.dr(io, "sc_w_in", [D, 3 * D], F32, "ExternalInput")
        scwout_d, b_scwoutd = k.dr(io, "sc_w_out", [D, D], F32, "ExternalInput")
        cw_d, b_cwd = k.dr(io, "sc_cw", [128, KC, 3], F32, "ExternalInput")
        vT_d, b_vTd = k.dr(io, "vT_scr", [D, NV], F32, "Internal")
        gbT_d, b_gbTd = k.dr(io, "gbT_scr", [D, NV], F32, "Internal")
        cw = k.sb("cw_sb", [128, KC, 3])
        b_cw = k.buf("cw")
        k.dma("sp", cw[:], cw_d, reads=[b_cwd], writes=[b_cw])
        halv = k.sb("halv_sb", [128, 2])
        b_halv = k.buf("halv")
        if NH == 2:
            k.dma("sp", halv[:], halv_d, reads=[b_halvd], writes=[b_halv])
        halb = k.sb("halb", [NH, D])
        b_halb = k.buf("halb")
        vwin = k.sb("vwin", [128, KC, 514])
        b_vwin = k.buf("vwin")
        gbw = k.sb("gbw", [128, KC, 512])
        b_gbw = k.buf("gbw")
        mT = k.sb("mT", [128, KC, 512], BF16)
        b_mT = k.buf("mT")
        if cfg.get("debug"):
            dbgm_d, b_dbgm = k.dr(io, "dbgm", [128, KC, 512], BF16, "ExternalOutput")
            dbgu_d, b_dbgu = k.dr(io, "dbgu", [128, KC, 512], BF16, "ExternalOutput")
            dbgv_d, b_dbgv = k.dr(io, "dbgv", [128, KC, 514], F32, "ExternalOutput")
            dbgg_d, b_dbgg = k.dr(io, "dbgg", [128, KC, 512], F32, "ExternalOutput")
        cacc = k.sb("cacc", [128, 2, 512])
        b_cacc = k.bufs(2, "cacc")
        gcs = k.sb("gcs", [128, 2, 512])
        b_gcs = k.bufs(2, "gcs")
        zcol = k.sb("zcol", [128, KC, 2])
        b_zcol = k.buf("zcol")
    if NE > 0 or mixer == "sc":
        uT = k.sb("uT", [128, KC, 512], BF16)
        b_uT = k.buf("uT")
    if NE > 0:
        w1_d, b_w1d = k.dr(io, "w1", [NE, D, F], F32, "ExternalInput")
        w3_d, b_w3d = k.dr(io, "w3", [NE, D, F], F32, "ExternalInput")
        w2_d, b_w2d = k.dr(io, "w2", [NE, F, D], F32, "ExternalInput")
        hT = k.sb("hT", [128, FC, 512], BF16)
        b_hT = k.buf("hT")
        sil = k.sb("sil", [128, 2, 512])
        b_sil = k.bufs(2, "sil")
    if NE > 1:
        w1s, w3s, w2s = w1_d, w3_d, w2_d
        w1_d = k.nc.dram_tensor(f"w1_bf{k.nsb}", [NE, D, F], BF16, kind="Internal").ap()
        w3_d = k.nc.dram_tensor(f"w3_bf{k.nsb}", [NE, D, F], BF16, kind="Internal").ap()
        w2_d = k.nc.dram_tensor(f"w2_bf{k.nsb}", [NE, F, D], BF16, kind="Internal").ap()
        b_w1e, b_w3e, b_w2e = k.bufs(NE, "w1e"), k.bufs(NE, "w3e"), k.bufs(NE, "w2e")
        for ex in range(NE):
            k.dma("pool", w1_d[ex], w1s[ex], reads=[b_w1d], writes=[b_w1e[ex]])
            k.dma("pool", w3_d[ex], w3s[ex], reads=[b_w3d], writes=[b_w3e[ex]])
            k.dma("pool", w2_d[ex], w2s[ex], reads=[b_w2d], writes=[b_w2e[ex]])
        wr_d, b_wrd = k.dr(io, "w_router", [D, NE], F32, "ExternalInput")
        wr = k.sb("wr_sb", [128, KC, NE])
        b_wr = k.buf("wr")
        k.dma("sp", wr[:], wr_d.rearrange("(c p) n -> p c n", p=128), reads=[b_wrd], writes=[b_wr])
        u32 = k.sb("u32", [128, KC, 128])
        b_u32 = k.buf("u32")
        gates = k.sb("gates", [128, 4, NE])
        b_gates = k.bufs(4, "gates")
        if cfg.get("debug"):
            dbg_d, b_dbgd = k.dr(io, "dbg", [128, NTOK // 128, NE], F32, "ExternalOutput")
        rt = k.sb("rt", [128, 4, NE])
        rs = k.sb("rs", [128, 8])
        b_rt = k.buf("rt")

    accbank = [0, 1, 6, 7]

    def racc(j, hf, ms, gidx, bank, bb, ge=None):
        gate = cbc[:, 4 + 2 * ms + gidx, hf * 512:(hf + 1) * 512]
        tb = tt[:, (j + hf) % 2, :]
        b_tb = b_tt[(j + hf) % 2]
        dst = hb[:, j, hf * 512:(hf + 1) * 512]
        if ge is None:
            k.op("dve", lambda e: e.tensor_tensor(out=tb, in0=bank[:, :], in1=gate, op=ALU.mult),
                 reads=[bb, b_cbc], writes=[b_tb])
        else:
            gap, b_g = ge
            k.op("dve", lambda e: e.scalar_tensor_tensor(out=tb, in0=bank[:, :], scalar=gap, in1=gate,
                                                         op0=ALU.mult, op1=ALU.mult),
                 reads=[bb, b_cbc, b_g], writes=[b_tb])
        k.op("pool", lambda e: e.tensor_tensor(out=dst, in0=dst, in1=tb, op=ALU.add),
             reads=[b_tb, b_hb[j]], writes=[b_hb[j]])

    def scale_res(j):
        k.op("act", lambda e: e.mul(hb[:, j, :], hb[:, j, :], ALPHA), reads=[b_hb[j]], writes=[b_hb[j]])

    def ln_tile(j, which):
        emit_ln(cx, hb[:, j, :], b_hb[j], cbc[:, 2 * which, :], cbc[:, 2 * which + 1, :], b_cbc,
                hb[:, j, :], b_hb[j], (lnst, lnmv), b_lnt)

    def sc_proj_group(src_tiles, ntoks, ms, is_halo):
        N = sum(ntoks)

        def mod_step(w, bw):
            col = 0
            for (ap, bsrc), ntk in zip(src_tiles, ntoks):
                emit_modT(cx, ap, bsrc, ntk, uT, b_uT, col, cT[:, 4 * ms + 1, :], cT[:, 4 * ms + 0, :], b_cT)
                col += ntk
        cx.step(None, mod_step)
        for half in range(2):
            held = {}

            def fn_gc(w, bw, half=half):
                for cc in range(4):
                    bank, bb = cx.bank[2 + cc % 2], cx.b_bank[2 + cc % 2]
                    for kc in range(KC):
                        k.op("pe", lambda e, kc=kc, cc=cc, bank=bank: e.matmul(
                            bank[:, 0:N], lhsT=w[:, kc, cc * 128:(cc + 1) * 128], rhs=uT[:, kc, 0:N],
                            start=(kc == 0), stop=(kc == KC - 1)), reads=[bw, b_uT], writes=[bb])
                    k.op("act", lambda e, cc=cc, bank=bank: e.activation(
                        out=gcsb[:, cc, 0:N], in_=bank[:, 0:N], func=AF.Copy), reads=[bb], writes=[b_gcsb])

            def fn_h(w, bw, half=half):
                for cc in range(4):
                    c = half * 4 + cc
                    bank, bb = cx.bank[4 + cc % 2], cx.b_bank[4 + cc % 2]
                    for kc in range(KC):
                        k.op("pe", lambda e, kc=kc, cc=cc, bank=bank: e.matmul(
                            bank[:, 0:N], lhsT=w[:, kc, cc * 128:(cc + 1) * 128], rhs=uT[:, kc, 0:N],
                            start=(kc == 0), stop=(kc == KC - 1)), reads=[bw, b_uT], writes=[bb])
                    k.op("dve", lambda e, cc=cc, c=c, bank=bank: e.tensor_tensor(
                        out=vwin[:, c, 0:N], in0=bank[:, 0:N], in1=gcsb[:, cc, 0:N], op=ALU.mult),
                        reads=[bb, b_gcsb], writes=[b_vwin])
                    if is_halo and NH == 2:
                        k.op("dve", lambda e, c=c: e.tensor_tensor(
                            out=vwin[:, c, 0:N], in0=vwin[:, c, 0:N], in1=halv[:, 0:N], op=ALU.mult),
                            reads=[b_vwin, b_halv], writes=[b_vwin])

            def fn_gb(w, bw, half=half):
                for cc in range(4):
                    c = half * 4 + cc
                    bank, bb = cx.bank[2 + cc % 2], cx.b_bank[2 + cc % 2]
                    for kc in range(KC):
                        k.op("pe", lambda e, kc=kc, cc=cc, bank=bank: e.matmul(
                            bank[:, 0:N], lhsT=w[:, kc, cc * 128:(cc + 1) * 128], rhs=uT[:, kc, 0:N],
                            start=(kc == 0), stop=(kc == KC - 1)), reads=[bw, b_uT], writes=[bb])
                    k.op("act", lambda e, c=c, bank=bank: e.activation(
                        out=gbw[:, c, 0:N], in_=bank[:, 0:N], func=AF.Copy), reads=[bb], writes=[b_gbw])

            c0 = half * 512
            cx.step((scwin_d[:, D + c0:D + c0 + 512], b_scwind, KC), fn_gc)
            cx.step((scwin_d[:, 2 * D + c0:2 * D + c0 + 512], b_scwind, KC), fn_h)
            cx.step((scwin_d[:, c0:c0 + 512], b_scwind, KC), fn_gb)
        return N

    if mixer == "sc":
        gcsb = k.sb("gcsb", [128, 4, 512])
        b_gcsb = k.buf("gcsb")
        k.op("pool", lambda e: e.memset(zcol[:], 0.0), writes=[b_zcol])
        for zc in cfg["zero_cols"]:
            k.dma("sp", vT_d[:, zc:zc + 1].rearrange("(c p) n -> p c n", p=128), zcol[:, :, 0:1],
                  reads=[b_zcol], writes=[b_vTd], allow_slow_non_contiguous=True)
        k.dma("sp", halb[:], hal_d, reads=[b_hald], writes=[b_halb])

        def st1_store(N, vcols):
            def fn(w, bw):
                for (s0, n, d0) in vcols:
                    kw = dict(allow_slow_non_contiguous=True) if n == 1 else {}
                    k.dma("sp", vT_d[:, d0:d0 + n].rearrange("(c p) n -> p c n", p=128), vwin[:, :, s0:s0 + n],
                          reads=[b_vwin], writes=[b_vTd], **kw)
                    k.dma("sp", gbT_d[:, d0:d0 + n].rearrange("(c p) n -> p c n", p=128), gbw[:, :, s0:s0 + n],
                          reads=[b_gbw], writes=[b_gbTd], **kw)
            return fn

        hc = cfg["halo_cols"]
        sc_proj_group([(halb[0:NH, :], b_halb)], [NH], 0, True)
        if NH == 2:
            cx.step(None, st1_store(2, [(0, 1, hc[0]), (1, 1, hc[1])]))
        else:
            def halsel_step(w, bw):
                for s in range(2):
                    k.op("dve", lambda e, s=s: e.tensor_tensor(out=hsel[:], in0=vwin[:, :, 0:NH], in1=halsel[:, s, :, :],
                                                               op=ALU.mult), reads=[b_vwin, b_halsel], writes=[b_hsel])
                    k.op("dve", lambda e, s=s: e.reduce_sum(hv[:, s, :, 0], hsel[:], axis=AX.X), reads=[b_hsel], writes=[b_hv])
                    k.dma("sp", vT_d[:, hc[s]:hc[s] + 1].rearrange("(c p) n -> p c n", p=128), hv[:, s, :, :],
                          reads=[b_hv], writes=[b_vTd], allow_slow_non_contiguous=True)
            cx.step(None, halsel_step)
        for (tok0, nt, ms, vcol0) in groups:
            def ld(w, bw, tok0=tok0, nt=nt):
                k.dma("sp", hb[:, 0:nt, :], h_in[tok0:tok0 + nt * 128, :].rearrange("(t p) d -> p t d", p=128),
                      reads=[b_hin], writes=b_hb[0:nt])
            cx.step(None, ld)
            sc_proj_group([(hb[:, j, :], b_hb[j]) for j in range(nt)], [128] * nt, ms, False)
            if cfg.get("debug") and tok0 == 0:
                def dbgu(w, bw):
                    k.dma("sp", dbgu_d, uT[:], reads=[b_uT], writes=[b_dbgu])
                cx.step(None, dbgu)
            cx.step(None, st1_store(nt * 128, [(0, nt * 128, vcol0)]))

    for (tok0, nt, ms, vcol0) in groups:
        N = nt * 128

        def ld(w, bw, tok0=tok0, nt=nt):
            k.dma("sp", hb[:, 0:nt, :], h_in[tok0:tok0 + nt * 128, :].rearrange("(t p) d -> p t d", p=128),
                  reads=[b_hin], writes=b_hb[0:nt])
        cx.step(None, ld)

        if mixer == "even":
            def prep(w, bw, tok0=tok0, nt=nt):
                for j in range(nt):
                    k.dma("sp", mixb[:], mix_d[tok0 + j * 128:tok0 + (j + 1) * 128, :], reads=[b_mixd],
                          writes=[b_mixb])
                    emit_copyT(cx, mixb, b_mixb, 128, 16, mT, b_mT, j * 128)
                    scale_res(j)
            cx.step(None, prep)
            for hf in range(2):
                for kp in range(2):
                    def fn(w, bw, hf=hf, kp=kp, nt=nt, ms=ms):
                        for j in range(nt):
                            bank, bb = cx.bank[accbank[j]], cx.b_bank[accbank[j]]
                            for kc in range(KC):
                                k.op("pe", lambda e, kc=kc, j=j, bank=bank: e.matmul(
                                    bank[:, :], lhsT=mT[:, kp * KC + kc, j * 128:(j + 1) * 128], rhs=w[:, kc, :],
                                    start=(kp == 0 and kc == 0), stop=(kp == 1 and kc == KC - 1)),
                                    reads=[bw, b_mT], writes=[bb])
                            if kp == 1:
                                racc(j, hf, ms, 0, bank, bb)
                    cx.step((wout_d[kp * D:(kp + 1) * D, hf * 512:(hf + 1) * 512], b_woutd, KC), fn)
        elif mixer == "sc":
            def prep(w, bw, nt=nt, N=N, vcol0=vcol0, tok0=tok0):
                k.dma("sp", vwin[:, :, 0:N + 2], vT_d[:, vcol0 - 1:vcol0 + N + 1].rearrange("(c p) n -> p c n", p=128),
                      reads=[b_vTd], writes=[b_vwin])
                k.dma("sp", gbw[:, :, 0:N], gbT_d[:, vcol0:vcol0 + N].rearrange("(c p) n -> p c n", p=128),
                      reads=[b_gbTd], writes=[b_gbw])
                for c in range(KC):
                    eng = "dve"
                    a = cacc[:, c % 2, 0:N]
                    ba = b_cacc[c % 2]
                    k.op(eng, lambda e, c=c, a=a: e.tensor_scalar(out=a, in0=vwin[:, c, 0:N], scalar1=cw[:, c, 0:1],
                                                                  scalar2=None, op0=ALU.mult),
                         reads=[b_vwin, b_cw], writes=[ba])
                    for tap in (1, 2):
                        k.op(eng, lambda e, c=c, a=a, tap=tap: e.scalar_tensor_tensor(
                            out=a, in0=vwin[:, c, tap:tap + N], scalar=cw[:, c, tap:tap + 1], in1=a,
                            op0=ALU.mult, op1=ALU.add), reads=[b_vwin, b_cw, ba], writes=[ba])
                    k.op(eng, lambda e, c=c, a=a: e.tensor_tensor(out=mT[:, c, 0:N], in0=a, in1=gbw[:, c, 0:N],
                                                                  op=ALU.mult),
                         reads=[ba, b_gbw], writes=[b_mT])
                for j in range(nt):
                    scale_res(j)
                if cfg.get("debug") and tok0 == 0:
                    k.dma("sp", dbgm_d, mT[:], reads=[b_mT], writes=[b_dbgm])
                    k.dma("sp", dbgv_d, vwin[:], reads=[b_vwin], writes=[b_dbgv])
                    k.dma("sp", dbgg_d, gbw[:], reads=[b_gbw], writes=[b_dbgg])
            cx.step(None, prep)
            for hf in range(2):
                def fn(w, bw, hf=hf, nt=nt, ms=ms):
                    for j in range(nt):
                        bank, bb = cx.bank[accbank[j]], cx.b_bank[accbank[j]]
                        for kc in range(KC):
                            k.op("pe", lambda e, kc=kc, j=j, bank=bank: e.matmul(
                                bank[:, :], lhsT=mT[:, kc, j * 128:(j + 1) * 128], rhs=w[:, kc, :],
                                start=(kc == 0), stop=(kc == KC - 1)), reads=[bw, b_mT], writes=[bb])
                        racc(j, hf, ms, 0, bank, bb)
                cx.step((scwout_d[:, hf * 512:(hf + 1) * 512], b_scwoutd, KC), fn)
        elif mixer == "pre":
            def prep(w, bw, tok0=tok0, nt=nt, ms=ms):
                for j in range(nt):
                    scale_res(j)
                    if ms == 0:
                        sap, bsrc = ylat_d[tok0 + j * 128:tok0 + (j + 1) * 128, :], b_ylat
                    else:
                        sap, bsrc = yctx_d[j * 128:(j + 1) * 128, :], b_yctx
                    gate = cbc[:, 4 + 2 * ms, :]
                    k.dma("sp", ypre[:], sap, reads=[bsrc], writes=[b_ypre])
                    k.op("dve", lambda e, gate=gate: e.tensor_tensor(out=ypre[:], in0=ypre[:], in1=gate, op=ALU.mult),
                         reads=[b_ypre, b_cbc], writes=[b_ypre])
                    k.op("pool", lambda e, j=j: e.tensor_tensor(out=hb[:, j, :], in0=hb[:, j, :], in1=ypre[:], op=ALU.add),
                         reads=[b_ypre, b_hb[j]], writes=[b_hb[j]])
            cx.step(None, prep)
        if mixer is not None:
            def ln1(w, bw, nt=nt):
                for j in range(nt):
                    ln_tile(j, 0)
            cx.step(None, ln1)

        if NE > 0:
            def pre_ffn(w, bw, nt=nt, ms=ms):
                for j in range(nt):
                    if NE > 1:
                        emit_modT(cx, hb[:, j, :], b_hb[j], 128, uT, b_uT, j * 128, cT[:, 4 * ms + 3, :],
                                  cT[:, 4 * ms + 2, :], b_cT, dst32=None)
                        def evac(bank, bb, g, n, ms=ms):
                            for c in range(n):
                                cc = g + c
                                k.op("act", lambda e, c=c, cc=cc, bank=bank: e.activation(
                                    out=u32[:, cc, :], in_=bank[:, c * 128:(c + 1) * 128], func=AF.Identity,
                                    scale=cT[:, 4 * ms + 3, cc:cc + 1], bias=cT[:, 4 * ms + 2, cc:cc + 1]),
                                    reads=[bb, b_cT], writes=[b_u32])
                        emit_transposes(cx, hb[:, j, :], b_hb[j], 128, KC, evac)
                        lb, blb = cx.bank[6], cx.b_bank[6]
                        for kc in range(KC):
                            k.op("pe", lambda e, kc=kc: e.matmul(lb[:, 0:NE], lhsT=u32[:, kc, :], rhs=wr[:, kc, :],
                                                                 start=(kc == 0), stop=(kc == KC - 1)),
                                 reads=[b_u32, b_wr], writes=[blb])
                        L = rt[:, 0, :]
                        E1 = rt[:, 1, :]
                        L2 = rt[:, 2, :]
                        E2 = rt[:, 3, :]
                        G = gates[:, j, :]
                        dv = lambda f, r=(), w_=(): k.op("dve", f, reads=[b_rt] + list(r), writes=[b_rt] + list(w_))
                        dv(lambda e: e.tensor_copy(L, lb[:, 0:NE]), r=[blb])
                        dv(lambda e: e.reduce_max(rs[:, 0:1], L, axis=AX.X))
                        dv(lambda e: e.tensor_scalar(out=E1, in0=L, scalar1=rs[:, 0:1], scalar2=None, op0=ALU.is_equal))
                        dv(lambda e: e.scalar_tensor_tensor(out=L2, in0=E1, scalar=-1e30, in1=L, op0=ALU.mult,
                                                            op1=ALU.add))
                        dv(lambda e: e.reduce_max(rs[:, 1:2], L2, axis=AX.X))
                        dv(lambda e: e.tensor_scalar(out=E2, in0=L2, scalar1=rs[:, 1:2], scalar2=None,
                                                     op0=ALU.is_equal))
                        dv(lambda e: e.tensor_tensor(out=rs[:, 2:3], in0=rs[:, 1:2], in1=rs[:, 0:1], op=ALU.subtract))
                        k.op("act", lambda e: e.activation(out=rs[:, 3:4], in_=rs[:, 2:3], func=AF.Exp),
                             reads=[b_rt], writes=[b_rt])
                        dv(lambda e: e.tensor_scalar_add(rs[:, 4:5], rs[:, 3:4], 1.0))
                        dv(lambda e: e.reciprocal(rs[:, 5:6], rs[:, 4:5]))
                        dv(lambda e: e.tensor_tensor(out=rs[:, 6:7], in0=rs[:, 3:4], in1=rs[:, 5:6], op=ALU.mult))
                        dv(lambda e: e.tensor_scalar(out=E1, in0=E1, scalar1=rs[:, 5:6], scalar2=None, op0=ALU.mult))
                        dv(lambda e, G=G: e.scalar_tensor_tensor(out=G, in0=E2, scalar=rs[:, 6:7], in1=E1,
                                                                 op0=ALU.mult, op1=ALU.add), w_=[b_gates[j]])
                    else:
                        emit_modT(cx, hb[:, j, :], b_hb[j], 128, uT, b_uT, j * 128, cT[:, 4 * ms + 3, :],
                                  cT[:, 4 * ms + 2, :], b_cT)
                    scale_res(j)
            cx.step(None, pre_ffn)
            if cfg.get("debug") and NE > 1:
                def dbgst(w, bw, nt=nt, tok0=tok0):
                    k.dma("sp", dbg_d[:, tok0 // 128:tok0 // 128 + nt, :], gates[:, 0:nt, :], reads=b_gates[0:nt],
                          writes=[b_dbgd])
                cx.step(None, dbgst)

            p13 = [(c0, min(4, FC - c0)) for c0 in range(0, FC, 4)]
            p2 = []
            c0 = 0
            while c0 < FC:
                n = min(8 if (FC - c0) % 7 else 7, FC - c0)
                p2.append((c0, n))
                c0 += n
            for ex in range(NE):
                for (f0, nf) in p13:
                    hold = {}

                    def fn1(w, bw, hold=hold):
                        hold["w1"] = (w, bw)

                    def fn3(w3, bw3, hold=hold, f0=f0, nf=nf, N=N):
                        w1s, bw1 = hold["w1"]
                        for ff in range(nf):
                            fc = f0 + ff
                            A, bA = cx.bank[2 + fc % 2], cx.b_bank[2 + fc % 2]
                            B, bB = cx.bank[4 + fc % 2], cx.b_bank[4 + fc % 2]
                            for kc in range(KC):
                                k.op("pe", lambda e, kc=kc, ff=ff, A=A: e.matmul(
                                    A[:, 0:N], lhsT=w1s[:, kc, ff * 128:(ff + 1) * 128], rhs=uT[:, kc, 0:N],
                                    start=(kc == 0), stop=(kc == KC - 1)), reads=[bw1, b_uT], writes=[bA])
                            for kc in range(KC):
                                k.op("pe", lambda e, kc=kc, ff=ff, B=B: e.matmul(
                                    B[:, 0:N], lhsT=w3[:, kc, ff * 128:(ff + 1) * 128], rhs=uT[:, kc, 0:N],
                                    start=(kc == 0), stop=(kc == KC - 1)), reads=[bw3, b_uT], writes=[bB])
                            k.op("act", lambda e, fc=fc, A=A: e.activation(out=sil[:, fc % 2, 0:N], in_=A[:, 0:N],
                                                                            func=AF.Silu),
                                 reads=[bA], writes=[b_sil[fc % 2]])
                            k.op("dve", lambda e, fc=fc, B=B: e.tensor_tensor(
                                out=hT[:, fc, 0:N], in0=B[:, 0:N], in1=sil[:, fc % 2, 0:N], op=ALU.mult),
                                reads=[bB, b_sil[fc % 2]], writes=[b_hT])
                    cx.step((w1_d[ex, :, f0 * 128:(f0 + nf) * 128], b_w1e[ex] if NE > 1 else b_w1d, KC), fn1)
                    cx.step((w3_d[ex, :, f0 * 128:(f0 + nf) * 128], b_w3e[ex] if NE > 1 else b_w3d, KC), fn3)
                for hf in range(2):
                    for pi, (f0, nf) in enumerate(p2):
                        def fn2(w, bw, hf=hf, pi=pi, f0=f0, nf=nf, nt=nt, ms=ms, ex=ex):
                            for j in range(nt):
                                bank, bb = cx.bank[accbank[j]], cx.b_bank[accbank[j]]
                                for ff in range(nf):
                                    fc = f0 + ff
                                    k.op("pe", lambda e, ff=ff, fc=fc, j=j, bank=bank: e.matmul(
                                        bank[:, :], lhsT=hT[:, fc, j * 128:(j + 1) * 128], rhs=w[:, ff, :],
                                        start=(fc == 0), stop=(fc == FC - 1)), reads=[bw, b_hT], writes=[bb])
                                if pi == len(p2) - 1:
                                    ge = None if NE == 1 else (gates[:, j, ex:ex + 1], b_gates[j])
                                    racc(j, hf, ms, 1, bank, bb, ge)
                        cx.step((w2_d[ex, f0 * 128:(f0 + nf) * 128, hf * 512:(hf + 1) * 512],
                                 b_w2e[ex] if NE > 1 else b_w2d, nf), fn2)

            def ln2(w, bw, nt=nt):
                for j in range(nt):
                    ln_tile(j, 1)
            cx.step(None, ln2)

        def st(w, bw, tok0=tok0, nt=nt):
            k.dma("sp", h_out[tok0:tok0 + nt * 128, :].rearrange("(t p) d -> p t d", p=128), hb[:, 0:nt, :],
                  reads=b_hb[0:nt], writes=[b_hout])
        cx.step(None, st)
    cx.flush()
    if own:
        return k.finish()


def build_mods():
    k = KB()
    cT_d, b_cTd = k.dram("cT3", [128, KC, 3], F32, "ExternalInput")
    w_d, b_wd = k.dram("ada_w", [D, 3072], F32, "ExternalInput")
    bias_d, b_biasd = k.dram("ada_b3", [3, 3072], F32, "ExternalInput")
    out_d, b_outd = k.dram("mods", [3, 3072], F32, "ExternalOutput")
    s = k.sb("s", [128, KC, 3])
    b_s = k.buf()
    bias = k.sb("bias", [3, 3072])
    b_bias = k.buf()
    res = k.sb("res", [3, 3072])
    b_res = k.buf()
    wt = [k.sb(f"wt{i}", [128, KC, 512]) for i in range(2)]
    b_wt = k.bufs(2, "wt")
    bank = [k.ps(f"bk{i}", [128, 512]) for i in range(2)]
    b_bank = k.bufs(2, "bk")
    k.dma("sp", s[:], cT_d, reads=[b_cTd], writes=[b_s])
    k.dma("sp", bias[:], bias_d, reads=[b_biasd], writes=[b_bias])
    k.op("act", lambda e: e.activation(out=s[:], in_=s[:], func=AF.Silu), reads=[b_s], writes=[b_s])
    for blk in range(6):
        i = blk % 2
        k.dma("sp", wt[i][:], w_d[:, blk * 512:(blk + 1) * 512].rearrange("(c p) n -> p c n", p=128),
              reads=[b_wd], writes=[b_wt[i]])
        for kc in range(KC):
            k.op("pe", lambda e, kc=kc, i=i: e.matmul(bank[i][0:3, :], lhsT=s[:, kc, :], rhs=wt[i][:, kc, :],
                                                     start=(kc == 0), stop=(kc == KC - 1)),
                 reads=[b_s, b_wt[i]], writes=[b_bank[i]])
        k.op("dve", lambda e, blk=blk, i=i: e.tensor_tensor(out=res[:, blk * 512:(blk + 1) * 512], in0=bank[i][0:3, :],
                                                            in1=bias[:, blk * 512:(blk + 1) * 512], op=ALU.add),
             reads=[b_bank[i], b_bias], writes=[b_res])
    k.dma("sp", out_d, res[:], reads=[b_res], writes=[b_outd])
    return k.finish()


def build_proj(cfg):
    k = KB()
    cx = Ctx(k)
    groups = cfg["groups"]
    NTOK = cfg["ntok"]
    nms = cfg["nmod"]
    NCOL = cfg["ncol"]
    h_in, b_hin = k.dram("h_in", [NTOK, D], F32, "ExternalInput")
    w_d, b_wd = k.dram("w", [D, NCOL], F32, "ExternalInput")
    out_d, b_outd = k.dram("proj", [NTOK, NCOL], F32, "ExternalOutput")
    cT_d, b_cTd = k.dram("cT", [128, 2 * nms, KC], F32, "ExternalInput")
    cT = k.sb("cT_sb", [128, 2 * nms, KC])
    b_cT = k.buf("cT")
    k.dma("sp", cT[:], cT_d, reads=[b_cTd], writes=[b_cT])
    for m in range(nms):
        k.op("dve", lambda e, m=m: e.tensor_scalar_add(cT[:, 2 * m + 1, :], cT[:, 2 * m + 1, :], 1.0),
             reads=[b_cT], writes=[b_cT])
    hb = k.sb("hb", [128, 4, D])
    b_hb = k.bufs(4, "hb")
    uT = k.sb("uT", [128, KC, 512], BF16)
    b_uT = k.buf("uT")
    ob = k.sb("ob", [128, 4, 2, 512])
    b_ob = [k.bufs(2, f"ob{j}") for j in range(4)]
    pieces = [(c0, min(512, NCOL - c0)) for c0 in range(0, NCOL, 512)]
    for (tok0, nt, ms) in groups:
        def ld(w, bw, tok0=tok0, nt=nt, ms=ms):
            k.dma("sp", hb[:, 0:nt, :], h_in[tok0:tok0 + nt * 128, :].rearrange("(t p) d -> p t d", p=128),
                  reads=[b_hin], writes=b_hb[0:nt])
            for j in range(nt):
                emit_modT(cx, hb[:, j, :], b_hb[j], 128, uT, b_uT, j * 128, cT[:, 2 * ms + 1, :], cT[:, 2 * ms, :], b_cT)
        cx.step(None, ld)
        for pi, (c0, nc_) in enumerate(pieces):
            def fn(w, bw, pi=pi, c0=c0, nc_=nc_, nt=nt, tok0=tok0):
                for j in range(nt):
                    bi = 2 + (pi * 4 + j) % 4
                    bank, bb = cx.bank[bi], cx.b_bank[bi]
                    for kc in range(KC):
                        k.op("pe", lambda e, kc=kc, j=j, bank=bank: e.matmul(
                            bank[:, 0:nc_], lhsT=uT[:, kc, j * 128:(j + 1) * 128], rhs=w[:, kc, 0:nc_],
                            start=(kc == 0), stop=(kc == KC - 1)), reads=[bw, b_uT], writes=[bb])
                    o = ob[:, j, pi % 2, 0:nc_]
                    bo = b_ob[j][pi % 2]
                    eng = "act" if (pi + j) % 2 == 0 else "dve"
                    if eng == "act":
                        k.op("act", lambda e, o=o, bank=bank: e.activation(out=o, in_=bank[:, 0:nc_], func=AF.Copy),
                             reads=[bb], writes=[bo])
                    else:
                        k.op("dve", lambda e, o=o, bank=bank: e.tensor_copy(o, bank[:, 0:nc_]), reads=[bb], writes=[bo])
                    k.dma("sp", out_d[tok0 + j * 128:tok0 + (j + 1) * 128, c0:c0 + nc_], o, reads=[bo], writes=[k.buf()])
            cx.step((w_d[:, c0:c0 + nc_], b_wd, KC), fn)
    cx.flush()
    return k.finish()


GRID_W = 64
NEG = -30000.0


def na_block_info(rows):
    info = []
    for t in range(rows // 2):
        r = 2 * t
        if r == 0:
            cls = 0
        elif r == 2:
            cls = 1
        elif r == rows - 4:
            cls = 3
        elif r == rows - 2:
            cls = 4
        else:
            cls = 2
        kt0 = min(max(r - 4, 0), rows - 10) // 2
        info.append((cls, kt0))
    return info


def na_mask_bias(rpb4, rows):
    out = np.full((5, 5, 128, 4, 128), NEG, np.float32)
    reps = [0, 2, 4, rows - 4, rows - 2]
    qc = np.arange(64)
    c0 = np.clip(qc - 8, 0, 64 - 16)
    for ci, r in enumerate(reps):
        R0 = min(max(r - 4, 0), rows - 10)
        for dq in range(2):
            qr = r + dq
            r0 = min(max(qr - 4, 0), rows - 8)
            for kr in range(r0, r0 + 8):
                lk = kr - R0
                assert 0 <= lk < 10
                for j in range(16):
                    kc = c0 + j
                    key = lk * 64 + kc
                    q = dq * 64 + qc
                    out[ci, key // 128, key % 128, :, q] = rpb4[:, kr - qr + 7, kc - qc + 15].T
    return out


def build_na(cfg, k=None, cx=None, io=None):
    own = k is None
    if own:
        k = KB()
        cx = Ctx(k)
    rows = cfg["rows"]
    NL = rows * GRID_W
    NCTX = 256
    with_ctx = cfg["with_ctx"]
    NQ = NL + (NCTX if with_ctx else 0)
    info = na_block_info(rows)
    qT_d, b_qTd = k.dr(io, "qT", [4, 64, NQ], F32, "ExternalInput")
    kT_d, b_kTd = k.dr(io, "kT", [4, 64, NL], F32, "ExternalInput")
    kcT_d, b_kcTd = k.dr(io, "kcT", [4, 64, NCTX], F32, "ExternalInput")
    v_d, b_vd = k.dr(io, "v", [NL, 256], F32, "ExternalInput")
    vc_d, b_vcd = k.dr(io, "vc", [NCTX, 256], F32, "ExternalInput")
    mb_d, b_mbd = k.dr(io, "mb", [25, 128, 512], F32, "ExternalInput")
    out_d, b_outd = k.dr(io, "attn", [NQ, 256], F32, "ExternalOutput")

    identb = k.sb("identb", [128, 128], BF16)
    b_identb = k.buf()
    k.op("dve", lambda e: e.tensor_copy(identb[:], cx.ident[:]), reads=[cx.b_ident], writes=[b_identb])
    mb = k.sb("mb_sb", [128, 25, 512], BF16)
    b_mb = k.buf()
    for c in range(5):
        k.dma("pool", mb[:, c * 5:(c + 1) * 5, :], mb_d[c * 5:(c + 1) * 5].rearrange("c p n -> p c n"),
              reads=[b_mbd], writes=[b_mb])
    kc = k.sb("kc_sb", [64, 4, NCTX], BF16)
    b_kc = k.buf()
    k.dma("pool", kc[:], kcT_d.rearrange("h p n -> p h n"), reads=[b_kcTd], writes=[b_kc])
    vcw = k.sb("vcw", [128, 2, 4, 66], BF16)
    b_vcw = k.buf()
    k.op("pool", lambda e: e.memset(vcw[:], 1.0), writes=[b_vcw])
    for c in range(2):
        k.dma("pool", vcw[:, c, :, 0:64], vc_d[c * 128:(c + 1) * 128, :].rearrange("p (h d) -> p h d", h=4),
              reads=[b_vcd], writes=[b_vcw])
    qb32 = [k.sb(f"qb32_{i}", [64, 4, 128]) for i in range(2)]
    b_qb32 = k.bufs(2, "qb32")
    qb = [k.sb(f"qb_{i}", [64, 4, 128], BF16) for i in range(2)]
    b_qb = k.bufs(2, "qb")
    kw = [k.sb(f"kw_{i}", [64, 4, 640], BF16) for i in range(2)]
    b_kw = k.bufs(2, "kw")
    vw = [k.sb(f"vw_{i}", [128, 5, 4, 66], BF16) for i in range(2)]
    b_vw = k.bufs(2, "vw")
    for i in range(2):
        k.op("pool", lambda e, i=i: e.memset(vw[i][:], 1.0), writes=[b_vw[i]])
    PT = [k.sb(f"PT_{i}", [128, 7, 512], BF16) for i in range(2)]
    b_PT = k.bufs(2, "PT")
    rs = k.sb("rs", [128, 2, 4])
    b_rs = k.bufs(2, "rs")
    ob = [k.sb(f"ob_{i}", [128, 4, 64]) for i in range(2)]
    b_ob = k.bufs(2, "ob")

    blocks = [(t, True) for t in range(len(info))]
    if with_ctx:
        blocks += [(NL // 128, False), (NL // 128 + 1, False)]
    sbank = 0
    for bi, (t, local) in enumerate(blocks):
        i = bi % 2
        k.dma("sp", qb32[i][:], qT_d[:, :, t * 128:(t + 1) * 128].rearrange("h p n -> p h n"), reads=[b_qTd],
              writes=[b_qb32[i]])
        k.op("act", lambda e, i=i: e.mul(qb[i][:], qb32[i][:], 0.125), reads=[b_qb32[i]], writes=[b_qb[i]])
        chunks = []
        if local:
            cls, kt0 = info[t]
            k.dma("pool", kw[i][:], kT_d[:, :, kt0 * 128:kt0 * 128 + 640].rearrange("h p n -> p h n"),
                  reads=[b_kTd], writes=[b_kw[i]])
            for c in range(5):
                k.dma("pool", vw[i][:, c, :, 0:64],
                      v_d[(kt0 + c) * 128:(kt0 + c + 1) * 128, :].rearrange("p (h d) -> p h d", h=4),
                      reads=[b_vd], writes=[b_vw[i]])
            for c in range(5):
                chunks.append(("l", c, cls))
        chunks += [("c", 0, 0), ("c", 1, 0)]
        nch = len(chunks)
        for ci, (kind, c, cls) in enumerate(chunks):
            bk = 2 + sbank % 4
            sbank += 1
            bank, bb = cx.bank[bk], cx.b_bank[bk]
            if kind == "l":
                k.op("pe", lambda e, bank=bank, c=c, cls=cls: e.matmul(bank[:, :], lhsT=identb[:], rhs=mb[:, cls * 5 + c, :],
                                                                      start=True, stop=False),
                     reads=[b_identb, b_mb], writes=[bb])
            for h in range(4):
                if kind == "l":
                    lhsT = kw[i][:, h, c * 128:(c + 1) * 128]
                    rd = [b_kw[i], b_qb[i]]
                else:
                    lhsT = kc[:, h, c * 128:(c + 1) * 128]
                    rd = [b_kc, b_qb[i]]
                k.op("pe", lambda e, bank=bank, h=h, lhsT=lhsT, i=i, kind=kind: e.matmul(
                    bank[:, h * 128:(h + 1) * 128], lhsT=lhsT, rhs=qb[i][:, h, :],
                    start=(kind == "c"), stop=(h == 3 or kind == "c")), reads=rd, writes=[bb])
            k.op("act", lambda e, bank=bank, ci=ci, i=i: e.activation(out=PT[i][:, ci, :], in_=bank[:, :], func=AF.Exp),
                 reads=[bb], writes=[b_PT[i]])
        obk = 6 + i
        obank, bob = cx.bank[obk], cx.b_bank[obk]
        for h in range(4):
            for ci, (kind, c, cls) in enumerate(chunks):
                rhs = vw[i][:, c, h, 0:65] if kind == "l" else vcw[:, c, h, 0:65]
                rd = [b_PT[i], b_vw[i] if kind == "l" else b_vcw]
                k.op("pe", lambda e, h=h, ci=ci, rhs=rhs, i=i, obank=obank: e.matmul(
                    obank[:, h * 65:(h + 1) * 65], lhsT=PT[i][:, ci, h * 128:(h + 1) * 128], rhs=rhs,
                    start=(ci == 0), stop=(ci == nch - 1)), reads=rd, writes=[bob])
        ov = obank[:, 0:260].rearrange("p (h d) -> p h d", h=4)
        k.op("dve", lambda e, ov=ov, i=i: e.reciprocal(rs[:, i, :], ov[:, :, 64]), reads=[bob], writes=[b_rs[i]])
        for h in range(4):
            k.op("dve", lambda e, ov=ov, i=i, h=h: e.tensor_scalar(out=ob[i][:, h, :], in0=ov[:, h, 0:64],
                                                                   scalar1=rs[:, i, h:h + 1], scalar2=None, op0=ALU.mult),
                 reads=[bob, b_rs[i]], writes=[b_ob[i]])
        k.dma("sp", out_d[t * 128:(t + 1) * 128, :], ob[i][:].rearrange("p h d -> p (h d)"), reads=[b_ob[i]],
              writes=[k.buf()])
    if own:
        return k.finish()


RMS_EPS = 1e-5


def build_ssd(cfg, k=None, cx=None, io=None):
    own = k is None
    if own:
        k = KB()
        cx = Ctx(k)
    NLAT = cfg["nlat"]
    L = 256 + NLAT
    NCH = L // 128
    LP = 260 + NLAT + 4
    xbc_d, b_xbcd = k.dr(io, "xbcT", [4, 128, LP], F32, "ExternalInput")
    cw_d, b_cwd = k.dr(io, "cwT", [128, 4, 5], F32, "ExternalInput")
    cb_d, b_cbd = k.dr(io, "cb", [128, 4], F32, "ExternalInput")
    dtr_d, b_dtrd = k.dr(io, "dtr", [L, 8], F32, "ExternalInput")
    sm_d, b_smd = k.dr(io, "small", [128, 16], F32, "ExternalInput")
    dn_d, b_dnd = k.dr(io, "dn", [128, 2, 256], F32, "ExternalInput")
    z_d, b_zd = k.dr(io, "z", [L, 256], F32, "ExternalInput")
    out_d, b_outd = k.dr(io, "ssm", [L, 256], F32, "ExternalOutput")
    yf_d, b_yfd = k.dr(io, "yf_scr", [L, 256], F32, "Internal")
    if cfg.get("debug"):
        dbgy_d, _ = k.dr(io, "dbgy", [L, 256], F32, "ExternalOutput")

    def const(name, shape, dt=F32):
        return k.sb(name + "_sb", shape, dt), k.buf(name)

    def const2(name, shape, dt=F32):
        return [k.sb(f"{name}{i}_sb", shape, dt) for i in range(2)], [k.buf(f"{name}{i}") for i in range(2)]
    cw, b_cw = const("cw", [128, 4, 5])
    cb, b_cb = const("cb", [128, 4])
    sm, b_sm = const("sm", [128, 16])
    dn, b_dn = const("dn", [128, 2, 256])
    k.dma("sp", cw[:], cw_d, reads=[b_cwd], writes=[b_cw])
    k.dma("sp", cb[:], cb_d, reads=[b_cbd], writes=[b_cb])
    k.dma("sp", sm[:], sm_d, reads=[b_smd], writes=[b_sm])
    k.dma("sp", dn[:], dn_d, reads=[b_dnd], writes=[b_dn])
    k.op("act", lambda e: e.activation(out=sm[:, 8:16], in_=sm[:, 8:16], func=AF.Exp), reads=[b_sm], writes=[b_sm])
    k.op("act", lambda e: e.mul(sm[:, 8:16], sm[:, 8:16], -1.0), reads=[b_sm], writes=[b_sm])
    ones, b_ones = const("ones", [128, 128])
    k.op("pool", lambda e: e.memset(ones[:], 1.0), writes=[b_ones])
    tri, mneg = [], []
    b_tri = k.buf("tri")
    for d, (cmp_, tin, tfill, min_, mfill) in enumerate(((ALU.is_gt, 0.0, 1.0, NEG, 0.0), (ALU.is_ge, 1.0, 0.0, 0.0, NEG))):
        t_ = k.sb(f"tri{d}", [128, 128])
        m_ = k.sb(f"mneg{d}", [128, 128])
        k.op("pool", lambda e, t_=t_, tin=tin: e.memset(t_[:], tin), writes=[b_tri])
        k.op("pool", lambda e, t_=t_, cmp_=cmp_, tfill=tfill: e.affine_select(
            out=t_[:], in_=t_[:], pattern=[[-1, 128]], compare_op=cmp_, fill=tfill, base=0, channel_multiplier=1),
            reads=[b_tri], writes=[b_tri])
        k.op("pool", lambda e, m_=m_, min_=min_: e.memset(m_[:], min_), writes=[b_tri])
        k.op("pool", lambda e, m_=m_, cmp_=cmp_, mfill=mfill: e.affine_select(
            out=m_[:], in_=m_[:], pattern=[[-1, 128]], compare_op=cmp_, fill=mfill, base=0, channel_multiplier=1),
            reads=[b_tri], writes=[b_tri])
        tri.append(t_)
        mneg.append(m_)

    xw_2, b_xw_2 = const2("xw", [128, 4, 132])
    acc, b_acc = const("acc", [128, 128])
    xs_2, b_xs_2 = const2("xs", [128, 4, 128])
    bcb_2, b_bcb_2 = const2("bcb", [128, 2, 128], BF16)
    xtm_2, b_xtm_2 = const2("xtm", [128, 256])
    btm_2, b_btm_2 = const2("btm", [128, 128], BF16)
    dt_2, b_dt_2 = const2("dt", [128, 8])
    la_2, b_la_2 = const2("la", [128, 8])
    cbT_2, b_cbT_2 = const2("cbT", [128, 128])
    labc_2, b_labc_2 = const2("labc", [128, 128])
    ccol_2, b_ccol_2 = const2("ccol", [128, 1])
    seg_2, b_seg_2 = const2("seg", [128, 128])
    LT_2, b_LT_2 = const2("LT", [128, 128])
    Ebc_2, b_Ebc_2 = const2("Ebc", [128, 128])
    MT_2, b_MT_2 = const2("MT", [128, 128], BF16)
    CsT_2, b_CsT_2 = const2("CsT", [128, 128], BF16)
    xdt_2, b_xdt_2 = const2("xdt", [128, 64], BF16)
    xwt_2, b_xwt_2 = const2("xwt", [128, 64], BF16)
    S = k.sb("S", [128, 4, 64])
    Sbf = k.sb("Sbf", [128, 4, 64], BF16)
    b_S = k.bufs(4, "S")
    b_Sbf = k.bufs(4, "Sbf")
    ysb, b_ysb = const("ysb", [128, 256])
    zt, b_zt = const("zt", [128, 256])
    yft, b_yft = const("yft", [128, 256])
    t2, b_t2 = const("t2", [128, 256])
    bst, b_bst = const("bst", [128, 6])
    bmv, b_bmv = const("bmv", [128, 4])
    B0, bB0 = cx.bank[0], cx.b_bank[0]
    B1, bB1 = cx.bank[1], cx.b_bank[1]
    BY, bBY = cx.bank[4], cx.b_bank[4]
    BS, bBS = cx.bank[5], cx.b_bank[5]

    def colof(c):
        return 2 + c * 128 if c < 2 else 262 + (c - 2) * 128

    def run_dir(d):
        last = 127 if d == 0 else 0
        order = list(range(NCH)) if d == 0 else [1, 0] + list(range(NCH - 1, 1, -1))
        for h in range(4):
            k.op("pool", lambda e, h=h: e.memset(S[:, h, :], 0.0), reads=[b_S[h]], writes=[b_S[h]])
            k.op("pool", lambda e, h=h: e.memset(Sbf[:, h, :], 0.0), reads=[b_Sbf[h]], writes=[b_Sbf[h]])
        for ci, c in enumerate(order):
            c0 = colof(c)
            p = ci % 2
            chunk_body(d, last, c, c0, xw_2[p], b_xw_2[p], xs_2[p], b_xs_2[p], bcb_2[p], b_bcb_2[p], xtm_2[p], b_xtm_2[p],
                       btm_2[p], b_btm_2[p], dt_2[p], b_dt_2[p], la_2[p], b_la_2[p], cbT_2[p], b_cbT_2[p])

    def chunk_body(d, last, c, c0, xw, b_xw, xs, b_xs, bcb, b_bcb, xtm, b_xtm, btm, b_btm, dt, b_dt, la, b_la, cbT, b_cbT):
        if True:
            k.dma("sp", xw[:], xbc_d[:, :, c0 - 2:c0 + 130].rearrange("c p n -> p c n"), reads=[b_xbcd], writes=[b_xw])
            k.dma("sp", dt[:], dtr_d[c * 128:(c + 1) * 128, :], reads=[b_dtrd], writes=[b_dt])
            if d == 1:
                k.dma("sp", zt[:], z_d[c * 128:(c + 1) * 128, :], reads=[b_zd], writes=[b_zt])
                k.dma("sp", yft[:], yf_d[c * 128:(c + 1) * 128, :], reads=[b_yfd], writes=[b_yft])
            for cc in range(4):
                k.op("dve", lambda e, cc=cc: e.tensor_scalar(out=acc[:], in0=xw[:, cc, 0:128], scalar1=cw[:, cc, 0:1],
                                                             scalar2=None, op0=ALU.mult),
                     reads=[b_xw, b_cw], writes=[b_acc])
                for tap in range(1, 5):
                    k.op("dve", lambda e, cc=cc, tap=tap: e.scalar_tensor_tensor(
                        out=acc[:], in0=xw[:, cc, tap:tap + 128], scalar=cw[:, cc, tap:tap + 1], in1=acc[:],
                        op0=ALU.mult, op1=ALU.add), reads=[b_xw, b_cw, b_acc], writes=[b_acc])
                k.op("act", lambda e, cc=cc: e.activation(out=xs[:, cc, :], in_=acc[:], func=AF.Silu, bias=cb[:, cc:cc + 1]),
                     reads=[b_acc, b_cb], writes=[b_xs])
            k.op("pool", lambda e: e.tensor_copy(bcb[:], xs[:, 2:4, :]), reads=[b_xs], writes=[b_bcb])
            for cc in range(3):
                k.op("pe", lambda e, cc=cc: e.transpose(B0[:, cc * 128:(cc + 1) * 128], xs[:, cc, :], cx.ident[:]),
                     reads=[b_xs, cx.b_ident], writes=[bB0])
            k.op("act", lambda e: e.activation(out=xtm[:], in_=B0[:, 0:256], func=AF.Copy), reads=[bB0], writes=[b_xtm])
            k.op("act", lambda e: e.activation(out=btm[:], in_=B0[:, 256:384], func=AF.Copy), reads=[bB0], writes=[b_btm])
            k.op("dve", lambda e: e.tensor_tensor(out=dt[:], in0=dt[:], in1=sm[:, 0:8], op=ALU.add), reads=[b_dt, b_sm],
                 writes=[b_dt])
            k.op("act", lambda e: e.activation(out=dt[:], in_=dt[:], func=AF.Exp), reads=[b_dt], writes=[b_dt])
            k.op("dve", lambda e: e.tensor_scalar_add(dt[:], dt[:], 1.0), reads=[b_dt], writes=[b_dt])
            k.op("act", lambda e: e.activation(out=dt[:], in_=dt[:], func=AF.Ln), reads=[b_dt], writes=[b_dt])
            k.op("dve", lambda e: e.tensor_tensor(out=la[:], in0=dt[:], in1=sm[:, 8:16], op=ALU.mult), reads=[b_dt, b_sm],
                 writes=[b_la])
            k.op("pe", lambda e: e.matmul(B1[:, 0:128], lhsT=bcb[:, 0, :], rhs=bcb[:, 1, :], start=True, stop=True),
                 reads=[b_bcb], writes=[bB1])
            k.op("dve", lambda e: e.tensor_copy(cbT[:], B1[:, 0:128]), reads=[bB1], writes=[b_cbT])
            for h in range(4):
                head_body(d, last, h, xs, b_xs, xtm, b_xtm, btm, b_btm, dt, b_dt, la, b_la, cbT, b_cbT)
            tail_body(d, c, xtm, b_xtm)

    def head_body(d, last, h, xs, b_xs, xtm, b_xtm, btm, b_btm, dt, b_dt, la, b_la, cbT, b_cbT):
        if True:
            if True:
                hp = h % 2
                labc, b_labc = labc_2[hp], b_labc_2[hp]
                ccol, b_ccol = ccol_2[hp], b_ccol_2[hp]
                seg, b_seg = seg_2[hp], b_seg_2[hp]
                LT, b_LT = LT_2[hp], b_LT_2[hp]
                Ebc, b_Ebc = Ebc_2[hp], b_Ebc_2[hp]
                MT, b_MT = MT_2[hp], b_MT_2[hp]
                CsT, b_CsT = CsT_2[hp], b_CsT_2[hp]
                xdt, b_xdt = xdt_2[hp], b_xdt_2[hp]
                xwt, b_xwt = xwt_2[hp], b_xwt_2[hp]
                dc = d * 4 + h
                BA, bBA = cx.bank[2 + h % 2], cx.b_bank[2 + h % 2]
                k.op("act", lambda e, dc=dc: e.activation(out=labc[:], in_=ones[:], func=AF.Copy, scale=la[:, dc:dc + 1]),
                     reads=[b_ones, b_la], writes=[b_labc])
                k.op("pe", lambda e, BA=BA: e.matmul(BA[:, 0:128], lhsT=labc[:], rhs=tri[d][:], start=True, stop=True),
                     reads=[b_labc, b_tri], writes=[bBA])
                k.op("pe", lambda e, BA=BA, dc=dc: e.matmul(BA[:, 128:129], lhsT=tri[d][:], rhs=la[:, dc:dc + 1],
                                                            start=True, stop=True), reads=[b_la, b_tri], writes=[bBA])
                k.op("dve", lambda e, BA=BA: e.tensor_copy(ccol[:], BA[:, 128:129]), reads=[bBA], writes=[b_ccol])
                k.op("dve", lambda e, BA=BA: e.scalar_tensor_tensor(out=seg[:], in0=BA[:, 0:128], scalar=ccol[:, 0:1],
                                                                    in1=mneg[d][:], op0=ALU.subtract, op1=ALU.add),
                     reads=[bBA, b_ccol, b_tri], writes=[b_seg])
                k.op("act", lambda e: e.activation(out=LT[:], in_=seg[:], func=AF.Exp), reads=[b_seg], writes=[b_LT])
                k.op("act", lambda e, BA=BA: e.activation(out=Ebc[:], in_=BA[:, 0:128], func=AF.Exp), reads=[bBA],
                     writes=[b_Ebc])
                k.op("pool", lambda e: e.tensor_tensor(out=MT[:], in0=LT[:], in1=cbT[:], op=ALU.mult),
                     reads=[b_LT, b_cbT], writes=[b_MT])
                k.op("pool", lambda e: e.tensor_tensor(out=CsT[:], in0=xs[:, 3, :], in1=Ebc[:], op=ALU.mult),
                     reads=[b_xs, b_Ebc], writes=[b_CsT])
                k.op("dve", lambda e, h=h, dc=dc: e.tensor_scalar(out=xdt[:], in0=xtm[:, h * 64:(h + 1) * 64],
                                                                 scalar1=dt[:, dc:dc + 1], scalar2=None, op0=ALU.mult),
                     reads=[b_xtm, b_dt], writes=[b_xdt])
                k.op("dve", lambda e, h=h, dc=dc: e.tensor_scalar(out=xwt[:], in0=xtm[:, h * 64:(h + 1) * 64],
                                                                 scalar1=dt[:, dc:dc + 1], scalar2=LT[:, last:last + 1],
                                                                 op0=ALU.mult, op1=ALU.mult),
                     reads=[b_xtm, b_dt, b_LT], writes=[b_xwt])
                k.op("pe", lambda e, h=h: e.matmul(BY[:, h * 64:(h + 1) * 64], lhsT=MT[:], rhs=xdt[:], start=True, stop=False),
                     reads=[b_MT, b_xdt], writes=[bBY])
                k.op("pe", lambda e, h=h: e.matmul(BY[:, h * 64:(h + 1) * 64], lhsT=CsT[:], rhs=Sbf[:, h, :], start=False,
                                                   stop=True), reads=[b_CsT, b_Sbf[h]], writes=[bBY])
                k.op("pe", lambda e, h=h: e.matmul(BS[:, h * 64:(h + 1) * 64], lhsT=btm[:], rhs=xwt[:], start=True, stop=True),
                     reads=[b_btm, b_xwt], writes=[bBS])
                k.op("dve", lambda e, h=h: e.scalar_tensor_tensor(out=S[:, h, :], in0=S[:, h, :], scalar=Ebc[:, last:last + 1],
                                                                  in1=BS[:, h * 64:(h + 1) * 64], op0=ALU.mult, op1=ALU.add),
                     reads=[b_S[h], b_Ebc, bBS], writes=[b_S[h]])
                k.op("pool", lambda e, h=h: e.tensor_copy(Sbf[:, h, :], S[:, h, :]), reads=[b_S[h]], writes=[b_Sbf[h]])

    def tail_body(d, c, xtm, b_xtm):
        if True:
            if d == 0:
                k.op("act", lambda e: e.activation(out=ysb[:], in_=BY[:, 0:256], func=AF.Copy), reads=[bBY], writes=[b_ysb])
                k.dma("sp", yf_d[c * 128:(c + 1) * 128, :], ysb[:], reads=[b_ysb], writes=[b_yfd])
                if cfg.get("debug"):
                    k.dma("sp", dbgy_d[c * 128:(c + 1) * 128, :], ysb[:], reads=[b_ysb], writes=[k.buf()])
            else:
                k.op("dve", lambda e: e.tensor_tensor(out=ysb[:], in0=BY[:, 0:256], in1=yft[:], op=ALU.add),
                     reads=[bBY, b_yft], writes=[b_ysb])
                k.op("pool", lambda e: e.tensor_tensor(out=t2[:], in0=xtm[:], in1=dn[:, 0, :], op=ALU.mult),
                     reads=[b_xtm, b_dn], writes=[b_t2])
                k.op("dve", lambda e: e.tensor_tensor(out=ysb[:], in0=ysb[:], in1=t2[:], op=ALU.add),
                     reads=[b_ysb, b_t2], writes=[b_ysb])
                k.op("act", lambda e: e.activation(out=zt[:], in_=zt[:], func=AF.Silu), reads=[b_zt], writes=[b_zt])
                k.op("dve", lambda e: e.tensor_tensor(out=ysb[:], in0=ysb[:], in1=zt[:], op=ALU.mult),
                     reads=[b_ysb, b_zt], writes=[b_ysb])
                k.op("dve", lambda e: e.bn_stats(bst[:], ysb[:]), reads=[b_ysb], writes=[b_bst])
                k.op("dve", lambda e: e.bn_aggr(bmv[:, 0:2], bst[:]), reads=[b_bst], writes=[b_bmv])
                k.op("dve", lambda e: e.scalar_tensor_tensor(out=bmv[:, 2:3], in0=bmv[:, 0:1], scalar=bmv[:, 0:1],
                                                             in1=bmv[:, 1:2], op0=ALU.mult, op1=ALU.add),
                     reads=[b_bmv], writes=[b_bmv])
                k.op("dve", lambda e: e.tensor_scalar_add(bmv[:, 2:3], bmv[:, 2:3], RMS_EPS), reads=[b_bmv], writes=[b_bmv])
                k.op("act", lambda e: e.activation(out=bmv[:, 3:4], in_=bmv[:, 2:3], func=AF.Ln), reads=[b_bmv], writes=[b_bmv])
                k.op("act", lambda e: e.activation(out=bmv[:, 3:4], in_=bmv[:, 3:4], func=AF.Exp, scale=-0.5), reads=[b_bmv],
                     writes=[b_bmv])
                k.op("dve", lambda e: e.tensor_scalar(out=ysb[:], in0=ysb[:], scalar1=bmv[:, 3:4], scalar2=None, op0=ALU.mult),
                     reads=[b_ysb, b_bmv], writes=[b_ysb])
                k.op("pool", lambda e: e.tensor_tensor(out=ysb[:], in0=ysb[:], in1=dn[:, 1, :], op=ALU.mult),
                     reads=[b_ysb, b_dn], writes=[b_ysb])
                k.dma("sp", out_d[c * 128:(c + 1) * 128, :], ysb[:], reads=[b_ysb], writes=[k.buf()])

    run_dir(0)
    run_dir(1)
    if own:
        return k.finish()


def emit_mods_all(k, cx, io):
    cT_d, b_cTd = io["cT2"]
    w_d, b_wd = io["ada_w"]
    bias_d, b_biasd = io["ada_b2"]
    out_d, b_outd = io["mods_s"]
    s = k.sb("s", [128, KC, 2])
    b_s = k.buf()
    bias = k.sb("bias", [2, 6144])
    b_bias = k.buf()
    res = k.sb("res", [2, 6144])
    b_res = k.buf()
    wt = [k.sb(f"wt{i}", [128, KC, 512]) for i in range(2)]
    b_wt = k.bufs(2, "wt")
    k.dma("sp", s[:], cT_d, reads=[b_cTd], writes=[b_s])
    k.op("act", lambda e: e.activation(out=s[:], in_=s[:], func=AF.Silu), reads=[b_s], writes=[b_s])
    n = 0
    for li in range(4):
        k.dma("sp", bias[:], bias_d[li], reads=[b_biasd], writes=[b_bias])
        for blk in range(12):
            i = n % 2
            n += 1
            bank, bb = cx.bank[2 + i], cx.b_bank[2 + i]
            k.dma("sp", wt[i][:], w_d[li, :, blk * 512:(blk + 1) * 512].rearrange("(c p) n -> p c n", p=128),
                  reads=[b_wd], writes=[b_wt[i]])
            for kc in range(KC):
                k.op("pe", lambda e, kc=kc, i=i, bank=bank: e.matmul(bank[0:2, :], lhsT=s[:, kc, :], rhs=wt[i][:, kc, :],
                                                                    start=(kc == 0), stop=(kc == KC - 1)),
                     reads=[b_s, b_wt[i]], writes=[bb])
            k.op("dve", lambda e, blk=blk, bank=bank: e.tensor_tensor(out=res[:, blk * 512:(blk + 1) * 512], in0=bank[0:2, :],
                                                                      in1=bias[:, blk * 512:(blk + 1) * 512], op=ALU.add),
                 reads=[bb, b_bias], writes=[b_res])
        k.dma("sp", out_d[li], res[:], reads=[b_res], writes=[b_outd])


def emit_proj_bg(k, cx, cfg, io):
    S = cfg["nlat"]
    mods_ap, b_mods, li = cfg["mods_src"]
    hf_d, b_hf = io["h_full"]
    hc_d, b_hc = io["h_ctx"]
    w_d, b_wd = io["w_bg"]
    q2 = io["qT_s"][0].rearrange("h d n -> (h d) n")
    k2 = io["kT_s"][0].rearrange("h d n -> (h d) n")
    x2 = io["xbc_s"][0].rearrange("c p n -> (c p) n")
    v_d = io["v_s"][0]
    z_d = io["z_s"][0]
    dt_d = io["dtr_s"][0]
    cT = k.sb("cTp", [128, 4, KC])
    b_cT = k.buf("cTp")
    for m in range(2):
        for ci in range(2):
            k.dma("sp", cT[:, 2 * m + ci, :], mods_ap[li, m, ci * D:(ci + 1) * D].rearrange("(c p) -> p c", p=128),
                  reads=[b_mods], writes=[b_cT], allow_slow_non_contiguous=True)
    for m in range(2):
        k.op("dve", lambda e, m=m: e.tensor_scalar_add(cT[:, 2 * m + 1, :], cT[:, 2 * m + 1, :], 1.0), reads=[b_cT], writes=[b_cT])
    zt = k.sb("zpad", [128, 4, 4])
    b_zt = k.buf("zpad")
    k.op("pool", lambda e: e.memset(zt[:], 0.0), writes=[b_zt])
    xs4 = io["xbc_s"][0]
    for c0, n in ((0, 2), (258, 4), (262 + S, 2)):
        k.dma("sp", xs4[:, :, c0:c0 + n].rearrange("c p n -> p c n"), zt[:, :, 0:n], reads=[b_zt], writes=[k.buf()],
              allow_slow_non_contiguous=True)
    hb = k.sb("hb", [128, 4, D])
    b_hb = k.bufs(4, "hb")
    uT = k.sb("uT", [128, KC, 512], BF16)
    b_uT = k.buf("uT")
    fmb = k.sb("fmb", [128, 4, 512])
    b_fmb = k.bufs(4, "fmb")
    tmb = k.sb("tmb", [128, 2, 512])
    b_tmb = k.bufs(2, "tmb")
    dtb = k.sb("dtb", [128, 2, 8])
    b_dtb = k.bufs(2, "dtb")
    for gi in range(S // 512 + 1):
        if gi < S // 512:
            tok0, nt, ms = gi * 512, 4, 0
            src_ap, bsrc = hf_d[tok0:tok0 + 512, :], b_hf
            na_col, ssd_col, ssd_row = tok0, 262 + tok0, 256 + tok0
        else:
            nt, ms = 2, 1
            src_ap, bsrc = hc_d, b_hc
            na_col, ssd_col, ssd_row = S, 2, 0
        N = nt * 128

        def ld(w, bw, src_ap=src_ap, bsrc=bsrc, nt=nt, ms=ms):
            k.dma("sp", hb[:, 0:nt, :], src_ap.rearrange("(t p) d -> p t d", p=128), reads=[bsrc], writes=b_hb[0:nt])
            for j in range(nt):
                emit_modT(cx, hb[:, j, :], b_hb[j], 128, uT, b_uT, j * 128, cT[:, 2 * ms + 1, :], cT[:, 2 * ms, :], b_cT)
        cx.step(None, ld)
        for p in range(2):
            def fn(w, bw, p=p, N=N, na_col=na_col, ssd_col=ssd_col):
                for cc in range(4):
                    ch = p * 4 + cc
                    bank, bb = cx.bank[2 + cc], cx.b_bank[2 + cc]
                    for kc in range(KC):
                        k.op("pe", lambda e, kc=kc, cc=cc, bank=bank: e.matmul(
                            bank[:, 0:N], lhsT=w[:, kc, cc * 128:(cc + 1) * 128], rhs=uT[:, kc, 0:N],
                            start=(kc == 0), stop=(kc == KC - 1)), reads=[bw, b_uT], writes=[bb])
                    o = fmb[:, cc, 0:N]
                    if cc % 2 == 0:
                        k.op("act", lambda e, o=o, bank=bank: e.activation(out=o, in_=bank[:, 0:N], func=AF.Copy),
                             reads=[bb], writes=[b_fmb[cc]])
                    else:
                        k.op("dve", lambda e, o=o, bank=bank: e.tensor_copy(o, bank[:, 0:N]), reads=[bb], writes=[b_fmb[cc]])
                    if ch < 2:
                        dst = q2[ch * 128:(ch + 1) * 128, na_col:na_col + N]
                    elif ch < 4:
                        dst = k2[(ch - 2) * 128:(ch - 1) * 128, na_col:na_col + N]
                    else:
                        dst = x2[(ch - 4) * 128:(ch - 3) * 128, ssd_col:ssd_col + N]
                    k.dma("sp", dst, o, reads=[b_fmb[cc]], writes=[k.buf()])
            cx.step((w_d[:, p * 512:(p + 1) * 512], b_wd, KC), fn)

        def fn_tm(w, bw, nt=nt, na_col=na_col, ssd_row=ssd_row):
            for j in range(nt):
                bank, bb = cx.bank[6 + j % 2], cx.b_bank[6 + j % 2]
                for kc in range(KC):
                    k.op("pe", lambda e, kc=kc, j=j, bank=bank: e.matmul(
                        bank[:, :], lhsT=uT[:, kc, j * 128:(j + 1) * 128], rhs=w[:, kc, :],
                        start=(kc == 0), stop=(kc == KC - 1)), reads=[bw, b_uT], writes=[bb])
                o = tmb[:, j % 2, :]
                k.op("act", lambda e, o=o, bank=bank: e.activation(out=o, in_=bank[:, :], func=AF.Copy), reads=[bb],
                     writes=[b_tmb[j % 2]])
                k.dma("sp", v_d[na_col + j * 128:na_col + (j + 1) * 128, :], tmb[:, j % 2, 0:256], reads=[b_tmb[j % 2]],
                      writes=[k.buf()])
                k.dma("sp", z_d[ssd_row + j * 128:ssd_row + (j + 1) * 128, :], tmb[:, j % 2, 256:512], reads=[b_tmb[j % 2]],
                      writes=[k.buf()])
        cx.step((w_d[:, 1024:1536], b_wd, KC), fn_tm)

        def fn_dt(w, bw, nt=nt, ssd_row=ssd_row):
            for j in range(nt):
                bank, bb = cx.bank[6 + j % 2], cx.b_bank[6 + j % 2]
                for kc in range(KC):
                    k.op("pe", lambda e, kc=kc, j=j, bank=bank: e.matmul(
                        bank[:, 0:8], lhsT=uT[:, kc, j * 128:(j + 1) * 128], rhs=w[:, kc, 0:8],
                        start=(kc == 0), stop=(kc == KC - 1)), reads=[bw, b_uT], writes=[bb])
                k.op("dve", lambda e, j=j, bank=bank: e.tensor_copy(dtb[:, j % 2, :], bank[:, 0:8]), reads=[bb],
                     writes=[b_dtb[j % 2]])
                k.dma("sp", dt_d[ssd_row + j * 128:ssd_row + (j + 1) * 128, :], dtb[:, j % 2, :], reads=[b_dtb[j % 2]],
                      writes=[k.buf()])
        cx.step((w_d[:, 1536:1544], b_wd, KC), fn_dt)
    cx.flush()


def emit_outproj_bg(k, cx, cfg, io):
    S = cfg["nlat"]
    live = cfg["live"]
    attn_d, b_attn = io["attn_s"]
    ssm_d, b_ssm = io["ssm_s"]
    wo_d, b_wod = io["wo_bg"]
    yp_d = io["y_part"][0]
    ypc_d = io["y_part_ctx"][0]
    wo = k.sb("wo", [128, 4, D], BF16)
    b_wo = k.buf("wo")
    k.dma("pool", wo[:], wo_d.rearrange("(c p) n -> p c n", p=128), reads=[b_wod], writes=[b_wo])
    mixb = [k.sb(f"mixb{i}", [128, 512]) for i in range(2)]
    b_mixb = k.bufs(2, "mixb")
    mT4 = k.sb("mT4", [128, 4, 128], BF16)
    b_mT4 = k.buf("mT4")
    yb = [k.sb(f"yb{i}", [128, D]) for i in range(2)]
    b_yb = k.bufs(2, "yb")
    ntile = S // 128 + (2 if live else 0)
    for t in range(ntile):
        i = t % 2
        if t < S // 128:
            arow, srow, dst = t * 128, 256 + t * 128, yp_d[t * 128:(t + 1) * 128, :]
        else:
            c = t - S // 128
            arow, srow, dst = S + c * 128, c * 128, ypc_d[c * 128:(c + 1) * 128, :]
        k.dma("sp", mixb[i][:, 0:256], attn_d[arow:arow + 128, :], reads=[b_attn], writes=[b_mixb[i]])
        k.dma("sp", mixb[i][:, 256:512], ssm_d[srow:srow + 128, :], reads=[b_ssm], writes=[b_mixb[i]])
        emit_copyT(cx, mixb[i], b_mixb[i], 128, 4, mT4, b_mT4, 0)
        for hf in range(2):
            bk = 2 + (t * 2 + hf) % 4
            bank, bb = cx.bank[bk], cx.b_bank[bk]
            for kc in range(4):
                k.op("pe", lambda e, kc=kc, hf=hf, bank=bank: e.matmul(bank[:, :], lhsT=mT4[:, kc, :],
                                                                      rhs=wo[:, kc, hf * 512:(hf + 1) * 512],
                                                                      start=(kc == 0), stop=(kc == 3)),
                     reads=[b_mT4, b_wo], writes=[bb])
            o = yb[i][:, hf * 512:(hf + 1) * 512]
            if hf == 0:
                k.op("act", lambda e, o=o, bank=bank: e.activation(out=o, in_=bank[:, :], func=AF.Copy), reads=[bb],
                     writes=[b_yb[i]])
            else:
                k.op("dve", lambda e, o=o, bank=bank: e.tensor_copy(o, bank[:, :]), reads=[bb], writes=[b_yb[i]])
        k.dma("sp", dst, yb[i][:], reads=[b_yb[i]], writes=[k.buf()])


RG4 = [[0, 1, 2, 3], [4, 5, 6, 7]]


def build_fused(S=16384):
    k = KB()
    cx = Ctx(k)
    SH = S // 4
    NCT = 256
    NTOT = S + NCT
    LP = 260 + S + 4
    rows = S // GRID_W
    NV = SH + NCT + 6
    F_E, F_O = 2816, 3584

    def ext(name, shape, dt=F32):
        return k.dram(name, shape, dt, "ExternalInput")

    def scr(name, shape, dt=F32):
        return k.dram(name, shape, dt, "Internal")
    x_in = ext("x_in", [SH + NCT, D])
    x_full = ext("x_full", [S, D])
    cT2 = ext("cT2", [128, KC, 2])
    ada_w = ext("ada_w", [4, D, 6 * D])
    ada_b2 = ext("ada_b2", [4, 2, 6 * D])
    cbc_ln = ext("cbc_ln", [4, 128, 4, D])
    w_bg = ext("w_bg", [2, D, 1544])
    wo_bg = ext("wo_bg", [2, 512, D])
    mb = ext("mb", [2, 25, 128, 512])
    cwT = ext("cwT", [2, 128, 4, 5])
    cb = ext("cb", [2, 128, 4])
    small = ext("small", [2, 128, 16])
    dn = ext("dn", [2, 128, 2, 256])
    f_w1 = ext("ffn_w1", [2, 1, D, F_E])
    f_w3 = ext("ffn_w3", [2, 1, D, F_E])
    f_w2 = ext("ffn_w2", [2, 1, F_E, D])
    sc_win = ext("sc_w_in", [2, D, 3 * D])
    sc_wout = ext("sc_w_out", [2, D, D])
    sc_cw = ext("sc_cw", [2, 128, KC, 3])
    w_router = ext("w_router", [2, D, 8])
    m_w1 = ext("moe_w1", [2, 8, D, F_O])
    m_w3 = ext("moe_w3", [2, 8, D, F_O])
    m_w2 = ext("moe_w2", [2, 8, F_O, D])
    halsel = ext("halsel", [128, 2, KC, 8])
    out = k.dram("out", [SH, D], F32, "ExternalOutput")

    mods_s = scr("mods_s", [4, 2, 6 * D])
    hA = scr("hA", [SH + NCT, D])
    hB = scr("hB", [SH + NCT, D])
    ag_in = scr("ag_in", [SH, D])
    h_full_s = scr("h_full_s", [S, D])
    qT_s = scr("qT_s", [4, 64, NTOT])
    kT_s = scr("kT_s", [4, 64, NTOT])
    xbc_s = scr("xbc_s", [4, 128, LP])
    v_s = scr("v_s", [NTOT, 256])
    z_s = scr("z_s", [NTOT, 256])
    dtr_s = scr("dtr_s", [NTOT, 8])
    attn_s = scr("attn_s", [NTOT, 256])
    ssm_s = scr("ssm_s", [NTOT, 256])
    yf_scr = scr("yf_scr", [NTOT, 256])
    y_part = scr("y_part", [S, D])
    y_part_ctx = scr("y_part_ctx", [NCT, D])
    y_rs = scr("y_rs", [SH, D])
    y_ctx = scr("y_ctx", [NCT, D])
    bnd = scr("bnd", [2, D])
    bnd_all = scr("bnd_all", [8, D])
    vT_scr = scr("vT_scr", [D, NV])
    gbT_scr = scr("gbT_scr", [D, NV])

    def sl(t, ap):
        return (ap, t[1])

    k.phase_begin()
    emit_mods_all(k, cx, {"cT2": cT2, "ada_w": ada_w, "ada_b2": ada_b2, "mods_s": mods_s})
    k.phase_end()

    h_cur = x_in
    h_nxt = [hA, hB]
    for i in range(4):
        j = i // 2
        live = i < 2
        nms = 2 if live else 1
        ntail = SH + (NCT if live else 0)
        tgroups = [(g * 512, 4, 0, 1 + g * 512) for g in range(SH // 512)]
        if live:
            tgroups.append((SH, 2, 1, SH + 4))
        h_out = (out if i == 3 else h_nxt[i % 2])
        msrc = (mods_s[0], mods_s[1], i)
        if i % 2 == 0:
            hf = x_full if i == 0 else h_full_s
            hctx = sl(h_cur, h_cur[0][SH:SH + NCT, :])
            k.phase_begin()
            emit_proj_bg(k, cx, dict(nlat=S, mods_src=msrc),
                         {"h_full": hf, "h_ctx": hctx, "w_bg": sl(w_bg, w_bg[0][j]), "qT_s": qT_s, "kT_s": kT_s, "xbc_s": xbc_s,
                          "v_s": v_s, "z_s": z_s, "dtr_s": dtr_s})
            k.phase_end()
            k.phase_begin()
            NQ = S + (NCT if live else 0)
            build_na(dict(rows=rows, with_ctx=live), k, cx,
                     {"qT": sl(qT_s, qT_s[0][:, :, 0:NQ]), "kT": sl(kT_s, kT_s[0][:, :, 0:S]), "kcT": sl(kT_s, kT_s[0][:, :, S:NTOT]),
                      "v": sl(v_s, v_s[0][0:S, :]), "vc": sl(v_s, v_s[0][S:NTOT, :]), "mb": sl(mb, mb[0][j]),
                      "attn": sl(attn_s, attn_s[0][0:NQ, :])})
            k.phase_end()
            k.phase_begin()
            build_ssd(dict(nlat=S), k, cx,
                      {"xbcT": xbc_s, "cwT": sl(cwT, cwT[0][j]), "cb": sl(cb, cb[0][j]), "dtr": dtr_s, "small": sl(small, small[0][j]),
                       "dn": sl(dn, dn[0][j]), "z": z_s, "ssm": ssm_s, "yf_scr": yf_scr})
            k.phase_end()
            k.phase_begin()
            emit_outproj_bg(k, cx, dict(nlat=S, live=live),
                            {"attn_s": attn_s, "ssm_s": ssm_s, "wo_bg": sl(wo_bg, wo_bg[0][j]), "y_part": y_part,
                             "y_part_ctx": y_part_ctx})
            k.phase_end()
            k.cc("ReduceScatter", ALU.add, RG4, y_part[0], y_rs[0], [y_part[1]], [y_rs[1]])
            if live:
                k.cc("AllReduce", ALU.add, RG4, y_part_ctx[0], y_ctx[0], [y_part_ctx[1]], [y_ctx[1]])
            k.phase_begin()
            build_tail(dict(mixer="pre", ne=1, f=F_E, groups=tgroups, ntok=ntail, nmod=nms, mods_src=msrc), k, cx,
                       {"h_in": sl(h_cur, h_cur[0][0:ntail, :]), "h_out": sl(h_out, h_out[0][0:ntail, :]) if i != 3 else h_out,
                        "cbc": sl(cbc_ln, cbc_ln[0][i]), "y_lat": y_rs, "y_ctx": y_ctx,
                        "w1": sl(f_w1, f_w1[0][j]), "w3": sl(f_w3, f_w3[0][j]), "w2": sl(f_w2, f_w2[0][j])})
            k.phase_end()
        else:
            k.phase_begin()
            bt = k.sb("bt", [2, D])
            b_bt = k.buf("bt")
            k.dma("sp", bt[0:1, :], h_cur[0][0:1, :], reads=[h_cur[1]], writes=[b_bt])
            k.dma("sp", bt[1:2, :], h_cur[0][SH - 1:SH, :], reads=[h_cur[1]], writes=[b_bt])
            k.dma("sp", bnd[0], bt[:], reads=[b_bt], writes=[bnd[1]])
            k.phase_end()
            k.cc("AllGather", ALU.bypass, RG4, bnd[0], bnd_all[0], [bnd[1]], [bnd_all[1]])
            zc = [SH + 3, SH + 4 + NCT] if live else []
            k.phase_begin()
            build_tail(dict(mixer="sc", ne=8, f=F_O, groups=tgroups, ntok=ntail, nmod=nms, nv=NV, zero_cols=zc,
                            halo_cols=(0, SH + 1), nhalo=8, mods_src=msrc), k, cx,
                       {"h_in": sl(h_cur, h_cur[0][0:ntail, :]), "h_out": sl(h_out, h_out[0][0:ntail, :]) if i != 3 else h_out,
                        "cbc": sl(cbc_ln, cbc_ln[0][i]), "hal": bnd_all, "halsel": halsel,
                        "sc_w_in": sl(sc_win, sc_win[0][j]), "sc_w_out": sl(sc_wout, sc_wout[0][j]), "sc_cw": sl(sc_cw, sc_cw[0][j]),
                        "vT_scr": vT_scr, "gbT_scr": gbT_scr, "w_router": sl(w_router, w_router[0][j]),
                        "w1": sl(m_w1, m_w1[0][j]), "w3": sl(m_w3, m_w3[0][j]), "w2": sl(m_w2, m_w2[0][j])})
            k.phase_end()
            if i == 1:
                k.phase_begin()
                cpb = k.sb("cpb", [128, 4, D])
                b_cpb = k.buf("cpb")
                for g in range(SH // 512):
                    k.dma("sp", cpb[:], h_out[0][g * 512:(g + 1) * 512, :].rearrange("(t p) d -> p t d", p=128),
                          reads=[h_out[1]], writes=[b_cpb])
                    k.dma("sp", ag_in[0][g * 512:(g + 1) * 512, :].rearrange("(t p) d -> p t d", p=128), cpb[:],
                          reads=[b_cpb], writes=[ag_in[1]])
                k.phase_end()
                k.cc("AllGather", ALU.bypass, RG4, ag_in[0], h_full_s[0], [ag_in[1]], [h_full_s[1]])
        h_cur = h_out
    k.barrier()
    return k.finish()


_PROGS = {}


def _prog(key, fn):
    if key not in _PROGS:
        _PROGS[key] = fn()
    return _PROGS[key]


def _colT(v):
    return np.ascontiguousarray(np.asarray(v, np.float32).reshape(KC, 128).T)


def _bc(v, n=128):
    return np.broadcast_to(np.asarray(v, np.float32)[None], (n,) + tuple(np.shape(v)))


def _run(nc, in_maps):
    in_maps = [{kk: np.ascontiguousarray(vv, dtype=np.float32) for kk, vv in m.items()} for m in in_maps]
    res = run_bass_kernel_spmd(nc, in_maps, core_ids=list(range(8)))
    return res.results


def kernel(x, c, ctx, c_ctx, ada_w, ada_b, ln_g, ln_b, even_w_in, na_rpb, ssm_conv_w, ssm_conv_b, ssm_dt_bias,
           ssm_a_log, ssm_d, ssm_norm_w, even_w_out, ffn_w1, ffn_w3, ffn_w2, sc_w_in, sc_conv_w, sc_w_out,
           moe_router, moe_w1, moe_w3, moe_w2):
    f32 = np.float32
    x = np.asarray(x, f32)
    B, S, _ = x.shape
    SH = S // 4
    NCT = 256
    rows = S // GRID_W
    cT3 = np.stack([_colT(c[0]), _colT(c[1]), _colT(c_ctx)], axis=-1)
    res = _run(_prog("mods", build_mods), [
        {"cT3": cT3, "ada_w": ada_w[r % 4][:, (r // 4) * 3072:(r // 4 + 1) * 3072],
         "ada_b3": _bc(ada_b[r % 4][(r // 4) * 3072:(r // 4 + 1) * 3072], 3)} for r in range(8)])
    mods = [np.concatenate([res[i]["mods"], res[i + 4]["mods"]], axis=1).reshape(3, 6, D) for i in range(4)]

    h_lat = x.copy()
    h_ctx = np.asarray(ctx, f32).copy()

    def shard(lat, cx_, with_ctx):
        out = []
        for r in range(8):
            b, q = r // 4, r % 4
            parts = [lat[b, q * SH:(q + 1) * SH]]
            if with_ctx:
                parts.append(cx_[b])
            out.append(np.concatenate(parts, 0) if with_ctx else parts[0])
        return out

    def consts(i, b, nms):
        cbc = np.zeros((128, 4 + 2 * nms, D), f32)
        cbc[:, 0], cbc[:, 1], cbc[:, 2], cbc[:, 3] = ln_g[i, 0], ln_b[i, 0], ln_g[i, 1], ln_b[i, 1]
        cT = np.zeros((128, 4 * nms, KC), f32)
        for m in range(nms):
            mv = mods[i][b if m == 0 else 2]
            cbc[:, 4 + 2 * m], cbc[:, 5 + 2 * m] = mv[2], mv[5]
            cT[:, 4 * m + 0], cT[:, 4 * m + 1] = _colT(mv[0]), _colT(mv[1])
            cT[:, 4 * m + 2], cT[:, 4 * m + 3] = _colT(mv[3]), _colT(mv[4])
        return cbc, cT

    for i in range(4):
        j = i // 2
        live = i < 2
        nms = 2 if live else 1
        ntail = SH + (NCT if live else 0)
        tgroups = [(g * 512, 4, 0, 1 + g * 512) for g in range(SH // 512)]
        if live:
            tgroups.append((SH, 2, 1, SH + 4))
        if i % 2 == 0:
            pg = [(g * 512, 4, 0) for g in range(SH // 512)] + [(SH, 2, 1)]
            nc = _prog("proj", lambda: build_proj(dict(groups=pg, ntok=SH + NCT, nmod=2, ncol=6176)))
            hs = shard(h_lat, h_ctx, True)
            ims = []
            for r in range(8):
                b = r // 4
                cT = np.stack([_colT(mods[i][b][0]), _colT(mods[i][b][1]), _colT(mods[i][2][0]), _colT(mods[i][2][1])], 1)
                ims.append({"h_in": hs[r], "w": even_w_in[j], "cT": cT})
            res = _run(nc, ims)
            P_lat = np.stack([np.concatenate([res[b * 4 + q]["proj"][:SH] for q in range(4)], 0) for b in range(B)])
            P_ctx = np.stack([res[b * 4]["proj"][SH:] for b in range(B)])
            del res
            nc = _prog(("na", live), lambda: build_na(dict(rows=rows, with_ctx=live)))
            ims = []
            for r in range(8):
                b, g = r // 4, r % 4
                sl = slice(g * 256, (g + 1) * 256)
                fm = lambda a: a.reshape(-1, 4, 64).transpose(1, 2, 0)
                ql, kl, vl = P_lat[b][:, 0:1024][:, sl], P_lat[b][:, 1024:2048][:, sl], P_lat[b][:, 2048:3072][:, sl]
                qc, kc_, vc_ = P_ctx[b][:, 0:1024][:, sl], P_ctx[b][:, 1024:2048][:, sl], P_ctx[b][:, 2048:3072][:, sl]
                qq = np.concatenate([ql, qc], 0) if live else ql
                ims.append({"qT": fm(qq), "kT": fm(kl), "kcT": fm(kc_), "v": vl, "vc": vc_,
                            "mb": na_mask_bias(np.asarray(na_rpb[j][4 * g:4 * g + 4], f32), rows).reshape(25, 128, 512)})
            res = _run(nc, ims)
            mix_lat = np.zeros((B, S, 2 * D), f32)
            mix_ctx = np.zeros((B, NCT, 2 * D), f32)
            for r in range(8):
                b, g = r // 4, r % 4
                mix_lat[b, :, g * 256:(g + 1) * 256] = res[r]["attn"][:S]
                if live:
                    mix_ctx[b, :, g * 256:(g + 1) * 256] = res[r]["attn"][S:]
            del res
            nc = _prog("ssd", lambda: build_ssd(dict(nlat=S)))
            LP = 260 + S + 4
            ims = []
            for r in range(8):
                b, g = r // 4, r % 4
                ch = np.concatenate([np.arange(g * 256, (g + 1) * 256), 1024 + np.arange(g * 128, (g + 1) * 128),
                                     1536 + np.arange(g * 128, (g + 1) * 128)])
                xbcT = np.zeros((512, LP), f32)
                xbcT[:, 2:258] = P_ctx[b][:, 4096 + ch].T
                xbcT[:, 262:262 + S] = P_lat[b][:, 4096 + ch].T
                hd = np.arange(4 * g, 4 * g + 4)
                dcols = np.concatenate([6144 + hd, 6144 + 16 + hd])
                small = np.concatenate([ssm_dt_bias[j][0, hd], ssm_dt_bias[j][1, hd], ssm_a_log[j][0, hd], ssm_a_log[j][1, hd]])
                dn = np.stack([np.repeat(np.asarray(ssm_d[j], f32)[hd], 64), np.asarray(ssm_norm_w[j], f32)[g * 256:(g + 1) * 256]])
                ims.append({"xbcT": xbcT.reshape(4, 128, LP),
                            "cwT": np.asarray(ssm_conv_w[j], f32)[:, ch].T.reshape(4, 128, 5).transpose(1, 0, 2),
                            "cb": np.asarray(ssm_conv_b[j], f32)[ch].reshape(4, 128).T,
                            "dtr": np.concatenate([P_ctx[b][:, dcols], P_lat[b][:, dcols]], 0),
                            "small": _bc(small), "dn": _bc(dn),
                            "z": np.concatenate([P_ctx[b][:, 3072 + g * 256:3072 + (g + 1) * 256],
                                                 P_lat[b][:, 3072 + g * 256:3072 + (g + 1) * 256]], 0)})
            res = _run(nc, ims)
            for r in range(8):
                b, g = r // 4, r % 4
                mix_lat[b, :, D + g * 256:D + (g + 1) * 256] = res[r]["ssm"][NCT:]
                mix_ctx[b, :, D + g * 256:D + (g + 1) * 256] = res[r]["ssm"][:NCT]
            del res, P_lat, P_ctx
            nc = _prog(("tail_e", live), lambda: build_tail(dict(mixer="even", ne=1, f=2816, groups=tgroups, ntok=ntail, nmod=nms)))
            hs = shard(h_lat, h_ctx, live)
            ms = shard(mix_lat, mix_ctx, live)
            ims = []
            for r in range(8):
                cbc, cT = consts(i, r // 4, nms)
                ims.append({"h_in": hs[r], "mix": ms[r], "cbc": cbc, "cT": cT, "w_out": even_w_out[j],
                            "w1": ffn_w1[j][None], "w3": ffn_w3[j][None], "w2": ffn_w2[j][None]})
            res = _run(nc, ims)
        else:
            zc = [SH + 3, SH + 4 + NCT] if live else []
            nc = _prog(("tail_o", live), lambda: build_tail(dict(mixer="sc", ne=8, f=3584, groups=tgroups, ntok=ntail, nmod=nms,
                                                                 nv=SH + NCT + 6, zero_cols=zc, halo_cols=(0, SH + 1))))
            hs = shard(h_lat, h_ctx, live)
            ims = []
            for r in range(8):
                b, q = r // 4, r % 4
                cbc, cT = consts(i, b, nms)
                hal = np.zeros((2, D), f32)
                if q > 0:
                    hal[0] = h_lat[b, q * SH - 1]
                if q < 3:
                    hal[1] = h_lat[b, (q + 1) * SH]
                halv = _bc(np.array([1.0 if q > 0 else 0.0, 1.0 if q < 3 else 0.0], f32))
                ims.append({"h_in": hs[r], "cbc": cbc, "cT": cT, "hal": hal, "halv": halv, "sc_w_in": sc_w_in[j],
                            "sc_w_out": sc_w_out[j],
                            "sc_cw": np.asarray(sc_conv_w[j], f32).reshape(3, KC, 128).transpose(2, 1, 0),
                            "w_router": moe_router[j], "w1": moe_w1[j], "w3": moe_w3[j], "w2": moe_w2[j]})
            res = _run(nc, ims)
        new_lat = np.empty_like(h_lat)
        for r in range(8):
            b, q = r // 4, r % 4
            new_lat[b, q * SH:(q + 1) * SH] = res[r]["h_out"][:SH]
            if live and q == 0:
                h_ctx[b] = res[r]["h_out"][SH:]
        h_lat = new_lat
        del res
    return h_lat


def kernel_fused_experimental(x, c, ctx, c_ctx, ada_w, ada_b, ln_g, ln_b, even_w_in, na_rpb, ssm_conv_w, ssm_conv_b,
                              ssm_dt_bias, ssm_a_log, ssm_d, ssm_norm_w, even_w_out, ffn_w1, ffn_w3, ffn_w2, sc_w_in,
                              sc_conv_w, sc_w_out, moe_router, moe_w1, moe_w3, moe_w2):
    f32 = np.float32
    A = lambda a: np.asarray(a, f32)
    x = A(x)
    B, S, _ = x.shape
    SH = S // 4
    rows = S // GRID_W
    nc = _prog("fused", lambda: build_fused(S))
    cbc_ln = np.stack([np.stack([_bc(A(ln_g)[i, 0]), _bc(A(ln_b)[i, 0]), _bc(A(ln_g)[i, 1]), _bc(A(ln_b)[i, 1])], 1)
                       for i in range(4)])
    ada_b2 = np.repeat(A(ada_b)[:, None, :], 2, axis=1)
    shared = {"ada_w": A(ada_w), "ada_b2": ada_b2, "cbc_ln": cbc_ln,
              "ffn_w1": A(ffn_w1)[:, None], "ffn_w3": A(ffn_w3)[:, None], "ffn_w2": A(ffn_w2)[:, None],
              "sc_w_in": A(sc_w_in), "sc_w_out": A(sc_w_out),
              "sc_cw": A(sc_conv_w).reshape(2, 3, KC, 128).transpose(0, 3, 2, 1),
              "w_router": A(moe_router), "moe_w1": A(moe_w1), "moe_w3": A(moe_w3), "moe_w2": A(moe_w2)}
    shared = {kk: np.ascontiguousarray(vv, dtype=f32) for kk, vv in shared.items()}
    ims = []
    for r in range(8):
        b, g = r // 4, r % 4
        q = g
        hd = np.arange(4 * g, 4 * g + 4)
        cols = np.concatenate([np.arange(g * 256, (g + 1) * 256), 1024 + np.arange(g * 256, (g + 1) * 256),
                               4096 + np.arange(g * 256, (g + 1) * 256), 4096 + 1024 + np.arange(g * 128, (g + 1) * 128),
                               4096 + 1536 + np.arange(g * 128, (g + 1) * 128), 2048 + np.arange(g * 256, (g + 1) * 256),
                               3072 + np.arange(g * 256, (g + 1) * 256), 6144 + hd, 6144 + 16 + hd])
        ch = np.concatenate([np.arange(g * 256, (g + 1) * 256), 1024 + np.arange(g * 128, (g + 1) * 128),
                             1536 + np.arange(g * 128, (g + 1) * 128)])
        orow = np.concatenate([np.arange(g * 256, (g + 1) * 256), 1024 + np.arange(g * 256, (g + 1) * 256)])
        hs = np.zeros((2, 8), f32)
        if q > 0:
            hs[0, 2 * (q - 1) + 1] = 1.0
        if q < 3:
            hs[1, 2 * (q + 1)] = 1.0
        m = {"x_in": np.concatenate([x[b, q * SH:(q + 1) * SH], A(ctx)[b]], 0), "x_full": x[b],
             "cT2": np.stack([_colT(A(c)[b]), _colT(A(c_ctx))], -1),
             "w_bg": A(even_w_in)[:, :, cols], "wo_bg": A(even_w_out)[:, orow, :],
             "mb": np.stack([na_mask_bias(A(na_rpb)[j][hd], rows).reshape(25, 128, 512) for j in range(2)]),
             "cwT": np.stack([A(ssm_conv_w)[j][:, ch].T.reshape(4, 128, 5).transpose(1, 0, 2) for j in range(2)]),
             "cb": np.stack([A(ssm_conv_b)[j][ch].reshape(4, 128).T for j in range(2)]),
             "small": np.stack([_bc(np.concatenate([A(ssm_dt_bias)[j][0, hd], A(ssm_dt_bias)[j][1, hd],
                                                    A(ssm_a_log)[j][0, hd], A(ssm_a_log)[j][1, hd]])) for j in range(2)]),
             "dn": np.stack([_bc(np.stack([np.repeat(A(ssm_d)[j][hd], 64), A(ssm_norm_w)[j][g * 256:(g + 1) * 256]]))
                             for j in range(2)]),
             "halsel": np.broadcast_to(hs[None, :, None, :], (128, 2, KC, 8))}
        m = {kk: np.ascontiguousarray(vv, dtype=f32) for kk, vv in m.items()}
        m.update(shared)
        ims.append(m)
    res = run_bass_kernel_spmd(nc, ims, core_ids=list(range(8)))
    outp = np.empty((B, S, D), f32)
    for r in range(8):
        outp[r // 4, (r % 4) * SH:(r % 4 + 1) * SH] = res.results[r]["out"]
    return outp
```

```python
import numpy as np
from contextlib import ExitStack
import concourse.bass as bass
import concourse.mybir as mybir
from concourse.bass_utils import run_bass_kernel_spmd

F32 = mybir.dt.float32
BF16 = mybir.dt.bfloat16
I32 = mybir.dt.int32
AF = mybir.ActivationFunctionType
ALU = mybir.AluOpType
AX = mybir.AxisListType

NDMA_SEMS = 12
import os as _os
SAME_ENG_DIST = int(_os.environ.get('SAME_ENG_DIST', '2'))


class Buf:
    __slots__ = ("name", "w", "r")

    def __init__(self, name):
        self.name = name
        self.w = None
        self.r = []


class Op:
    __slots__ = ("eng", "fn", "deps", "idx", "signal", "count", "dma", "slot", "pos", "waits", "cc")


class KB:
    ENGS = ("pe", "act", "dve", "pool", "sp")

    def __init__(self):
        self.nc = bass.Bass("TRN2", target_bir_lowering=False)
        self.ops = []
        self.es = ExitStack()
        self.nbuf = 0
        self.eng_ops = {e: [] for e in self.ENGS}
        self.ndma = {e: 0 for e in self.ENGS}
        self.es_phase = None
        self.nsb = 0
        self.ncc = 0
        self.open_dma = []
        self.bar_d = self.nc.dram_tensor("bar_scr", [1, 2], F32, kind="Internal").ap()
        self.b_bar = Buf("bar")
        self.junk = self.es.enter_context(self.nc.sbuf_tensor("junk", [128, 4], F32))
        self.junk_bf = self.es.enter_context(self.nc.sbuf_tensor("junk_bf", [128, 4], BF16))
        self.junk_ps = None
        self.b_junk = [Buf(f"junk{i}") for i in range(4)]

    def dram(self, name, shape, dtype, kind):
        t = self.nc.dram_tensor(name, list(shape), dtype, kind=kind)
        return t.ap(), Buf(name)

    def sb(self, name, shape, dtype=F32):
        es = self.es_phase if self.es_phase is not None else self.es
        self.nsb += 1
        t = es.enter_context(self.nc.sbuf_tensor(f"{name}_{self.nsb}", list(shape), dtype))
        return t

    def dr(self, io, name, shape, dtype, kind):
        if io is not None and name in io:
            return io[name]
        return self.dram(name, shape, dtype, kind)

    def phase_begin(self):
        self.es_phase = ExitStack()

    def phase_end(self):
        self.barrier()
        self.es_phase.close()
        self.es_phase = None

    def barrier(self):
        deps = set(self.open_dma)
        for e in self.ENGS:
            if self.eng_ops[e]:
                deps.add(self.eng_ops[e][-1].idx)
        bop = self._rec("sp", lambda e: e.dma_start(out=self.bar_d[0:1, 0:1], in_=self.bar_d[0:1, 1:2]), [], [self.b_bar], True,
                        extra=deps)
        self.open_dma = []
        jk = self.junk
        self.op("pool", lambda e: e.memset(jk[:, 0:1], 0.0), reads=[self.b_bar], writes=[self.b_junk[0]])
        self.op("dve", lambda e: e.memset(jk[:, 1:2], 0.0), reads=[self.b_bar], writes=[self.b_junk[1]])
        self.op("act", lambda e: e.mul(jk[:, 2:3], jk[:, 3:4], 1.0), reads=[self.b_bar], writes=[self.b_junk[2]])
        self.op("pe", lambda e: e.matmul(self.junk_ps[0:1, 0:1], lhsT=self.junk_bf[0:1, 0:1], rhs=self.junk_bf[0:1, 0:1],
                                         start=True, stop=True), reads=[self.b_bar], writes=[self.b_junk[3]])

    def cc(self, kind, alu, groups, in_ap, out_ap, reads, writes):
        if _os.environ.get("FUSED_NOCC"):
            return None
        op = self._rec("pool", lambda e: e.collective_compute(kind, alu, replica_groups=groups, ins=[in_ap.opt()],
                                                              outs=[out_ap.opt()]), reads, writes, True)
        op.cc = self.ncc
        self.ncc += 1
        self.ndma["pool"] -= 1
        op.slot = None
        return op

    def ps(self, name, shape, dtype=F32):
        t = self.es.enter_context(self.nc.psum_tensor(name, list(shape), dtype))
        return t

    def buf(self, name=None):
        self.nbuf += 1
        return Buf(name or f"b{self.nbuf}")

    def bufs(self, n, name="b"):
        return [self.buf(f"{name}{i}") for i in range(n)]

    def _rec(self, eng, fn, reads, writes, dma, extra=None):
        op = Op()
        op.cc = None
        op.eng = eng
        op.fn = fn
        op.idx = len(self.ops)
        op.signal = False
        op.count = 0
        op.dma = dma
        op.slot = None
        op.waits = None
        deps = set()
        for b in reads:
            if b.w is not None:
                deps.add(b.w)
        for b in writes:
            if b.w is not None:
                deps.add(b.w)
            for r in b.r:
                deps.add(r)
        if extra:
            deps |= set(extra)
        op.deps = deps
        if dma:
            self.open_dma.append(op.idx)
        for b in reads:
            if not dma:
                b.r = [r for r in b.r if self.ops[r].dma or self.ops[r].eng != eng]
            b.r.append(op.idx)
        for b in writes:
            b.w = op.idx
            b.r = []
        op.pos = len(self.eng_ops[eng])
        self.eng_ops[eng].append(op)
        if dma:
            op.slot = self.ndma[eng]
            self.ndma[eng] += 1
        self.ops.append(op)
        return op

    def op(self, eng, fn, reads=(), writes=()):
        return self._rec(eng, fn, reads, writes, False)

    def dma(self, eng, out, in_, reads=(), writes=(), **kw):
        return self._rec(eng, lambda e: e.dma_start(out=out, in_=in_, **kw), reads, writes, True)

    def finish(self):
        nc = self.nc
        ops = self.ops
        for op in ops:
            for d in op.deps:
                p = ops[d]
                if p.dma:
                    continue
                if p.eng == op.eng and not op.dma:
                    if p.eng == "pe":
                        continue
                    if op.pos - p.pos > SAME_ENG_DIST:
                        continue
                p.signal = True
        cnt = {e: 0 for e in self.ENGS}
        for op in ops:
            if op.dma:
                continue
            if op.signal:
                cnt[op.eng] += 1
                op.count = cnt[op.eng]
        self.sig_counts = dict(cnt)
        seen = {e: {} for e in self.ENGS}
        for op in ops:
            w = {}
            for d in op.deps:
                p = ops[d]
                if p.cc is not None:
                    key = ("cc", p.cc)
                    val = 1
                elif p.dma:
                    key = ("d", p.eng, p.slot % NDMA_SEMS)
                    val = 16 * (p.slot // NDMA_SEMS + 1)
                else:
                    if not p.signal:
                        continue
                    if p.eng == op.eng and not op.dma:
                        if p.eng == "pe" or op.pos - p.pos > SAME_ENG_DIST:
                            continue
                    key = ("c", p.eng)
                    val = p.count
                if val > w.get(key, 0):
                    w[key] = val
            if op.dma and op.cc is None and op.slot >= NDMA_SEMS:
                key = ("d", op.eng, op.slot % NDMA_SEMS)
                val = 16 * (op.slot // NDMA_SEMS)
                if val > w.get(key, 0):
                    w[key] = val
            sw = seen[op.eng]
            op.waits = []
            for key, val in w.items():
                if sw.get(key, 0) >= val:
                    continue
                sw[key] = val
                op.waits.append((key, val))
        sems = {}
        for e in self.ENGS:
            sems[("c", e)] = self.es.enter_context(nc.semaphore(f"c_{e}"))
            if self.ndma[e] > 0:
                for s in range(min(NDMA_SEMS, self.ndma[e])):
                    sems[("d", e, s)] = self.es.enter_context(nc.semaphore(f"d_{e}_{s}"))
        final_waits = []
        for ci in range(self.ncc):
            sems[("cc", ci)] = self.es.enter_context(nc.semaphore(f"cc_{ci}"))
            final_waits.append((("cc", ci), 1))
        for e in self.ENGS:
            n = self.ndma[e]
            for s in range(min(NDMA_SEMS, n)):
                k = (n - 1 - s) // NDMA_SEMS + 1
                final_waits.append((("d", e, s), 16 * k))

        def run(e, engobj):
            for op in self.eng_ops[e]:
                for key, val in op.waits:
                    engobj.wait_ge(sems[key], val)
                ins = op.fn(engobj)
                if op.cc is not None:
                    ins.then_inc(sems[("cc", op.cc)])
                elif op.dma:
                    ins.then_inc(sems[("d", e, op.slot % NDMA_SEMS)], 16)
                elif op.signal:
                    ins.then_inc(sems[("c", e)], 1)
            if e == "sp":
                for key, val in final_waits:
                    engobj.wait_ge(sems[key], val)

        with nc.Block() as block:
            @block.tensor
            def _(eng):
                run("pe", eng)

            @block.scalar
            def _(eng):
                run("act", eng)

            @block.vector
            def _(eng):
                run("dve", eng)

            @block.gpsimd
            def _(eng):
                run("pool", eng)

            @block.sync
            def _(eng):
                run("sp", eng)
        self.es.close()
        return nc


D = 1024
KC = 8
ALPHA = float((2 * 4) ** 0.25)
LN_EPS = 1e-5
NSLOT = 6
WLOOK = 4


def _bc(ap, shape):
    return ap.to_broadcast(list(shape))


class Ctx:
    def __init__(self, k):
        self.k = k
        nc = k.nc
        self.ident = k.sb("ident", [128, 128])
        self.b_ident = k.buf("ident")
        k.op("pool", lambda e: e.memset(self.ident[:], 0.0), writes=[self.b_ident])
        k.op("pool", lambda e: e.affine_select(out=self.ident[:], in_=self.ident[:], pattern=[[-1, 128]],
                                               compare_op=ALU.not_equal, fill=1.0, base=0, channel_multiplier=1),
             reads=[self.b_ident], writes=[self.b_ident])
        self.bank = [k.ps(f"bank{i}", [128, 512]) for i in range(8)]
        self.b_bank = [k.buf(f"bank{i}") for i in range(8)]
        k.junk_ps = self.bank[7]
        k.b_junk[3] = self.b_bank[7]
        k.op("pool", lambda e: e.memset(k.junk[:], 0.0), writes=[k.b_junk[0], k.b_junk[1], k.b_junk[2]])
        k.op("pool", lambda e: e.memset(k.junk_bf[:], 0.0), writes=[k.b_junk[0]])
        self.wslot = [k.sb(f"wslot{i}", [128, 8, 512], BF16) for i in range(NSLOT)]
        self.b_wslot = [k.buf(f"wslot{i}") for i in range(NSLOT)]
        self.steps = []

    def step(self, wspec, fn):
        self.steps.append((wspec, fn))

    def flush(self):
        k = self.k
        steps = self.steps
        widx = [i for i, s in enumerate(steps) if s[0] is not None]
        slot_of = {}
        for j, i in enumerate(widx):
            slot_of[i] = j % NSLOT
        loaded = 0

        def load(j):
            i = widx[j]
            ap, dbuf, n = steps[i][0]
            ncols = ap.shape[1]
            s = slot_of[i]
            k.dma("pool", self.wslot[s][:, 0:n, 0:ncols], ap.rearrange("(c p) n -> p c n", p=128),
                  reads=[dbuf], writes=[self.b_wslot[s]])

        nw_done = 0
        for i, (wspec, fn) in enumerate(steps):
            if wspec is not None:
                while loaded < len(widx) and loaded <= nw_done + WLOOK:
                    load(loaded)
                    loaded += 1
                s = slot_of[i]
                fn(self.wslot[s], self.b_wslot[s])
                nw_done += 1
            else:
                fn(None, None)
        self.steps = []


def emit_transposes(cx, src, b_src, ntok, nchunk, evac):
    k = cx.k
    for g in range(0, nchunk, 4):
        n = min(4, nchunk - g)
        bi = (g // 4) % 2
        bank, bb = cx.bank[bi], cx.b_bank[bi]
        for c in range(n):
            k.op("pe", lambda e, c=c, g=g, bank=bank: e.transpose(
                bank[:, c * 128:c * 128 + ntok], src[0:ntok, (g + c) * 128:(g + c + 1) * 128],
                cx.ident[0:ntok, 0:ntok]), reads=[b_src, cx.b_ident], writes=[bb])
        evac(bank, bb, g, n)


def emit_modT(cx, src, b_src, ntok, dst, b_dst, col0, scale1p, shift, b_mod, dst32=None):
    k = cx.k

    def evac(bank, bb, g, n):
        for c in range(n):
            cc = g + c
            if dst32 is None:
                k.op("act", lambda e, c=c, cc=cc, bank=bank: e.activation(
                    out=dst[:, cc, col0:col0 + ntok], in_=bank[:, c * 128:c * 128 + ntok], func=AF.Identity,
                    scale=scale1p[:, cc:cc + 1], bias=shift[:, cc:cc + 1]),
                    reads=[bb, b_mod], writes=[b_dst])
            else:
                d32, b_d32 = dst32
                k.op("act", lambda e, c=c, cc=cc, bank=bank: e.activation(
                    out=d32[:, cc, col0:col0 + ntok], in_=bank[:, c * 128:c * 128 + ntok], func=AF.Identity,
                    scale=scale1p[:, cc:cc + 1], bias=shift[:, cc:cc + 1]),
                    reads=[bb, b_mod], writes=[b_d32])
        if dst32 is not None:
            d32, b_d32 = dst32
            k.op("pool", lambda e, g=g, n=n: e.tensor_copy(dst[:, g:g + n, col0:col0 + ntok],
                                                           d32[:, g:g + n, col0:col0 + ntok]),
                 reads=[b_d32], writes=[b_dst])

    emit_transposes(cx, src, b_src, ntok, KC, evac)


def emit_copyT(cx, src, b_src, ntok, nchunk, dst, b_dst, col0):
    k = cx.k

    def evac(bank, bb, g, n):
        k.op("act", lambda e, bank=bank, g=g, n=n: e.activation(
            out=dst[:, g:g + n, col0:col0 + ntok],
            in_=bank[:, 0:n * 128].rearrange("p (a b) -> p a b", a=n)[:, :, 0:ntok], func=AF.Copy),
            reads=[bb], writes=[b_dst])

    emit_transposes(cx, src, b_src, ntok, nchunk, evac)


def emit_ln(cx, r, b_r, lng, lnb, b_ln, out, b_out, tmp, b_tmp):
    k = cx.k
    st, mv = tmp
    for hf in range(2):
        k.op("dve", lambda e, hf=hf: e.bn_stats(st[:, hf, :], r[:, hf * 512:(hf + 1) * 512]),
             reads=[b_r], writes=[b_tmp])
    k.op("dve", lambda e: e.bn_aggr(mv[:, 0:2], st[:, :, :]), reads=[b_tmp], writes=[b_tmp])
    k.op("dve", lambda e: e.tensor_scalar_add(mv[:, 1:2], mv[:, 1:2], LN_EPS), reads=[b_tmp], writes=[b_tmp])
    k.op("act", lambda e: e.activation(out=mv[:, 2:3], in_=mv[:, 1:2], func=AF.Ln), reads=[b_tmp], writes=[b_tmp])
    k.op("act", lambda e: e.activation(out=mv[:, 2:3], in_=mv[:, 2:3], func=AF.Exp, scale=-0.5),
         reads=[b_tmp], writes=[b_tmp])
    k.op("dve", lambda e: e.scalar_tensor_tensor(out=mv[:, 3:4], in0=mv[:, 0:1], scalar=-1.0, in1=mv[:, 2:3],
                                                 op0=ALU.mult, op1=ALU.mult), reads=[b_tmp], writes=[b_tmp])
    k.op("act", lambda e: e.activation(out=r, in_=r, func=AF.Identity, scale=mv[:, 2:3], bias=mv[:, 3:4]),
         reads=[b_tmp, b_r], writes=[b_r])
    k.op("pool", lambda e: e.tensor_tensor(out=r, in0=r, in1=lng, op=ALU.mult), reads=[b_r, b_ln], writes=[b_r])
    k.op("pool", lambda e: e.tensor_tensor(out=out, in0=r, in1=lnb, op=ALU.add), reads=[b_r, b_ln],
         writes=[b_out] if b_out is not b_r else [b_r])


def build_tail(cfg, k=None, cx=None, io=None):
    own = k is None
    if own:
        k = KB()
        cx = Ctx(k)
    mixer = cfg["mixer"]
    NE = cfg["ne"]
    F = cfg.get("f", 0)
    FC = F // 128
    groups = cfg["groups"]
    NTOK = cfg["ntok"]
    nms = cfg["nmod"]

    h_in, b_hin = k.dr(io, "h_in", [NTOK, D], F32, "ExternalInput")
    h_out, b_hout = k.dr(io, "h_out", [NTOK, D], F32, "ExternalOutput")
    ncb = 4 + 2 * nms
    cbc_d, b_cbcd = k.dr(io, "cbc", [128, ncb, D], F32, "ExternalInput")
    if "mods_src" not in cfg:
        cT_d, b_cTd = k.dr(io, "cT", [128, 4 * nms, KC], F32, "ExternalInput")
    cbc = k.sb("cbc_sb", [128, ncb, D])
    b_cbc = k.buf("cbc")
    cT = k.sb("cT_sb", [128, 4 * nms, KC])
    b_cT = k.buf("cT")
    if "mods_src" in cfg:
        mods_ap, b_mods, li = cfg["mods_src"]
        k.dma("sp", cbc[:, 0:4, :], cbc_d, reads=[b_cbcd], writes=[b_cbc])
        for m in range(nms):
            for gi, blk in ((0, 2), (1, 5)):
                k.dma("sp", cbc[:, 4 + 2 * m + gi, :], mods_ap[li, m, blk * D:(blk + 1) * D].partition_broadcast(128),
                      reads=[b_mods], writes=[b_cbc])
            for ci, blk in ((0, 0), (1, 1), (2, 3), (3, 4)):
                k.dma("sp", cT[:, 4 * m + ci, :], mods_ap[li, m, blk * D:(blk + 1) * D].rearrange("(c p) -> p c", p=128),
                      reads=[b_mods], writes=[b_cT], allow_slow_non_contiguous=True)
    else:
        k.dma("sp", cbc[:], cbc_d, reads=[b_cbcd], writes=[b_cbc])
        k.dma("sp", cT[:], cT_d, reads=[b_cTd], writes=[b_cT])
    for m in range(nms):
        for i in (1, 3):
            k.op("dve", lambda e, m=m, i=i: e.tensor_scalar_add(cT[:, 4 * m + i, :], cT[:, 4 * m + i, :], 1.0),
                 reads=[b_cT], writes=[b_cT])

    hb = k.sb("hb", [128, 4, D])
    b_hb = k.bufs(4, "hb")
    lnst = k.sb("lnst", [128, 2, 6])
    lnmv = k.sb("lnmv", [128, 4])
    b_lnt = k.buf("lnt")
    tt = k.sb("tt", [128, 2, 512])
    b_tt = k.bufs(2, "tt")
    mT = None
    if mixer == "pre":
        ylat_d, b_ylat = io["y_lat"]
        yctx_d, b_yctx = io["y_ctx"]
        ypre = k.sb("ypre", [128, D])
        b_ypre = k.buf("ypre")
    if mixer == "even":
        mix_d, b_mixd = k.dr(io, "mix", [NTOK, 2 * D], F32, "ExternalInput")
        wout_d, b_woutd = k.dr(io, "w_out", [2 * D, D], F32, "ExternalInput")
        mixb = k.sb("mixb", [128, 2 * D])
        b_mixb = k.buf("mixb")
        mT = k.sb("mT", [128, 16, 512], BF16)
        b_mT = k.buf("mT")
    if mixer == "sc":
        NV = cfg["nv"]
        NH = cfg.get("nhalo", 2)
        hal_d, b_hald = k.dr(io, "hal", [NH, D], F32, "ExternalInput")
        if NH == 2:
            halv_d, b_halvd = k.dr(io, "halv", [128, 2], F32, "ExternalInput")
        else:
            halsel_d, b_halseld = k.dr(io, "halsel", [128, 2, KC, NH], F32, "ExternalInput")
            halsel = k.sb("halsel_sb", [128, 2, KC, NH])
            b_halsel = k.buf("halsel")
            k.dma("sp", halsel[:], halsel_d, reads=[b_halseld], writes=[b_halsel])
            hsel = k.sb("hsel", [128, KC, NH])
            b_hsel = k.buf("hsel")
            hv = k.sb("hv", [128, 2, KC, 1])
            b_hv = k.buf("hv")
        scwin_d, b_scwind = k.dr(io, "sc_w_in", [D, 3 * D], F32, "ExternalInput")
        scwout_d, b_scwoutd = k.dr(io, "sc_w_out", [D, D], F32, "ExternalInput")
        cw_d, b_cwd = k.dr(io, "sc_cw", [128, KC, 3], F32, "ExternalInput")
        vT_d, b_vTd = k.dr(io, "vT_scr", [D, NV], F32, "Internal")
        gbT_d, b_gbTd = k.dr(io, "gbT_scr", [D, NV], F32, "Internal")
        cw = k.sb("cw_sb", [128, KC, 3])
        b_cw = k.buf("cw")
        k.dma("sp", cw[:], cw_d, reads=[b_cwd], writes=[b_cw])
        halv = k.sb("halv_sb", [128, 2])
        b_halv = k.buf("halv")
        if NH == 2:
            k.dma("sp", halv[:], halv_d, reads=[b_halvd], writes=[b_halv])
        halb = k.sb("halb", [NH, D])
        b_halb = k.buf("halb")
        vwin = k.sb("vwin", [128, KC, 514])
        b_vwin = k.buf("vwin")
        gbw = k.sb("gbw", [128, KC, 512])
        b_gbw = k.buf("gbw")
        mT = k.sb("mT", [128, KC, 512], BF16)
        b_mT = k.buf("mT")
        if cfg.get("debug"):
            dbgm_d, b_dbgm = k.dr(io, "dbgm", [128, KC, 512], BF16, "ExternalOutput")
            dbgu_d, b_dbgu = k.dr(io, "dbgu", [128, KC, 512], BF16, "ExternalOutput")
            dbgv_d, b_dbgv = k.dr(io, "dbgv", [128, KC, 514], F32, "ExternalOutput")
            dbgg_d, b_dbgg = k.dr(io, "dbgg", [128, KC, 512], F32, "ExternalOutput")
        cacc = k.sb("cacc", [128, 2, 512])
        b_cacc = k.bufs(2, "cacc")
        gcs = k.sb("gcs", [128, 2, 512])
        b_gcs = k.bufs(2, "gcs")
        zcol = k.sb("zcol", [128, KC, 2])
        b_zcol = k.buf("zcol")
    if NE > 0 or mixer == "sc":
        uT = k.sb("uT", [128, KC, 512], BF16)
        b_uT = k.buf("uT")
    if NE > 0:
        w1_d, b_w1d = k.dr(io, "w1", [NE, D, F], F32, "ExternalInput")
        w3_d, b_w3d = k.dr(io, "w3", [NE, D, F], F32, "ExternalInput")
        w2_d, b_w2d = k.dr(io, "w2", [NE, F, D], F32, "ExternalInput")
        hT = k.sb("hT", [128, FC, 512], BF16)
        b_hT = k.buf("hT")
        sil = k.sb("sil", [128, 2, 512])
        b_sil = k.bufs(2, "sil")
    if NE > 1:
        w1s, w3s, w2s = w1_d, w3_d, w2_d
        w1_d = k.nc.dram_tensor(f"w1_bf{k.nsb}", [NE, D, F], BF16, kind="Internal").ap()
        w3_d = k.nc.dram_tensor(f"w3_bf{k.nsb}", [NE, D, F], BF16, kind="Internal").ap()
        w2_d = k.nc.dram_tensor(f"w2_bf{k.nsb}", [NE, F, D], BF16, kind="Internal").ap()
        b_w1e, b_w3e, b_w2e = k.bufs(NE, "w1e"), k.bufs(NE, "w3e"), k.bufs(NE, "w2e")
        for ex in range(NE):
            k.dma("pool", w1_d[ex], w1s[ex], reads=[b_w1d], writes=[b_w1e[ex]])
            k.dma("pool", w3_d[ex], w3s[ex], reads=[b_w3d], writes=[b_w3e[ex]])
            k.dma("pool", w2_d[ex], w2s[ex], reads=[b_w2d], writes=[b_w2e[ex]])
        wr_d, b_wrd = k.dr(io, "w_router", [D, NE], F32, "ExternalInput")
        wr = k.sb("wr_sb", [128, KC, NE])
        b_wr = k.buf("wr")
        k.dma("sp", wr[:], wr_d.rearrange("(c p) n -> p c n", p=128), reads=[b_wrd], writes=[b_wr])
        u32 = k.sb("u32", [128, KC, 128])
        b_u32 = k.buf("u32")
        gates = k.sb("gates", [128, 4, NE])
        b_gates = k.bufs(4, "gates")
        if cfg.get("debug"):
            dbg_d, b_dbgd = k.dr(io, "dbg", [128, NTOK // 128, NE], F32, "ExternalOutput")
        rt = k.sb("rt", [128, 4, NE])
        rs = k.sb("rs", [128, 8])
        b_rt = k.buf("rt")

    accbank = [0, 1, 6, 7]

    def racc(j, hf, ms, gidx, bank, bb, ge=None):
        gate = cbc[:, 4 + 2 * ms + gidx, hf * 512:(hf + 1) * 512]
        tb = tt[:, (j + hf) % 2, :]
        b_tb = b_tt[(j + hf) % 2]
        dst = hb[:, j, hf * 512:(hf + 1) * 512]
        if ge is None:
            k.op("dve", lambda e: e.tensor_tensor(out=tb, in0=bank[:, :], in1=gate, op=ALU.mult),
                 reads=[bb, b_cbc], writes=[b_tb])
        else:
            gap, b_g = ge
            k.op("dve", lambda e: e.scalar_tensor_tensor(out=tb, in0=bank[:, :], scalar=gap, in1=gate,
                                                         op0=ALU.mult, op1=ALU.mult),
                 reads=[bb, b_cbc, b_g], writes=[b_tb])
        k.op("pool", lambda e: e.tensor_tensor(out=dst, in0=dst, in1=tb, op=ALU.add),
             reads=[b_tb, b_hb[j]], writes=[b_hb[j]])

    def scale_res(j):
        k.op("act", lambda e: e.mul(hb[:, j, :], hb[:, j, :], ALPHA), reads=[b_hb[j]], writes=[b_hb[j]])

    def ln_tile(j, which):
        emit_ln(cx, hb[:, j, :], b_hb[j], cbc[:, 2 * which, :], cbc[:, 2 * which + 1, :], b_cbc,
                hb[:, j, :], b_hb[j], (lnst, lnmv), b_lnt)

    def sc_proj_group(src_tiles, ntoks, ms, is_halo):
        N = sum(ntoks)

        def mod_step(w, bw):
            col = 0
            for (ap, bsrc), ntk in zip(src_tiles, ntoks):
                emit_modT(cx, ap, bsrc, ntk, uT, b_uT, col, cT[:, 4 * ms + 1, :], cT[:, 4 * ms + 0, :], b_cT)
                col += ntk
        cx.step(None, mod_step)
        for half in range(2):
            held = {}

            def fn_gc(w, bw, half=half):
                for cc in range(4):
                    bank, bb = cx.bank[2 + cc % 2], cx.b_bank[2 + cc % 2]
                    for kc in range(KC):
                        k.op("pe", lambda e, kc=kc, cc=cc, bank=bank: e.matmul(
                            bank[:, 0:N], lhsT=w[:, kc, cc * 128:(cc + 1) * 128], rhs=uT[:, kc, 0:N],
                            start=(kc == 0), stop=(kc == KC - 1)), reads=[bw, b_uT], writes=[bb])
                    k.op("act", lambda e, cc=cc, bank=bank: e.activation(
                        out=gcsb[:, cc, 0:N], in_=bank[:, 0:N], func=AF.Copy), reads=[bb], writes=[b_gcsb])

            def fn_h(w, bw, half=half):
                for cc in range(4):
                    c = half * 4 + cc
                    bank, bb = cx.bank[4 + cc % 2], cx.b_bank[4 + cc % 2]
                    for kc in range(KC):
                        k.op("pe", lambda e, kc=kc, cc=cc, bank=bank: e.matmul(
                            bank[:, 0:N], lhsT=w[:, kc, cc * 128:(cc + 1) * 128], rhs=uT[:, kc, 0:N],
                            start=(kc == 0), stop=(kc == KC - 1)), reads=[bw, b_uT], writes=[bb])
                    k.op("dve", lambda e, cc=cc, c=c, bank=bank: e.tensor_tensor(
                        out=vwin[:, c, 0:N], in0=bank[:, 0:N], in1=gcsb[:, cc, 0:N], op=ALU.mult),
                        reads=[bb, b_gcsb], writes=[b_vwin])
                    if is_halo and NH == 2:
                        k.op("dve", lambda e, c=c: e.tensor_tensor(
                            out=vwin[:, c, 0:N], in0=vwin[:, c, 0:N], in1=halv[:, 0:N], op=ALU.mult),
                            reads=[b_vwin, b_halv], writes=[b_vwin])

            def fn_gb(w, bw, half=half):
                for cc in range(4):
                    c = half * 4 + cc
                    bank, bb = cx.bank[2 + cc % 2], cx.b_bank[2 + cc % 2]
                    for kc in range(KC):
                        k.op("pe", lambda e, kc=kc, cc=cc, bank=bank: e.matmul(
                            bank[:, 0:N], lhsT=w[:, kc, cc * 128:(cc + 1) * 128], rhs=uT[:, kc, 0:N],
                            start=(kc == 0), stop=(kc == KC - 1)), reads=[bw, b_uT], writes=[bb])
                    k.op("act", lambda e, c=c, bank=bank: e.activation(
                        out=gbw[:, c, 0:N], in_=bank[:, 0:N], func=AF.Copy), reads=[bb], writes=[b_gbw])

            c0 = half * 512
            cx.step((scwin_d[:, D + c0:D + c0 + 512], b_scwind, KC), fn_gc)
            cx.step((scwin_d[:, 2 * D + c0:2 * D + c0 + 512], b_scwind, KC), fn_h)
            cx.step((scwin_d[:, c0:c0 + 512], b_scwind, KC), fn_gb)
        return N

    if mixer == "sc":
        gcsb = k.sb("gcsb", [128, 4, 512])
        b_gcsb = k.buf("gcsb")
        k.op("pool", lambda e: e.memset(zcol[:], 0.0), writes=[b_zcol])
        for zc in cfg["zero_cols"]:
            k.dma("sp", vT_d[:, zc:zc + 1].rearrange("(c p) n -> p c n", p=128), zcol[:, :, 0:1],
                  reads=[b_zcol], writes=[b_vTd], allow_slow_non_contiguous=True)
        k.dma("sp", halb[:], hal_d, reads=[b_hald], writes=[b_halb])

        def st1_store(N, vcols):
            def fn(w, bw):
                for (s0, n, d0) in vcols:
                    kw = dict(allow_slow_non_contiguous=True) if n == 1 else {}
                    k.dma("sp", vT_d[:, d0:d0 + n].rearrange("(c p) n -> p c n", p=128), vwin[:, :, s0:s0 + n],
                          reads=[b_vwin], writes=[b_vTd], **kw)
                    k.dma("sp", gbT_d[:, d0:d0 + n].rearrange("(c p) n -> p c n", p=128), gbw[:, :, s0:s0 + n],
                          reads=[b_gbw], writes=[b_gbTd], **kw)
            return fn

        hc = cfg["halo_cols"]
        sc_proj_group([(halb[0:NH, :], b_halb)], [NH], 0, True)
        if NH == 2:
            cx.step(None, st1_store(2, [(0, 1, hc[0]), (1, 1, hc[1])]))
        else:
            def halsel_step(w, bw):
                for s in range(2):
                    k.op("dve", lambda e, s=s: e.tensor_tensor(out=hsel[:], in0=vwin[:, :, 0:NH], in1=halsel[:, s, :, :],
                                                               op=ALU.mult), reads=[b_vwin, b_halsel], writes=[b_hsel])
                    k.op("dve", lambda e, s=s: e.reduce_sum(hv[:, s, :, 0], hsel[:], axis=AX.X), reads=[b_hsel], writes=[b_hv])
                    k.dma("sp", vT_d[:, hc[s]:hc[s] + 1].rearrange("(c p) n -> p c n", p=128), hv[:, s, :, :],
                          reads=[b_hv], writes=[b_vTd], allow_slow_non_contiguous=True)
            cx.step(None, halsel_step)
        for (tok0, nt, ms, vcol0) in groups:
            def ld(w, bw, tok0=tok0, nt=nt):
                k.dma("sp", hb[:, 0:nt, :], h_in[tok0:tok0 + nt * 128, :].rearrange("(t p) d -> p t d", p=128),
                      reads=[b_hin], writes=b_hb[0:nt])
            cx.step(None, ld)
            sc_proj_group([(hb[:, j, :], b_hb[j]) for j in range(nt)], [128] * nt, ms, False)
            if cfg.get("debug") and tok0 == 0:
                def dbgu(w, bw):
                    k.dma("sp", dbgu_d, uT[:], reads=[b_uT], writes=[b_dbgu])
                cx.step(None, dbgu)
            cx.step(None, st1_store(nt * 128, [(0, nt * 128, vcol0)]))

    for (tok0, nt, ms, vcol0) in groups:
        N = nt * 128

        def ld(w, bw, tok0=tok0, nt=nt):
            k.dma("sp", hb[:, 0:nt, :], h_in[tok0:tok0 + nt * 128, :].rearrange("(t p) d -> p t d", p=128),
                  reads=[b_hin], writes=b_hb[0:nt])
        cx.step(None, ld)

        if mixer == "even":
            def prep(w, bw, tok0=tok0, nt=nt):
                for j in range(nt):
                    k.dma("sp", mixb[:], mix_d[tok0 + j * 128:tok0 + (j + 1) * 128, :], reads=[b_mixd],
                          writes=[b_mixb])
                    emit_copyT(cx, mixb, b_mixb, 128, 16, mT, b_mT, j * 128)
                    scale_res(j)
            cx.step(None, prep)
            for hf in range(2):
                for kp in range(2):
                    def fn(w, bw, hf=hf, kp=kp, nt=nt, ms=ms):
                        for j in range(nt):
                            bank, bb = cx.bank[accbank[j]], cx.b_bank[accbank[j]]
                            for kc in range(KC):
                                k.op("pe", lambda e, kc=kc, j=j, bank=bank: e.matmul(
                                    bank[:, :], lhsT=mT[:, kp * KC + kc, j * 128:(j + 1) * 128], rhs=w[:, kc, :],
                                    start=(kp == 0 and kc == 0), stop=(kp == 1 and kc == KC - 1)),
                                    reads=[bw, b_mT], writes=[bb])
                            if kp == 1:
                                racc(j, hf, ms, 0, bank, bb)
                    cx.step((wout_d[kp * D:(kp + 1) * D, hf * 512:(hf + 1) * 512], b_woutd, KC), fn)
        elif mixer == "sc":
            def prep(w, bw, nt=nt, N=N, vcol0=vcol0, tok0=tok0):
                k.dma("sp", vwin[:, :, 0:N + 2], vT_d[:, vcol0 - 1:vcol0 + N + 1].rearrange("(c p) n -> p c n", p=128),
                      reads=[b_vTd], writes=[b_vwin])
                k.dma("sp", gbw[:, :, 0:N], gbT_d[:, vcol0:vcol0 + N].rearrange("(c p) n -> p c n", p=128),
                      reads=[b_gbTd], writes=[b_gbw])
                for c in range(KC):
                    eng = "dve"
                    a = cacc[:, c % 2, 0:N]
                    ba = b_cacc[c % 2]
                    k.op(eng, lambda e, c=c, a=a: e.tensor_scalar(out=a, in0=vwin[:, c, 0:N], scalar1=cw[:, c, 0:1],
                                                                  scalar2=None, op0=ALU.mult),
                         reads=[b_vwin, b_cw], writes=[ba])
                    for tap in (1, 2):
                        k.op(eng, lambda e, c=c, a=a, tap=tap: e.scalar_tensor_tensor(
                            out=a, in0=vwin[:, c, tap:tap + N], scalar=cw[:, c, tap:tap + 1], in1=a,
                            op0=ALU.mult, op1=ALU.add), reads=[b_vwin, b_cw, ba], writes=[ba])
                    k.op(eng, lambda e, c=c, a=a: e.tensor_tensor(out=mT[:, c, 0:N], in0=a, in1=gbw[:, c, 0:N],
                                                                  op=ALU.mult),
                         reads=[ba, b_gbw], writes=[b_mT])
                for j in range(nt):
                    scale_res(j)
                if cfg.get("debug") and tok0 == 0:
                    k.dma("sp", dbgm_d, mT[:], reads=[b_mT], writes=[b_dbgm])
                    k.dma("sp", dbgv_d, vwin[:], reads=[b_vwin], writes=[b_dbgv])
                    k.dma("sp", dbgg_d, gbw[:], reads=[b_gbw], writes=[b_dbgg])
            cx.step(None, prep)
            for hf in range(2):
                def fn(w, bw, hf=hf, nt=nt, ms=ms):
                    for j in range(nt):
                        bank, bb = cx.bank[accbank[j]], cx.b_bank[accbank[j]]
                        for kc in range(KC):
                            k.op("pe", lambda e, kc=kc, j=j, bank=bank: e.matmul(
                                bank[:, :], lhsT=mT[:, kc, j * 128:(j + 1) * 128], rhs=w[:, kc, :],
                                start=(kc == 0), stop=(kc == KC - 1)), reads=[bw, b_mT], writes=[bb])
                        racc(j, hf, ms, 0, bank, bb)
                cx.step((scwout_d[:, hf * 512:(hf + 1) * 512], b_scwoutd, KC), fn)
        elif mixer == "pre":
            def prep(w, bw, tok0=tok0, nt=nt, ms=ms):
                for j in range(nt):
                    scale_res(j)
                    if ms == 0:
                        sap, bsrc = ylat_d[tok0 + j * 128:tok0 + (j + 1) * 128, :], b_ylat
                    else:
                        sap, bsrc = yctx_d[j * 128:(j + 1) * 128, :], b_yctx
                    gate = cbc[:, 4 + 2 * ms, :]
                    k.dma("sp", ypre[:], sap, reads=[bsrc], writes=[b_ypre])
                    k.op("dve", lambda e, gate=gate: e.tensor_tensor(out=ypre[:], in0=ypre[:], in1=gate, op=ALU.mult),
                         reads=[b_ypre, b_cbc], writes=[b_ypre])
                    k.op("pool", lambda e, j=j: e.tensor_tensor(out=hb[:, j, :], in0=hb[:, j, :], in1=ypre[:], op=ALU.add),
                         reads=[b_ypre, b_hb[j]], writes=[b_hb[j]])
            cx.step(None, prep)
        if mixer is not None:
            def ln1(w, bw, nt=nt):
                for j in range(nt):
                    ln_tile(j, 0)
            cx.step(None, ln1)

        if NE > 0:
            def pre_ffn(w, bw, nt=nt, ms=ms):
                for j in range(nt):
                    if NE > 1:
                        emit_modT(cx, hb[:, j, :], b_hb[j], 128, uT, b_uT, j * 128, cT[:, 4 * ms + 3, :],
                                  cT[:, 4 * ms + 2, :], b_cT, dst32=None)
                        def evac(bank, bb, g, n, ms=ms):
                            for c in range(n):
                                cc = g + c
                                k.op("act", lambda e, c=c, cc=cc, bank=bank: e.activation(
                                    out=u32[:, cc, :], in_=bank[:, c * 128:(c + 1) * 128], func=AF.Identity,
                                    scale=cT[:, 4 * ms + 3, cc:cc + 1], bias=cT[:, 4 * ms + 2, cc:cc + 1]),
                                    reads=[bb, b_cT], writes=[b_u32])
                        emit_transposes(cx, hb[:, j, :], b_hb[j], 128, KC, evac)
                        lb, blb = cx.bank[6], cx.b_bank[6]
                        for kc in range(KC):
                            k.op("pe", lambda e, kc=kc: e.matmul(lb[:, 0:NE], lhsT=u32[:, kc, :], rhs=wr[:, kc, :],
                                                                 start=(kc == 0), stop=(kc == KC - 1)),
                                 reads=[b_u32, b_wr], writes=[blb])
                        L = rt[:, 0, :]
                        E1 = rt[:, 1, :]
                        L2 = rt[:, 2, :]
                        E2 = rt[:, 3, :]
                        G = gates[:, j, :]
                        dv = lambda f, r=(), w_=(): k.op("dve", f, reads=[b_rt] + list(r), writes=[b_rt] + list(w_))
                        dv(lambda e: e.tensor_copy(L, lb[:, 0:NE]), r=[blb])
                        dv(lambda e: e.reduce_max(rs[:, 0:1], L, axis=AX.X))
                        dv(lambda e: e.tensor_scalar(out=E1, in0=L, scalar1=rs[:, 0:1], scalar2=None, op0=ALU.is_equal))
                        dv(lambda e: e.scalar_tensor_tensor(out=L2, in0=E1, scalar=-1e30, in1=L, op0=ALU.mult,
                                                            op1=ALU.add))
                        dv(lambda e: e.reduce_max(rs[:, 1:2], L2, axis=AX.X))
                        dv(lambda e: e.tensor_scalar(out=E2, in0=L2, scalar1=rs[:, 1:2], scalar2=None,
                                                     op0=ALU.is_equal))
                        dv(lambda e: e.tensor_tensor(out=rs[:, 2:3], in0=rs[:, 1:2], in1=rs[:, 0:1], op=ALU.subtract))
                        k.op("act", lambda e: e.activation(out=rs[:, 3:4], in_=rs[:, 2:3], func=AF.Exp),
                             reads=[b_rt], writes=[b_rt])
                        dv(lambda e: e.tensor_scalar_add(rs[:, 4:5], rs[:, 3:4], 1.0))
                        dv(lambda e: e.reciprocal(rs[:, 5:6], rs[:, 4:5]))
                        dv(lambda e: e.tensor_tensor(out=rs[:, 6:7], in0=rs[:, 3:4], in1=rs[:, 5:6], op=ALU.mult))
                        dv(lambda e: e.tensor_scalar(out=E1, in0=E1, scalar1=rs[:, 5:6], scalar2=None, op0=ALU.mult))
                        dv(lambda e, G=G: e.scalar_tensor_tensor(out=G, in0=E2, scalar=rs[:, 6:7], in1=E1,
                                                                 op0=ALU.mult, op1=ALU.add), w_=[b_gates[j]])
                    else:
                        emit_modT(cx, hb[:, j, :], b_hb[j], 128, uT, b_uT, j * 128, cT[:, 4 * ms + 3, :],
                                  cT[:, 4 * ms + 2, :], b_cT)
                    scale_res(j)
            cx.step(None, pre_ffn)
            if cfg.get("debug") and NE > 1:
                def dbgst(w, bw, nt=nt, tok0=tok0):
                    k.dma("sp", dbg_d[:, tok0 // 128:tok0 // 128 + nt, :], gates[:, 0:nt, :], reads=b_gates[0:nt],
                          writes=[b_dbgd])
                cx.step(None, dbgst)

            p13 = [(c0, min(4, FC - c0)) for c0 in range(0, FC, 4)]
            p2 = []
            c0 = 0
            while c0 < FC:
                n = min(8 if (FC - c0) % 7 else 7, FC - c0)
                p2.append((c0, n))
                c0 += n
            for ex in range(NE):
                for (f0, nf) in p13:
                    hold = {}

                    def fn1(w, bw, hold=hold):
                        hold["w1"] = (w, bw)

                    def fn3(w3, bw3, hold=hold, f0=f0, nf=nf, N=N):
                        w1s, bw1 = hold["w1"]
                        for ff in range(nf):
                            fc = f0 + ff
                            A, bA = cx.bank[2 + fc % 2], cx.b_bank[2 + fc % 2]
                            B, bB = cx.bank[4 + fc % 2], cx.b_bank[4 + fc % 2]
                            for kc in range(KC):
                                k.op("pe", lambda e, kc=kc, ff=ff, A=A: e.matmul(
                                    A[:, 0:N], lhsT=w1s[:, kc, ff * 128:(ff + 1) * 128], rhs=uT[:, kc, 0:N],
                                    start=(kc == 0), stop=(kc == KC - 1)), reads=[bw1, b_uT], writes=[bA])
                            for kc in range(KC):
                                k.op("pe", lambda e, kc=kc, ff=ff, B=B: e.matmul(
                                    B[:, 0:N], lhsT=w3[:, kc, ff * 128:(ff + 1) * 128], rhs=uT[:, kc, 0:N],
                                    start=(kc == 0), stop=(kc == KC - 1)), reads=[bw3, b_uT], writes=[bB])
                            k.op("act", lambda e, fc=fc, A=A: e.activation(out=sil[:, fc % 2, 0:N], in_=A[:, 0:N],
                                                                            func=AF.Silu),
                                 reads=[bA], writes=[b_sil[fc % 2]])
                            k.op("dve", lambda e, fc=fc, B=B: e.tensor_tensor(
                                out=hT[:, fc, 0:N], in0=B[:, 0:N], in1=sil[:, fc % 2, 0:N], op=ALU.mult),
                                reads=[bB, b_sil[fc % 2]], writes=[b_hT])
                    cx.step((w1_d[ex, :, f0 * 128:(f0 + nf) * 128], b_w1e[ex] if NE > 1 else b_w1d, KC), fn1)
                    cx.step((w3_d[ex, :, f0 * 128:(f0 + nf) * 128], b_w3e[ex] if NE > 1 else b_w3d, KC), fn3)
                for hf in range(2):
                    for pi, (f0, nf) in enumerate(p2):
                        def fn2(w, bw, hf=hf, pi=pi, f0=f0, nf=nf, nt=nt, ms=ms, ex=ex):
                            for j in range(nt):
                                bank, bb = cx.bank[accbank[j]], cx.b_bank[accbank[j]]
                                for ff in range(nf):
                                    fc = f0 + ff
                                    k.op("pe", lambda e, ff=ff, fc=fc, j=j, bank=bank: e.matmul(
                                        bank[:, :], lhsT=hT[:, fc, j * 128:(j + 1) * 128], rhs=w[:, ff, :],
                                        start=(fc == 0), stop=(fc == FC - 1)), reads=[bw, b_hT], writes=[bb])
                                if pi == len(p2) - 1:
                                    ge = None if NE == 1 else (gates[:, j, ex:ex + 1], b_gates[j])
                                    racc(j, hf, ms, 1, bank, bb, ge)
                        cx.step((w2_d[ex, f0 * 128:(f0 + nf) * 128, hf * 512:(hf + 1) * 512],
                                 b_w2e[ex] if NE > 1 else b_w2d, nf), fn2)

            def ln2(w, bw, nt=nt):
                for j in range(nt):
                    ln_tile(j, 1)
            cx.step(None, ln2)

        def st(w, bw, tok0=tok0, nt=nt):
            k.dma("sp", h_out[tok0:tok0 + nt * 128, :].rearrange("(t p) d -> p t d", p=128), hb[:, 0:nt, :],
                  reads=b_hb[0:nt], writes=[b_hout])
        cx.step(None, st)
    cx.flush()
    if own:
        return k.finish()


def build_mods():
    k = KB()
    cT_d, b_cTd = k.dram("cT3", [128, KC, 3], F32, "ExternalInput")
    w_d, b_wd = k.dram("ada_w", [D, 3072], F32, "ExternalInput")
    bias_d, b_biasd = k.dram("ada_b3", [3, 3072], F32, "ExternalInput")
    out_d, b_outd = k.dram("mods", [3, 3072], F32, "ExternalOutput")
    s = k.sb("s", [128, KC, 3])
    b_s = k.buf()
    bias = k.sb("bias", [3, 3072])
    b_bias = k.buf()
    res = k.sb("res", [3, 3072])
    b_res = k.buf()
    wt = [k.sb(f"wt{i}", [128, KC, 512]) for i in range(2)]
    b_wt = k.bufs(2, "wt")
    bank = [k.ps(f"bk{i}", [128, 512]) for i in range(2)]
    b_bank = k.bufs(2, "bk")
    k.dma("sp", s[:], cT_d, reads=[b_cTd], writes=[b_s])
    k.dma("sp", bias[:], bias_d, reads=[b_biasd], writes=[b_bias])
    k.op("act", lambda e: e.activation(out=s[:], in_=s[:], func=AF.Silu), reads=[b_s], writes=[b_s])
    for blk in range(6):
        i = blk % 2
        k.dma("sp", wt[i][:], w_d[:, blk * 512:(blk + 1) * 512].rearrange("(c p) n -> p c n", p=128),
              reads=[b_wd], writes=[b_wt[i]])
        for kc in range(KC):
            k.op("pe", lambda e, kc=kc, i=i: e.matmul(bank[i][0:3, :], lhsT=s[:, kc, :], rhs=wt[i][:, kc, :],
                                                     start=(kc == 0), stop=(kc == KC - 1)),
                 reads=[b_s, b_wt[i]], writes=[b_bank[i]])
        k.op("dve", lambda e, blk=blk, i=i: e.tensor_tensor(out=res[:, blk * 512:(blk + 1) * 512], in0=bank[i][0:3, :],
                                                            in1=bias[:, blk * 512:(blk + 1) * 512], op=ALU.add),
             reads=[b_bank[i], b_bias], writes=[b_res])
    k.dma("sp", out_d, res[:], reads=[b_res], writes=[b_outd])
    return k.finish()


def build_proj(cfg):
    k = KB()
    cx = Ctx(k)
    groups = cfg["groups"]
    NTOK = cfg["ntok"]
    nms = cfg["nmod"]
    NCOL = cfg["ncol"]
    h_in, b_hin = k.dram("h_in", [NTOK, D], F32, "ExternalInput")
    w_d, b_wd = k.dram("w", [D, NCOL], F32, "ExternalInput")
    out_d, b_outd = k.dram("proj", [NTOK, NCOL], F32, "ExternalOutput")
    cT_d, b_cTd = k.dram("cT", [128, 2 * nms, KC], F32, "ExternalInput")
    cT = k.sb("cT_sb", [128, 2 * nms, KC])
    b_cT = k.buf("cT")
    k.dma("sp", cT[:], cT_d, reads=[b_cTd], writes=[b_cT])
    for m in range(nms):
        k.op("dve", lambda e, m=m: e.tensor_scalar_add(cT[:, 2 * m + 1, :], cT[:, 2 * m + 1, :], 1.0),
             reads=[b_cT], writes=[b_cT])
    hb = k.sb("hb", [128, 4, D])
    b_hb = k.bufs(4, "hb")
    uT = k.sb("uT", [128, KC, 512], BF16)
    b_uT = k.buf("uT")
    ob = k.sb("ob", [128, 4, 2, 512])
    b_ob = [k.bufs(2, f"ob{j}") for j in range(4)]
    pieces = [(c0, min(512, NCOL - c0)) for c0 in range(0, NCOL, 512)]
    for (tok0, nt, ms) in groups:
        def ld(w, bw, tok0=tok0, nt=nt, ms=ms):
            k.dma("sp", hb[:, 0:nt, :], h_in[tok0:tok0 + nt * 128, :].rearrange("(t p) d -> p t d", p=128),
                  reads=[b_hin], writes=b_hb[0:nt])
            for j in range(nt):
                emit_modT(cx, hb[:, j, :], b_hb[j], 128, uT, b_uT, j * 128, cT[:, 2 * ms + 1, :], cT[:, 2 * ms, :], b_cT)
        cx.step(None, ld)
        for pi, (c0, nc_) in enumerate(pieces):
            def fn(w, bw, pi=pi, c0=c0, nc_=nc_, nt=nt, tok0=tok0):
                for j in range(nt):
                    bi = 2 + (pi * 4 + j) % 4
                    bank, bb = cx.bank[bi], cx.b_bank[bi]
                    for kc in range(KC):
                        k.op("pe", lambda e, kc=kc, j=j, bank=bank: e.matmul(
                            bank[:, 0:nc_], lhsT=uT[:, kc, j * 128:(j + 1) * 128], rhs=w[:, kc, 0:nc_],
                            start=(kc == 0), stop=(kc == KC - 1)), reads=[bw, b_uT], writes=[bb])
                    o = ob[:, j, pi % 2, 0:nc_]
                    bo = b_ob[j][pi % 2]
                    eng = "act" if (pi + j) % 2 == 0 else "dve"
                    if eng == "act":
                        k.op("act", lambda e, o=o, bank=bank: e.activation(out=o, in_=bank[:, 0:nc_], func=AF.Copy),
                             reads=[bb], writes=[bo])
                    else:
                        k.op("dve", lambda e, o=o, bank=bank: e.tensor_copy(o, bank[:, 0:nc_]), reads=[bb], writes=[bo])
                    k.dma("sp", out_d[tok0 + j * 128:tok0 + (j + 1) * 128, c0:c0 + nc_], o, reads=[bo], writes=[k.buf()])
            cx.step((w_d[:, c0:c0 + nc_], b_wd, KC), fn)
    cx.flush()
    return k.finish()


GRID_W = 64
NEG = -30000.0


def na_block_info(rows):
    info = []
    for t in range(rows // 2):
        r = 2 * t
        if r == 0:
            cls = 0
        elif r == 2:
            cls = 1
        elif r == rows - 4:
            cls = 3
        elif r == rows - 2:
            cls = 4
        else:
            cls = 2
        kt0 = min(max(r - 4, 0), rows - 10) // 2
        info.append((cls, kt0))
    return info


def na_mask_bias(rpb4, rows):
    out = np.full((5, 5, 128, 4, 128), NEG, np.float32)
    reps = [0, 2, 4, rows - 4, rows - 2]
    qc = np.arange(64)
    c0 = np.clip(qc - 8, 0, 64 - 16)
    for ci, r in enumerate(reps):
        R0 = min(max(r - 4, 0), rows - 10)
        for dq in range(2):
            qr = r + dq
            r0 = min(max(qr - 4, 0), rows - 8)
            for kr in range(r0, r0 + 8):
                lk = kr - R0
                assert 0 <= lk < 10
                for j in range(16):
                    kc = c0 + j
                    key = lk * 64 + kc
                    q = dq * 64 + qc
                    out[ci, key // 128, key % 128, :, q] = rpb4[:, kr - qr + 7, kc - qc + 15].T
    return out


def build_na(cfg, k=None, cx=None, io=None):
    own = k is None
    if own:
        k = KB()
        cx = Ctx(k)
    rows = cfg["rows"]
    NL = rows * GRID_W
    NCTX = 256
    with_ctx = cfg["with_ctx"]
    NQ = NL + (NCTX if with_ctx else 0)
    info = na_block_info(rows)
    qT_d, b_qTd = k.dr(io, "qT", [4, 64, NQ], F32, "ExternalInput")
    kT_d, b_kTd = k.dr(io, "kT", [4, 64, NL], F32, "ExternalInput")
    kcT_d, b_kcTd = k.dr(io, "kcT", [4, 64, NCTX], F32, "ExternalInput")
    v_d, b_vd = k.dr(io, "v", [NL, 256], F32, "ExternalInput")
    vc_d, b_vcd = k.dr(io, "vc", [NCTX, 256], F32, "ExternalInput")
    mb_d, b_mbd = k.dr(io, "mb", [25, 128, 512], F32, "ExternalInput")
    out_d, b_outd = k.dr(io, "attn", [NQ, 256], F32, "ExternalOutput")

    identb = k.sb("identb", [128, 128], BF16)
    b_identb = k.buf()
    k.op("dve", lambda e: e.tensor_copy(identb[:], cx.ident[:]), reads=[cx.b_ident], writes=[b_identb])
    mb = k.sb("mb_sb", [128, 25, 512], BF16)
    b_mb = k.buf()
    for c in range(5):
        k.dma("pool", mb[:, c * 5:(c + 1) * 5, :], mb_d[c * 5:(c + 1) * 5].rearrange("c p n -> p c n"),
              reads=[b_mbd], writes=[b_mb])
    kc = k.sb("kc_sb", [64, 4, NCTX], BF16)
    b_kc = k.buf()
    k.dma("pool", kc[:], kcT_d.rearrange("h p n -> p h n"), reads=[b_kcTd], writes=[b_kc])
    vcw = k.sb("vcw", [128, 2, 4, 66], BF16)
    b_vcw = k.buf()
    k.op("pool", lambda e: e.memset(vcw[:], 1.0), writes=[b_vcw])
    for c in range(2):
        k.dma("pool", vcw[:, c, :, 0:64], vc_d[c * 128:(c + 1) * 128, :].rearrange("p (h d) -> p h d", h=4),
              reads=[b_vcd], writes=[b_vcw])
    qb32 = [k.sb(f"qb32_{i}", [64, 4, 128]) for i in range(2)]
    b_qb32 = k.bufs(2, "qb32")
    qb = [k.sb(f"qb_{i}", [64, 4, 128], BF16) for i in range(2)]
    b_qb = k.bufs(2, "qb")
    kw = [k.sb(f"kw_{i}", [64, 4, 640], BF16) for i in range(2)]
    b_kw = k.bufs(2, "kw")
    vw = [k.sb(f"vw_{i}", [128, 5, 4, 66], BF16) for i in range(2)]
    b_vw = k.bufs(2, "vw")
    for i in range(2):
        k.op("pool", lambda e, i=i: e.memset(vw[i][:], 1.0), writes=[b_vw[i]])
    PT = [k.sb(f"PT_{i}", [128, 7, 512], BF16) for i in range(2)]
    b_PT = k.bufs(2, "PT")
    rs = k.sb("rs", [128, 2, 4])
    b_rs = k.bufs(2, "rs")
    ob = [k.sb(f"ob_{i}", [128, 4, 64]) for i in range(2)]
    b_ob = k.bufs(2, "ob")

    blocks = [(t, True) for t in range(len(info))]
    if with_ctx:
        blocks += [(NL // 128, False), (NL // 128 + 1, False)]
    sbank = 0
    for bi, (t, local) in enumerate(blocks):
        i = bi % 2
        k.dma("sp", qb32[i][:], qT_d[:, :, t * 128:(t + 1) * 128].rearrange("h p n -> p h n"), reads=[b_qTd],
              writes=[b_qb32[i]])
        k.op("act", lambda e, i=i: e.mul(qb[i][:], qb32[i][:], 0.125), reads=[b_qb32[i]], writes=[b_qb[i]])
        chunks = []
        if local:
            cls, kt0 = info[t]
            k.dma("pool", kw[i][:], kT_d[:, :, kt0 * 128:kt0 * 128 + 640].rearrange("h p n -> p h n"),
                  reads=[b_kTd], writes=[b_kw[i]])
            for c in range(5):
                k.dma("pool", vw[i][:, c, :, 0:64],
                      v_d[(kt0 + c) * 128:(kt0 + c + 1) * 128, :].rearrange("p (h d) -> p h d", h=4),
                      reads=[b_vd], writes=[b_vw[i]])
            for c in range(5):
                chunks.append(("l", c, cls))
        chunks += [("c", 0, 0), ("c", 1, 0)]
        nch = len(chunks)
        for ci, (kind, c, cls) in enumerate(chunks):
            bk = 2 + sbank % 4
            sbank += 1
            bank, bb = cx.bank[bk], cx.b_bank[bk]
            if kind == "l":
                k.op("pe", lambda e, bank=bank, c=c, cls=cls: e.matmul(bank[:, :], lhsT=identb[:], rhs=mb[:, cls * 5 + c, :],
                                                                      start=True, stop=False),
                     reads=[b_identb, b_mb], writes=[bb])
            for h in range(4):
                if kind == "l":
                    lhsT = kw[i][:, h, c * 128:(c + 1) * 128]
                    rd = [b_kw[i], b_qb[i]]
                else:
                    lhsT = kc[:, h, c * 128:(c + 1) * 128]
                    rd = [b_kc, b_qb[i]]
                k.op("pe", lambda e, bank=bank, h=h, lhsT=lhsT, i=i, kind=kind: e.matmul(
                    bank[:, h * 128:(h + 1) * 128], lhsT=lhsT, rhs=qb[i][:, h, :],
                    start=(kind == "c"), stop=(h == 3 or kind == "c")), reads=rd, writes=[bb])
            k.op("act", lambda e, bank=bank, ci=ci, i=i: e.activation(out=PT[i][:, ci, :], in_=bank[:, :], func=AF.Exp),
                 reads=[bb], writes=[b_PT[i]])
        obk = 6 + i
        obank, bob = cx.bank[obk], cx.b_bank[obk]
        for h in range(4):
            for ci, (kind, c, cls) in enumerate(chunks):
                rhs = vw[i][:, c, h, 0:65] if kind == "l" else vcw[:, c, h, 0:65]
                rd = [b_PT[i], b_vw[i] if kind == "l" else b_vcw]
                k.op("pe", lambda e, h=h, ci=ci, rhs=rhs, i=i, obank=obank: e.matmul(
                    obank[:, h * 65:(h + 1) * 65], lhsT=PT[i][:, ci, h * 128:(h + 1) * 128], rhs=rhs,
                    start=(ci == 0), stop=(ci == nch - 1)), reads=rd, writes=[bob])
        ov = obank[:, 0:260].rearrange("p (h d) -> p h d", h=4)
        k.op("dve", lambda e, ov=ov, i=i: e.reciprocal(rs[:, i, :], ov[:, :, 64]), reads=[bob], writes=[b_rs[i]])
        for h in range(4):
            k.op("dve", lambda e, ov=ov, i=i, h=h: e.tensor_scalar(out=ob[i][:, h, :], in0=ov[:, h, 0:64],
                                                                   scalar1=rs[:, i, h:h + 1], scalar2=None, op0=ALU.mult),
                 reads=[bob, b_rs[i]], writes=[b_ob[i]])
        k.dma("sp", out_d[t * 128:(t + 1) * 128, :], ob[i][:].rearrange("p h d -> p (h d)"), reads=[b_ob[i]],
              writes=[k.buf()])
    if own:
        return k.finish()


RMS_EPS = 1e-5


def build_ssd(cfg, k=None, cx=None, io=None):
    own = k is None
    if own:
        k = KB()
        cx = Ctx(k)
    NLAT = cfg["nlat"]
    L = 256 + NLAT
    NCH = L // 128
    LP = 260 + NLAT + 4
    xbc_d, b_xbcd = k.dr(io, "xbcT", [4, 128, LP], F32, "ExternalInput")
    cw_d, b_cwd = k.dr(io, "cwT", [128, 4, 5], F32, "ExternalInput")
    cb_d, b_cbd = k.dr(io, "cb", [128, 4], F32, "ExternalInput")
    dtr_d, b_dtrd = k.dr(io, "dtr", [L, 8], F32, "ExternalInput")
    sm_d, b_smd = k.dr(io, "small", [128, 16], F32, "ExternalInput")
    dn_d, b_dnd = k.dr(io, "dn", [128, 2, 256], F32, "ExternalInput")
    z_d, b_zd = k.dr(io, "z", [L, 256], F32, "ExternalInput")
    out_d, b_outd = k.dr(io, "ssm", [L, 256], F32, "ExternalOutput")
    yf_d, b_yfd = k.dr(io, "yf_scr", [L, 256], F32, "Internal")
    tag = k.nsb
    pxs_d = k.nc.dram_tensor(f"pxs{tag}", [NCH, 128, 512], F32, kind="Internal").ap()
    pxt_d = k.nc.dram_tensor(f"pxt{tag}", [NCH, 128, 256], F32, kind="Internal").ap()
    pbt_d = k.nc.dram_tensor(f"pbt{tag}", [NCH, 128, 128], BF16, kind="Internal").ap()
    pbc_d = k.nc.dram_tensor(f"pbc{tag}", [NCH, 128, 256], BF16, kind="Internal").ap()
    pdt_d = k.nc.dram_tensor(f"pdt{tag}", [NCH, 128, 16], F32, kind="Internal").ap()
    b_prep = [[k.buf(f"prep{c}_{i}") for i in range(6)] for c in range(NCH)]
    if cfg.get("debug"):
        dbgy_d, _ = k.dr(io, "dbgy", [L, 256], F32, "ExternalOutput")

    def const(name, shape, dt=F32):
        return k.sb(name + "_sb", shape, dt), k.buf(name)

    def const2(name, shape, dt=F32):
        return [k.sb(f"{name}{i}_sb", shape, dt) for i in range(2)], [k.buf(f"{name}{i}") for i in range(2)]
    cw, b_cw = const("cw", [128, 4, 5])
    cb, b_cb = const("cb", [128, 4])
    sm, b_sm = const("sm", [128, 16])
    dn, b_dn = const("dn", [128, 2, 256])
    k.dma("sp", cw[:], cw_d, reads=[b_cwd], writes=[b_cw])
    k.dma("sp", cb[:], cb_d, reads=[b_cbd], writes=[b_cb])
    k.dma("sp", sm[:], sm_d, reads=[b_smd], writes=[b_sm])
    k.dma("sp", dn[:], dn_d, reads=[b_dnd], writes=[b_dn])
    k.op("act", lambda e: e.activation(out=sm[:, 8:16], in_=sm[:, 8:16], func=AF.Exp), reads=[b_sm], writes=[b_sm])
    k.op("act", lambda e: e.mul(sm[:, 8:16], sm[:, 8:16], -1.0), reads=[b_sm], writes=[b_sm])
    ones, b_ones = const("ones", [128, 128])
    k.op("pool", lambda e: e.memset(ones[:], 1.0), writes=[b_ones])
    tri, mneg = [], []
    b_tri = k.buf("tri")
    for d, (cmp_, tin, tfill, min_, mfill) in enumerate(((ALU.is_gt, 0.0, 1.0, NEG, 0.0), (ALU.is_ge, 1.0, 0.0, 0.0, NEG))):
        t_ = k.sb(f"tri{d}", [128, 128])
        m_ = k.sb(f"mneg{d}", [128, 128])
        k.op("pool", lambda e, t_=t_, tin=tin: e.memset(t_[:], tin), writes=[b_tri])
        k.op("pool", lambda e, t_=t_, cmp_=cmp_, tfill=tfill: e.affine_select(
            out=t_[:], in_=t_[:], pattern=[[-1, 128]], compare_op=cmp_, fill=tfill, base=0, channel_multiplier=1),
            reads=[b_tri], writes=[b_tri])
        k.op("pool", lambda e, m_=m_, min_=min_: e.memset(m_[:], min_), writes=[b_tri])
        k.op("pool", lambda e, m_=m_, cmp_=cmp_, mfill=mfill: e.affine_select(
            out=m_[:], in_=m_[:], pattern=[[-1, 128]], compare_op=cmp_, fill=mfill, base=0, channel_multiplier=1),
            reads=[b_tri], writes=[b_tri])
        tri.append(t_)
        mneg.append(m_)

    xw_2, b_xw_2 = const2("xw", [128, 4, 132])
    acc, b_acc = const("acc", [128, 128])
    xs_2, b_xs_2 = const2("xs", [128, 4, 128])
    bcb_2, b_bcb_2 = const2("bcb", [128, 2, 128], BF16)
    xtm_2, b_xtm_2 = const2("xtm", [128, 256])
    btm_2, b_btm_2 = const2("btm", [128, 128], BF16)
    dt_2, b_dt_2 = const2("dt", [128, 8])
    la_2, b_la_2 = const2("la", [128, 8])
    cbT_2, b_cbT_2 = const2("cbT", [128, 128])
    labc_2, b_labc_2 = const2("labc", [128, 128])
    ccol_2, b_ccol_2 = const2("ccol", [128, 1])
    seg_2, b_seg_2 = const2("seg", [128, 128])
    LT_2, b_LT_2 = const2("LT", [128, 128])
    Ebc_2, b_Ebc_2 = const2("Ebc", [128, 128])
    MT_2, b_MT_2 = const2("MT", [128, 128], BF16)
    CsT_2, b_CsT_2 = const2("CsT", [128, 128], BF16)
    xdt_2, b_xdt_2 = const2("xdt", [128, 64], BF16)
    xwt_2, b_xwt_2 = const2("xwt", [128, 64], BF16)
    S = k.sb("S", [128, 4, 64])
    Sbf = k.sb("Sbf", [128, 4, 64], BF16)
    b_S = k.bufs(4, "S")
    b_Sbf = k.bufs(4, "Sbf")
    ysb, b_ysb = const("ysb", [128, 256])
    zt, b_zt = const("zt", [128, 256])
    yft, b_yft = const("yft", [128, 256])
    t2, b_t2 = const("t2", [128, 256])
    bst, b_bst = const("bst", [128, 6])
    bmv, b_bmv = const("bmv", [128, 4])
    B0, bB0 = cx.bank[0], cx.b_bank[0]
    B1, bB1 = cx.bank[1], cx.b_bank[1]
    BY, bBY = cx.bank[4], cx.b_bank[4]
    BS, bBS = cx.bank[5], cx.b_bank[5]

    def colof(c):
        return 2 + c * 128 if c < 2 else 262 + (c - 2) * 128

    def run_dir(d):
        last = 127 if d == 0 else 0
        order = list(range(NCH)) if d == 0 else [1, 0] + list(range(NCH - 1, 1, -1))
        for h in range(4):
            k.op("pool", lambda e, h=h: e.memset(S[:, h, :], 0.0), reads=[b_S[h]], writes=[b_S[h]])
            k.op("pool", lambda e, h=h: e.memset(Sbf[:, h, :], 0.0), reads=[b_Sbf[h]], writes=[b_Sbf[h]])
        for ci, c in enumerate(order):
            c0 = colof(c)
            p = ci % 2
            chunk_body(d, last, c, c0, xw_2[p], b_xw_2[p], xs_2[p], b_xs_2[p], bcb_2[p], b_bcb_2[p], xtm_2[p], b_xtm_2[p],
                       btm_2[p], b_btm_2[p], dt_2[p], b_dt_2[p], la_2[p], b_la_2[p], cbT_2[p], b_cbT_2[p])

    def chunk_body(d, last, c, c0, xw, b_xw, xs, b_xs, bcb, b_bcb, xtm, b_xtm, btm, b_btm, dt, b_dt, la, b_la, cbT, b_cbT):
        if d == 1:
            k.dma("sp", xs[:], pxs_d[c].rearrange("p (c n) -> p c n", c=4), reads=[b_prep[c][0]], writes=[b_xs])
            k.dma("sp", xtm[:], pxt_d[c], reads=[b_prep[c][1]], writes=[b_xtm])
            k.dma("sp", btm[:], pbt_d[c], reads=[b_prep[c][2]], writes=[b_btm])
            k.dma("sp", bcb[:], pbc_d[c].rearrange("p (c n) -> p c n", c=2), reads=[b_prep[c][3]], writes=[b_bcb])
            k.dma("sp", dt[:], pdt_d[c][:, 0:8], reads=[b_prep[c][4]], writes=[b_dt])
            k.dma("sp", la[:], pdt_d[c][:, 8:16], reads=[b_prep[c][5]], writes=[b_la])
            k.dma("sp", zt[:], z_d[c * 128:(c + 1) * 128, :], reads=[b_zd], writes=[b_zt])
            k.dma("sp", yft[:], yf_d[c * 128:(c + 1) * 128, :], reads=[b_yfd], writes=[b_yft])
        else:
            k.dma("sp", xw[:], xbc_d[:, :, c0 - 2:c0 + 130].rearrange("c p n -> p c n"), reads=[b_xbcd], writes=[b_xw])
            k.dma("sp", dt[:], dtr_d[c * 128:(c + 1) * 128, :], reads=[b_dtrd], writes=[b_dt])
            if False:
                k.dma("sp", zt[:], z_d[c * 128:(c + 1) * 128, :], reads=[b_zd], writes=[b_zt])
                k.dma("sp", yft[:], yf_d[c * 128:(c + 1) * 128, :], reads=[b_yfd], writes=[b_yft])
            for cc in range(4):
                k.op("dve", lambda e, cc=cc: e.tensor_scalar(out=acc[:], in0=xw[:, cc, 0:128], scalar1=cw[:, cc, 0:1],
                                                             scalar2=None, op0=ALU.mult),
                     reads=[b_xw, b_cw], writes=[b_acc])
                for tap in range(1, 5):
                    k.op("dve", lambda e, cc=cc, tap=tap: e.scalar_tensor_tensor(
                        out=acc[:], in0=xw[:, cc, tap:tap + 128], scalar=cw[:, cc, tap:tap + 1], in1=acc[:],
                        op0=ALU.mult, op1=ALU.add), reads=[b_xw, b_cw, b_acc], writes=[b_acc])
                k.op("act", lambda e, cc=cc: e.activation(out=xs[:, cc, :], in_=acc[:], func=AF.Silu, bias=cb[:, cc:cc + 1]),
                     reads=[b_acc, b_cb], writes=[b_xs])
            k.op("pool", lambda e: e.tensor_copy(bcb[:], xs[:, 2:4, :]), reads=[b_xs], writes=[b_bcb])
            for cc in range(3):
                k.op("pe", lambda e, cc=cc: e.transpose(B0[:, cc * 128:(cc + 1) * 128], xs[:, cc, :], cx.ident[:]),
                     reads=[b_xs, cx.b_ident], writes=[bB0])
            k.op("act", lambda e: e.activation(out=xtm[:], in_=B0[:, 0:256], func=AF.Copy), reads=[bB0], writes=[b_xtm])
            k.op("act", lambda e: e.activation(out=btm[:], in_=B0[:, 256:384], func=AF.Copy), reads=[bB0], writes=[b_btm])
            k.op("dve", lambda e: e.tensor_tensor(out=dt[:], in0=dt[:], in1=sm[:, 0:8], op=ALU.add), reads=[b_dt, b_sm],
                 writes=[b_dt])
            k.op("act", lambda e: e.activation(out=dt[:], in_=dt[:], func=AF.Exp), reads=[b_dt], writes=[b_dt])
            k.op("dve", lambda e: e.tensor_scalar_add(dt[:], dt[:], 1.0), reads=[b_dt], writes=[b_dt])
            k.op("act", lambda e: e.activation(out=dt[:], in_=dt[:], func=AF.Ln), reads=[b_dt], writes=[b_dt])
            k.op("dve", lambda e: e.tensor_tensor(out=la[:], in0=dt[:], in1=sm[:, 8:16], op=ALU.mult), reads=[b_dt, b_sm],
                 writes=[b_la])
            k.dma("sp", pxs_d[c].rearrange("p (c n) -> p c n", c=4), xs[:], reads=[b_xs], writes=[b_prep[c][0]])
            k.dma("sp", pxt_d[c], xtm[:], reads=[b_xtm], writes=[b_prep[c][1]])
            k.dma("sp", pbt_d[c], btm[:], reads=[b_btm], writes=[b_prep[c][2]])
            k.dma("sp", pbc_d[c].rearrange("p (c n) -> p c n", c=2), bcb[:], reads=[b_bcb], writes=[b_prep[c][3]])
            k.dma("sp", pdt_d[c][:, 0:8], dt[:], reads=[b_dt], writes=[b_prep[c][4]])
            k.dma("sp", pdt_d[c][:, 8:16], la[:], reads=[b_la], writes=[b_prep[c][5]])
        if True:
            k.op("pe", lambda e: e.matmul(B1[:, 0:128], lhsT=bcb[:, 0, :], rhs=bcb[:, 1, :], start=True, stop=True),
                 reads=[b_bcb], writes=[bB1])
            k.op("dve", lambda e: e.tensor_copy(cbT[:], B1[:, 0:128]), reads=[bB1], writes=[b_cbT])
            for h in range(4):
                head_body(d, last, h, xs, b_xs, xtm, b_xtm, btm, b_btm, dt, b_dt, la, b_la, cbT, b_cbT)
            tail_body(d, c, xtm, b_xtm)

    def head_body(d, last, h, xs, b_xs, xtm, b_xtm, btm, b_btm, dt, b_dt, la, b_la, cbT, b_cbT):
        if True:
            if True:
                hp = h % 2
                labc, b_labc = labc_2[hp], b_labc_2[hp]
                ccol, b_ccol = ccol_2[hp], b_ccol_2[hp]
                seg, b_seg = seg_2[hp], b_seg_2[hp]
                LT, b_LT = LT_2[hp], b_LT_2[hp]
                Ebc, b_Ebc = Ebc_2[hp], b_Ebc_2[hp]
                MT, b_MT = MT_2[hp], b_MT_2[hp]
                CsT, b_CsT = CsT_2[hp], b_CsT_2[hp]
                xdt, b_xdt = xdt_2[hp], b_xdt_2[hp]
                xwt, b_xwt = xwt_2[hp], b_xwt_2[hp]
                dc = d * 4 + h
                BA, bBA = cx.bank[2 + h % 2], cx.b_bank[2 + h % 2]
                k.op("act", lambda e, dc=dc: e.activation(out=labc[:], in_=ones[:], func=AF.Copy, scale=la[:, dc:dc + 1]),
                     reads=[b_ones, b_la], writes=[b_labc])
                k.op("pe", lambda e, BA=BA: e.matmul(BA[:, 0:128], lhsT=labc[:], rhs=tri[d][:], start=True, stop=True),
                     reads=[b_labc, b_tri], writes=[bBA])
                k.op("pe", lambda e, BA=BA, dc=dc: e.matmul(BA[:, 128:129], lhsT=tri[d][:], rhs=la[:, dc:dc + 1],
                                                            start=True, stop=True), reads=[b_la, b_tri], writes=[bBA])
                k.op("dve", lambda e, BA=BA: e.tensor_copy(ccol[:], BA[:, 128:129]), reads=[bBA], writes=[b_ccol])
                k.op("dve", lambda e, BA=BA: e.scalar_tensor_tensor(out=seg[:], in0=BA[:, 0:128], scalar=ccol[:, 0:1],
                                                                    in1=mneg[d][:], op0=ALU.subtract, op1=ALU.add),
                     reads=[bBA, b_ccol, b_tri], writes=[b_seg])
                k.op("act", lambda e: e.activation(out=LT[:], in_=seg[:], func=AF.Exp), reads=[b_seg], writes=[b_LT])
                k.op("act", lambda e, BA=BA: e.activation(out=Ebc[:], in_=BA[:, 0:128], func=AF.Exp), reads=[bBA],
                     writes=[b_Ebc])
                k.op("pool", lambda e: e.tensor_tensor(out=MT[:], in0=LT[:], in1=cbT[:], op=ALU.mult),
                     reads=[b_LT, b_cbT], writes=[b_MT])
                k.op("pool", lambda e: e.tensor_tensor(out=CsT[:], in0=xs[:, 3, :], in1=Ebc[:], op=ALU.mult),
                     reads=[b_xs, b_Ebc], writes=[b_CsT])
                k.op("dve", lambda e, h=h, dc=dc: e.tensor_scalar(out=xdt[:], in0=xtm[:, h * 64:(h + 1) * 64],
                                                                 scalar1=dt[:, dc:dc + 1], scalar2=None, op0=ALU.mult),
                     reads=[b_xtm, b_dt], writes=[b_xdt])
                k.op("dve", lambda e, h=h, dc=dc: e.tensor_scalar(out=xwt[:], in0=xtm[:, h * 64:(h + 1) * 64],
                                                                 scalar1=dt[:, dc:dc + 1], scalar2=LT[:, last:last + 1],
                                                                 op0=ALU.mult, op1=ALU.mult),
                     reads=[b_xtm, b_dt, b_LT], writes=[b_xwt])
                k.op("pe", lambda e, h=h: e.matmul(BY[:, h * 64:(h + 1) * 64], lhsT=MT[:], rhs=xdt[:], start=True, stop=False),
                     reads=[b_MT, b_xdt], writes=[bBY])
                k.op("pe", lambda e, h=h: e.matmul(BY[:, h * 64:(h + 1) * 64], lhsT=CsT[:], rhs=Sbf[:, h, :], start=False,
                                                   stop=True), reads=[b_CsT, b_Sbf[h]], writes=[bBY])
                k.op("pe", lambda e, h=h: e.matmul(BS[:, h * 64:(h + 1) * 64], lhsT=btm[:], rhs=xwt[:], start=True, stop=True),
                     reads=[b_btm, b_xwt], writes=[bBS])
                k.op("dve", lambda e, h=h: e.scalar_tensor_tensor(out=S[:, h, :], in0=S[:, h, :], scalar=Ebc[:, last:last + 1],
                                                                  in1=BS[:, h * 64:(h + 1) * 64], op0=ALU.mult, op1=ALU.add),
                     reads=[b_S[h], b_Ebc, bBS], writes=[b_S[h]])
                k.op("pool", lambda e, h=h: e.tensor_copy(Sbf[:, h, :], S[:, h, :]), reads=[b_S[h]], writes=[b_Sbf[h]])

    def tail_body(d, c, xtm, b_xtm):
        if True:
            if d == 0:
                k.op("act", lambda e: e.activation(out=ysb[:], in_=BY[:, 0:256], func=AF.Copy), reads=[bBY], writes=[b_ysb])
                k.dma("sp", yf_d[c * 128:(c + 1) * 128, :], ysb[:], reads=[b_ysb], writes=[b_yfd])
                if cfg.get("debug"):
                    k.dma("sp", dbgy_d[c * 128:(c + 1) * 128, :], ysb[:], reads=[b_ysb], writes=[k.buf()])
            else:
                k.op("dve", lambda e: e.tensor_tensor(out=ysb[:], in0=BY[:, 0:256], in1=yft[:], op=ALU.add),
                     reads=[bBY, b_yft], writes=[b_ysb])
                k.op("pool", lambda e: e.tensor_tensor(out=t2[:], in0=xtm[:], in1=dn[:, 0, :], op=ALU.mult),
                     reads=[b_xtm, b_dn], writes=[b_t2])
                k.op("dve", lambda e: e.tensor_tensor(out=ysb[:], in0=ysb[:], in1=t2[:], op=ALU.add),
                     reads=[b_ysb, b_t2], writes=[b_ysb])
                k.op("act", lambda e: e.activation(out=zt[:], in_=zt[:], func=AF.Silu), reads=[b_zt], writes=[b_zt])
                k.op("dve", lambda e: e.tensor_tensor(out=ysb[:], in0=ysb[:], in1=zt[:], op=ALU.mult),
                     reads=[b_ysb, b_zt], writes=[b_ysb])
                k.op("dve", lambda e: e.bn_stats(bst[:], ysb[:]), reads=[b_ysb], writes=[b_bst])
                k.op("dve", lambda e: e.bn_aggr(bmv[:, 0:2], bst[:]), reads=[b_bst], writes=[b_bmv])
                k.op("dve", lambda e: e.scalar_tensor_tensor(out=bmv[:, 2:3], in0=bmv[:, 0:1], scalar=bmv[:, 0:1],
                                                             in1=bmv[:, 1:2], op0=ALU.mult, op1=ALU.add),
                     reads=[b_bmv], writes=[b_bmv])
                k.op("dve", lambda e: e.tensor_scalar_add(bmv[:, 2:3], bmv[:, 2:3], RMS_EPS), reads=[b_bmv], writes=[b_bmv])
                k.op("act", lambda e: e.activation(out=bmv[:, 3:4], in_=bmv[:, 2:3], func=AF.Ln), reads=[b_bmv], writes=[b_bmv])
                k.op("act", lambda e: e.activation(out=bmv[:, 3:4], in_=bmv[:, 3:4], func=AF.Exp, scale=-0.5), reads=[b_bmv],
                     writes=[b_bmv])
                k.op("dve", lambda e: e.tensor_scalar(out=ysb[:], in0=ysb[:], scalar1=bmv[:, 3:4], scalar2=None, op0=ALU.mult),
                     reads=[b_ysb, b_bmv], writes=[b_ysb])
                k.op("pool", lambda e: e.tensor_tensor(out=ysb[:], in0=ysb[:], in1=dn[:, 1, :], op=ALU.mult),
                     reads=[b_ysb, b_dn], writes=[b_ysb])
                k.dma("sp", out_d[c * 128:(c + 1) * 128, :], ysb[:], reads=[b_ysb], writes=[k.buf()])

    run_dir(0)
    run_dir(1)
    if own:
        return k.finish()


def emit_mods_all(k, cx, io):
    cT_d, b_cTd = io["cT2"]
    w_d, b_wd = io["ada_w"]
    bias_d, b_biasd = io["ada_b2"]
    out_d, b_outd = io["mods_s"]
    s = k.sb("s", [128, KC, 2])
    b_s = k.buf()
    bias = k.sb("bias", [2, 6144])
    b_bias = k.buf()
    res = k.sb("res", [2, 6144])
    b_res = k.buf()
    wt = [k.sb(f"wt{i}", [128, KC, 512]) for i in range(2)]
    b_wt = k.bufs(2, "wt")
    k.dma("sp", s[:], cT_d, reads=[b_cTd], writes=[b_s])
    k.op("act", lambda e: e.activation(out=s[:], in_=s[:], func=AF.Silu), reads=[b_s], writes=[b_s])
    n = 0
    for li in range(4):
        k.dma("sp", bias[:], bias_d[li], reads=[b_biasd], writes=[b_bias])
        for blk in range(12):
            i = n % 2
            n += 1
            bank, bb = cx.bank[2 + i], cx.b_bank[2 + i]
            k.dma("sp", wt[i][:], w_d[li, :, blk * 512:(blk + 1) * 512].rearrange("(c p) n -> p c n", p=128),
                  reads=[b_wd], writes=[b_wt[i]])
            for kc in range(KC):
                k.op("pe", lambda e, kc=kc, i=i, bank=bank: e.matmul(bank[0:2, :], lhsT=s[:, kc, :], rhs=wt[i][:, kc, :],
                                                                    start=(kc == 0), stop=(kc == KC - 1)),
                     reads=[b_s, b_wt[i]], writes=[bb])
            k.op("dve", lambda e, blk=blk, bank=bank: e.tensor_tensor(out=res[:, blk * 512:(blk + 1) * 512], in0=bank[0:2, :],
                                                                      in1=bias[:, blk * 512:(blk + 1) * 512], op=ALU.add),
                 reads=[bb, b_bias], writes=[b_res])
        k.dma("sp", out_d[li], res[:], reads=[b_res], writes=[b_outd])


def emit_proj_bg(k, cx, cfg, io):
    S = cfg["nlat"]
    mods_ap, b_mods, li = cfg["mods_src"]
    hf_d, b_hf = io["h_full"]
    hc_d, b_hc = io["h_ctx"]
    w_d, b_wd = io["w_bg"]
    q2 = io["qT_s"][0].rearrange("h d n -> (h d) n")
    k2 = io["kT_s"][0].rearrange("h d n -> (h d) n")
    x2 = io["xbc_s"][0].rearrange("c p n -> (c p) n")
    v_d = io["v_s"][0]
    z_d = io["z_s"][0]
    dt_d = io["dtr_s"][0]
    cT = k.sb("cTp", [128, 4, KC])
    b_cT = k.buf("cTp")
    for m in range(2):
        for ci in range(2):
            k.dma("sp", cT[:, 2 * m + ci, :], mods_ap[li, m, ci * D:(ci + 1) * D].rearrange("(c p) -> p c", p=128),
                  reads=[b_mods], writes=[b_cT], allow_slow_non_contiguous=True)
    for m in range(2):
        k.op("dve", lambda e, m=m: e.tensor_scalar_add(cT[:, 2 * m + 1, :], cT[:, 2 * m + 1, :], 1.0), reads=[b_cT], writes=[b_cT])
    zt = k.sb("zpad", [128, 4, 4])
    b_zt = k.buf("zpad")
    k.op("pool", lambda e: e.memset(zt[:], 0.0), writes=[b_zt])
    xs4 = io["xbc_s"][0]
    for c0, n in ((0, 2), (258, 4), (262 + S, 2)):
        k.dma("sp", xs4[:, :, c0:c0 + n].rearrange("c p n -> p c n"), zt[:, :, 0:n], reads=[b_zt], writes=[k.buf()],
              allow_slow_non_contiguous=True)
    hb = k.sb("hb", [128, 4, D])
    b_hb = k.bufs(4, "hb")
    uT = k.sb("uT", [128, KC, 512], BF16)
    b_uT = k.buf("uT")
    fmb = k.sb("fmb", [128, 4, 512])
    b_fmb = k.bufs(4, "fmb")
    tmb = k.sb("tmb", [128, 2, 512])
    b_tmb = k.bufs(2, "tmb")
    dtb = k.sb("dtb", [128, 2, 8])
    b_dtb = k.bufs(2, "dtb")
    for gi in range(S // 512 + 1):
        if gi < S // 512:
            tok0, nt, ms = gi * 512, 4, 0
            src_ap, bsrc = hf_d[tok0:tok0 + 512, :], b_hf
            na_col, ssd_col, ssd_row = tok0, 262 + tok0, 256 + tok0
        else:
            nt, ms = 2, 1
            src_ap, bsrc = hc_d, b_hc
            na_col, ssd_col, ssd_row = S, 2, 0
        N = nt * 128

        def ld(w, bw, src_ap=src_ap, bsrc=bsrc, nt=nt, ms=ms):
            k.dma("sp", hb[:, 0:nt, :], src_ap.rearrange("(t p) d -> p t d", p=128), reads=[bsrc], writes=b_hb[0:nt])
            for j in range(nt):
                emit_modT(cx, hb[:, j, :], b_hb[j], 128, uT, b_uT, j * 128, cT[:, 2 * ms + 1, :], cT[:, 2 * ms, :], b_cT)
        cx.step(None, ld)
        for p in range(2):
            def fn(w, bw, p=p, N=N, na_col=na_col, ssd_col=ssd_col):
                for cc in range(4):
                    ch = p * 4 + cc
                    bank, bb = cx.bank[2 + cc], cx.b_bank[2 + cc]
                    for kc in range(KC):
                        k.op("pe", lambda e, kc=kc, cc=cc, bank=bank: e.matmul(
                            bank[:, 0:N], lhsT=w[:, kc, cc * 128:(cc + 1) * 128], rhs=uT[:, kc, 0:N],
                            start=(kc == 0), stop=(kc == KC - 1)), reads=[bw, b_uT], writes=[bb])
                    o = fmb[:, cc, 0:N]
                    if cc % 2 == 0:
                        k.op("act", lambda e, o=o, bank=bank: e.activation(out=o, in_=bank[:, 0:N], func=AF.Copy),
                             reads=[bb], writes=[b_fmb[cc]])
                    else:
                        k.op("dve", lambda e, o=o, bank=bank: e.tensor_copy(o, bank[:, 0:N]), reads=[bb], writes=[b_fmb[cc]])
                    if ch < 2:
                        dst = q2[ch * 128:(ch + 1) * 128, na_col:na_col + N]
                    elif ch < 4:
                        dst = k2[(ch - 2) * 128:(ch - 1) * 128, na_col:na_col + N]
                    else:
                        dst = x2[(ch - 4) * 128:(ch - 3) * 128, ssd_col:ssd_col + N]
                    k.dma("sp", dst, o, reads=[b_fmb[cc]], writes=[k.buf()])
            cx.step((w_d[:, p * 512:(p + 1) * 512], b_wd, KC), fn)

        def fn_tm(w, bw, nt=nt, na_col=na_col, ssd_row=ssd_row):
            for j in range(nt):
                bank, bb = cx.bank[6 + j % 2], cx.b_bank[6 + j % 2]
                for kc in range(KC):
                    k.op("pe", lambda e, kc=kc, j=j, bank=bank: e.matmul(
                        bank[:, :], lhsT=uT[:, kc, j * 128:(j + 1) * 128], rhs=w[:, kc, :],
                        start=(kc == 0), stop=(kc == KC - 1)), reads=[bw, b_uT], writes=[bb])
                o = tmb[:, j % 2, :]
                k.op("act", lambda e, o=o, bank=bank: e.activation(out=o, in_=bank[:, :], func=AF.Copy), reads=[bb],
                     writes=[b_tmb[j % 2]])
                k.dma("sp", v_d[na_col + j * 128:na_col + (j + 1) * 128, :], tmb[:, j % 2, 0:256], reads=[b_tmb[j % 2]],
                      writes=[k.buf()])
                k.dma("sp", z_d[ssd_row + j * 128:ssd_row + (j + 1) * 128, :], tmb[:, j % 2, 256:512], reads=[b_tmb[j % 2]],
                      writes=[k.buf()])
        cx.step((w_d[:, 1024:1536], b_wd, KC), fn_tm)

        def fn_dt(w, bw, nt=nt, ssd_row=ssd_row):
            for j in range(nt):
                bank, bb = cx.bank[6 + j % 2], cx.b_bank[6 + j % 2]
                for kc in range(KC):
                    k.op("pe", lambda e, kc=kc, j=j, bank=bank: e.matmul(
                        bank[:, 0:8], lhsT=uT[:, kc, j * 128:(j + 1) * 128], rhs=w[:, kc, 0:8],
                        start=(kc == 0), stop=(kc == KC - 1)), reads=[bw, b_uT], writes=[bb])
                k.op("dve", lambda e, j=j, bank=bank: e.tensor_copy(dtb[:, j % 2, :], bank[:, 0:8]), reads=[bb],
                     writes=[b_dtb[j % 2]])
                k.dma("sp", dt_d[ssd_row + j * 128:ssd_row + (j + 1) * 128, :], dtb[:, j % 2, :], reads=[b_dtb[j % 2]],
                      writes=[k.buf()])
        cx.step((w_d[:, 1536:1544], b_wd, KC), fn_dt)
    cx.flush()


def emit_outproj_bg(k, cx, cfg, io):
    S = cfg["nlat"]
    live = cfg["live"]
    attn_d, b_attn = io["attn_s"]
    ssm_d, b_ssm = io["ssm_s"]
    wo_d, b_wod = io["wo_bg"]
    yp_d = io["y_part"][0]
    ypc_d = io["y_part_ctx"][0]
    wo = k.sb("wo", [128, 4, D], BF16)
    b_wo = k.buf("wo")
    k.dma("pool", wo[:], wo_d.rearrange("(c p) n -> p c n", p=128), reads=[b_wod], writes=[b_wo])
    mixb = [k.sb(f"mixb{i}", [128, 512]) for i in range(2)]
    b_mixb = k.bufs(2, "mixb")
    mT4 = k.sb("mT4", [128, 4, 128], BF16)
    b_mT4 = k.buf("mT4")
    yb = [k.sb(f"yb{i}", [128, D]) for i in range(2)]
    b_yb = k.bufs(2, "yb")
    ntile = S // 128 + (2 if live else 0)
    for t in range(ntile):
        i = t % 2
        if t < S // 128:
            arow, srow, dst = t * 128, 256 + t * 128, yp_d[t * 128:(t + 1) * 128, :]
        else:
            c = t - S // 128
            arow, srow, dst = S + c * 128, c * 128, ypc_d[c * 128:(c + 1) * 128, :]
        k.dma("sp", mixb[i][:, 0:256], attn_d[arow:arow + 128, :], reads=[b_attn], writes=[b_mixb[i]])
        k.dma("sp", mixb[i][:, 256:512], ssm_d[srow:srow + 128, :], reads=[b_ssm], writes=[b_mixb[i]])
        emit_copyT(cx, mixb[i], b_mixb[i], 128, 4, mT4, b_mT4, 0)
        for hf in range(2):
            bk = 2 + (t * 2 + hf) % 4
            bank, bb = cx.bank[bk], cx.b_bank[bk]
            for kc in range(4):
                k.op("pe", lambda e, kc=kc, hf=hf, bank=bank: e.matmul(bank[:, :], lhsT=mT4[:, kc, :],
                                                                      rhs=wo[:, kc, hf * 512:(hf + 1) * 512],
                                                                      start=(kc == 0), stop=(kc == 3)),
                     reads=[b_mT4, b_wo], writes=[bb])
            o = yb[i][:, hf * 512:(hf + 1) * 512]
            if hf == 0:
                k.op("act", lambda e, o=o, bank=bank: e.activation(out=o, in_=bank[:, :], func=AF.Copy), reads=[bb],
                     writes=[b_yb[i]])
            else:
                k.op("dve", lambda e, o=o, bank=bank: e.tensor_copy(o, bank[:, :]), reads=[bb], writes=[b_yb[i]])
        k.dma("sp", dst, yb[i][:], reads=[b_yb[i]], writes=[k.buf()])


RG4 = [[0, 1, 2, 3], [4, 5, 6, 7]]


def build_fused(S=16384):
    k = KB()
    cx = Ctx(k)
    SH = S // 4
    NCT = 256
    NTOT = S + NCT
    LP = 260 + S + 4
    rows = S // GRID_W
    NV = SH + NCT + 6
    F_E, F_O = 2816, 3584

    def ext(name, shape, dt=F32):
        return k.dram(name, shape, dt, "ExternalInput")

    def scr(name, shape, dt=F32):
        return k.dram(name, shape, dt, "Internal")
    x_in = ext("x_in", [SH + NCT, D])
    x_full = ext("x_full", [S, D])
    cT2 = ext("cT2", [128, KC, 2])
    ada_w = ext("ada_w", [4, D, 6 * D])
    ada_b2 = ext("ada_b2", [4, 2, 6 * D])
    cbc_ln = ext("cbc_ln", [4, 128, 4, D])
    w_bg = ext("w_bg", [2, D, 1544])
    wo_bg = ext("wo_bg", [2, 512, D])
    mb = ext("mb", [2, 25, 128, 512])
    cwT = ext("cwT", [2, 128, 4, 5])
    cb = ext("cb", [2, 128, 4])
    small = ext("small", [2, 128, 16])
    dn = ext("dn", [2, 128, 2, 256])
    f_w1 = ext("ffn_w1", [2, 1, D, F_E])
    f_w3 = ext("ffn_w3", [2, 1, D, F_E])
    f_w2 = ext("ffn_w2", [2, 1, F_E, D])
    sc_win = ext("sc_w_in", [2, D, 3 * D])
    sc_wout = ext("sc_w_out", [2, D, D])
    sc_cw = ext("sc_cw", [2, 128, KC, 3])
    w_router = ext("w_router", [2, D, 8])
    m_w1 = ext("moe_w1", [2, 8, D, F_O])
    m_w3 = ext("moe_w3", [2, 8, D, F_O])
    m_w2 = ext("moe_w2", [2, 8, F_O, D])
    halsel = ext("halsel", [128, 2, KC, 8])
    out = k.dram("out", [SH, D], F32, "ExternalOutput")

    mods_s = scr("mods_s", [4, 2, 6 * D])
    hA = scr("hA", [SH + NCT, D])
    hB = scr("hB", [SH + NCT, D])
    ag_in = scr("ag_in", [SH, D])
    h_full_s = scr("h_full_s", [S, D])
    qT_s = scr("qT_s", [4, 64, NTOT])
    kT_s = scr("kT_s", [4, 64, NTOT])
    xbc_s = scr("xbc_s", [4, 128, LP])
    v_s = scr("v_s", [NTOT, 256])
    z_s = scr("z_s", [NTOT, 256])
    dtr_s = scr("dtr_s", [NTOT, 8])
    attn_s = scr("attn_s", [NTOT, 256])
    ssm_s = scr("ssm_s", [NTOT, 256])
    yf_scr = scr("yf_scr", [NTOT, 256])
    y_part = scr("y_part", [S, D])
    y_part_ctx = scr("y_part_ctx", [NCT, D])
    y_rs = scr("y_rs", [SH, D])
    y_ctx = scr("y_ctx", [NCT, D])
    bnd = scr("bnd", [2, D])
    bnd_all = scr("bnd_all", [8, D])
    vT_scr = scr("vT_scr", [D, NV])
    gbT_scr = scr("gbT_scr", [D, NV])

    def sl(t, ap):
        return (ap, t[1])

    k.phase_begin()
    emit_mods_all(k, cx, {"cT2": cT2, "ada_w": ada_w, "ada_b2": ada_b2, "mods_s": mods_s})
    k.phase_end()

    h_cur = x_in
    h_nxt = [hA, hB]
    for i in range(4):
        j = i // 2
        live = i < 2
        nms = 2 if live else 1
        ntail = SH + (NCT if live else 0)
        tgroups = [(g * 512, 4, 0, 1 + g * 512) for g in range(SH // 512)]
        if live:
            tgroups.append((SH, 2, 1, SH + 4))
        h_out = (out if i == 3 else h_nxt[i % 2])
        msrc = (mods_s[0], mods_s[1], i)
        if i % 2 == 0:
            hf = x_full if i == 0 else h_full_s
            hctx = sl(h_cur, h_cur[0][SH:SH + NCT, :])
            k.phase_begin()
            emit_proj_bg(k, cx, dict(nlat=S, mods_src=msrc),
                         {"h_full": hf, "h_ctx": hctx, "w_bg": sl(w_bg, w_bg[0][j]), "qT_s": qT_s, "kT_s": kT_s, "xbc_s": xbc_s,
                          "v_s": v_s, "z_s": z_s, "dtr_s": dtr_s})
            k.phase_end()
            k.phase_begin()
            NQ = S + (NCT if live else 0)
            build_na(dict(rows=rows, with_ctx=live), k, cx,
                     {"qT": sl(qT_s, qT_s[0][:, :, 0:NQ]), "kT": sl(kT_s, kT_s[0][:, :, 0:S]), "kcT": sl(kT_s, kT_s[0][:, :, S:NTOT]),
                      "v": sl(v_s, v_s[0][0:S, :]), "vc": sl(v_s, v_s[0][S:NTOT, :]), "mb": sl(mb, mb[0][j]),
                      "attn": sl(attn_s, attn_s[0][0:NQ, :])})
            k.phase_end()
            k.phase_begin()
            build_ssd(dict(nlat=S), k, cx,
                      {"xbcT": xbc_s, "cwT": sl(cwT, cwT[0][j]), "cb": sl(cb, cb[0][j]), "dtr": dtr_s, "small": sl(small, small[0][j]),
                       "dn": sl(dn, dn[0][j]), "z": z_s, "ssm": ssm_s, "yf_scr": yf_scr})
            k.phase_end()
            k.phase_begin()
            emit_outproj_bg(k, cx, dict(nlat=S, live=live),
                            {"attn_s": attn_s, "ssm_s": ssm_s, "wo_bg": sl(wo_bg, wo_bg[0][j]), "y_part": y_part,
                             "y_part_ctx": y_part_ctx})
            k.phase_end()
            k.cc("ReduceScatter", ALU.add, RG4, y_part[0], y_rs[0], [y_part[1]], [y_rs[1]])
            if live:
                k.cc("AllReduce", ALU.add, RG4, y_part_ctx[0], y_ctx[0], [y_part_ctx[1]], [y_ctx[1]])
            k.phase_begin()
            build_tail(dict(mixer="pre", ne=1, f=F_E, groups=tgroups, ntok=ntail, nmod=nms, mods_src=msrc), k, cx,
                       {"h_in": sl(h_cur, h_cur[0][0:ntail, :]), "h_out": sl(h_out, h_out[0][0:ntail, :]) if i != 3 else h_out,
                        "cbc": sl(cbc_ln, cbc_ln[0][i]), "y_lat": y_rs, "y_ctx": y_ctx,
                        "w1": sl(f_w1, f_w1[0][j]), "w3": sl(f_w3, f_w3[0][j]), "w2": sl(f_w2, f_w2[0][j])})
            k.phase_end()
        else:
            k.phase_begin()
            bt = k.sb("bt", [2, D])
            b_bt = k.buf("bt")
            k.dma("sp", bt[0:1, :], h_cur[0][0:1, :], reads=[h_cur[1]], writes=[b_bt])
            k.dma("sp", bt[1:2, :], h_cur[0][SH - 1:SH, :], reads=[h_cur[1]], writes=[b_bt])
            k.dma("sp", bnd[0], bt[:], reads=[b_bt], writes=[bnd[1]])
            k.phase_end()
            k.cc("AllGather", ALU.bypass, RG4, bnd[0], bnd_all[0], [bnd[1]], [bnd_all[1]])
            zc = [SH + 3, SH + 4 + NCT] if live else []
            k.phase_begin()
            build_tail(dict(mixer="sc", ne=8, f=F_O, groups=tgroups, ntok=ntail, nmod=nms, nv=NV, zero_cols=zc,
                            halo_cols=(0, SH + 1), nhalo=8, mods_src=msrc), k, cx,
                       {"h_in": sl(h_cur, h_cur[0][0:ntail, :]), "h_out": sl(h_out, h_out[0][0:ntail, :]) if i != 3 else h_out,
                        "cbc": sl(cbc_ln, cbc_ln[0][i]), "hal": bnd_all, "halsel": halsel,
                        "sc_w_in": sl(sc_win, sc_win[0][j]), "sc_w_out": sl(sc_wout, sc_wout[0][j]), "sc_cw": sl(sc_cw, sc_cw[0][j]),
                        "vT_scr": vT_scr, "gbT_scr": gbT_scr, "w_router": sl(w_router, w_router[0][j]),
                        "w1": sl(m_w1, m_w1[0][j]), "w3": sl(m_w3, m_w3[0][j]), "w2": sl(m_w2, m_w2[0][j])})
            k.phase_end()
            if i == 1:
                k.phase_begin()
                cpb = k.sb("cpb", [128, 4, D])
                b_cpb = k.buf("cpb")
                for g in range(SH // 512):
                    k.dma("sp", cpb[:], h_out[0][g * 512:(g + 1) * 512, :].rearrange("(t p) d -> p t d", p=128),
                          reads=[h_out[1]], writes=[b_cpb])
                    k.dma("sp", ag_in[0][g * 512:(g + 1) * 512, :].rearrange("(t p) d -> p t d", p=128), cpb[:],
                          reads=[b_cpb], writes=[ag_in[1]])
                k.phase_end()
                k.cc("AllGather", ALU.bypass, RG4, ag_in[0], h_full_s[0], [ag_in[1]], [h_full_s[1]])
        h_cur = h_out
    k.barrier()
    return k.finish()


_PROGS = {}


def _prog(key, fn):
    if key not in _PROGS:
        _PROGS[key] = fn()
    return _PROGS[key]


def _colT(v):
    return np.ascontiguousarray(np.asarray(v, np.float32).reshape(KC, 128).T)


def _bc(v, n=128):
    return np.broadcast_to(np.asarray(v, np.float32)[None], (n,) + tuple(np.shape(v)))


def _run(nc, in_maps):
    in_maps = [{kk: np.ascontiguousarray(vv, dtype=np.float32) for kk, vv in m.items()} for m in in_maps]
    res = run_bass_kernel_spmd(nc, in_maps, core_ids=list(range(8)))
    return res.results


def kernel(x, c, ctx, c_ctx, ada_w, ada_b, ln_g, ln_b, even_w_in, na_rpb, ssm_conv_w, ssm_conv_b, ssm_dt_bias,
           ssm_a_log, ssm_d, ssm_norm_w, even_w_out, ffn_w1, ffn_w3, ffn_w2, sc_w_in, sc_conv_w, sc_w_out,
           moe_router, moe_w1, moe_w3, moe_w2):
    f32 = np.float32
    x = np.asarray(x, f32)
    B, S, _ = x.shape
    SH = S // 4
    NCT = 256
    rows = S // GRID_W
    cT3 = np.stack([_colT(c[0]), _colT(c[1]), _colT(c_ctx)], axis=-1)
    res = _run(_prog("mods", build_mods), [
        {"cT3": cT3, "ada_w": ada_w[r % 4][:, (r // 4) * 3072:(r // 4 + 1) * 3072],
         "ada_b3": _bc(ada_b[r % 4][(r // 4) * 3072:(r // 4 + 1) * 3072], 3)} for r in range(8)])
    mods = [np.concatenate([res[i]["mods"], res[i + 4]["mods"]], axis=1).reshape(3, 6, D) for i in range(4)]

    h_lat = x.copy()
    h_ctx = np.asarray(ctx, f32).copy()

    def shard(lat, cx_, with_ctx):
        out = []
        for r in range(8):
            b, q = r // 4, r % 4
            parts = [lat[b, q * SH:(q + 1) * SH]]
            if with_ctx:
                parts.append(cx_[b])
            out.append(np.concatenate(parts, 0) if with_ctx else parts[0])
        return out

    def consts(i, b, nms):
        cbc = np.zeros((128, 4 + 2 * nms, D), f32)
        cbc[:, 0], cbc[:, 1], cbc[:, 2], cbc[:, 3] = ln_g[i, 0], ln_b[i, 0], ln_g[i, 1], ln_b[i, 1]
        cT = np.zeros((128, 4 * nms, KC), f32)
        for m in range(nms):
            mv = mods[i][b if m == 0 else 2]
            cbc[:, 4 + 2 * m], cbc[:, 5 + 2 * m] = mv[2], mv[5]
            cT[:, 4 * m + 0], cT[:, 4 * m + 1] = _colT(mv[0]), _colT(mv[1])
            cT[:, 4 * m + 2], cT[:, 4 * m + 3] = _colT(mv[3]), _colT(mv[4])
        return cbc, cT

    for i in range(4):
        j = i // 2
        live = i < 2
        nms = 2 if live else 1
        ntail = SH + (NCT if live else 0)
        tgroups = [(g * 512, 4, 0, 1 + g * 512) for g in range(SH // 512)]
        if live:
            tgroups.append((SH, 2, 1, SH + 4))
        if i % 2 == 0:
            pg = [(g * 512, 4, 0) for g in range(SH // 512)] + [(SH, 2, 1)]
            nc = _prog("proj", lambda: build_proj(dict(groups=pg, ntok=SH + NCT, nmod=2, ncol=6176)))
            hs = shard(h_lat, h_ctx, True)
            ims = []
            for r in range(8):
                b = r // 4
                cT = np.stack([_colT(mods[i][b][0]), _colT(mods[i][b][1]), _colT(mods[i][2][0]), _colT(mods[i][2][1])], 1)
                ims.append({"h_in": hs[r], "w": even_w_in[j], "cT": cT})
            res = _run(nc, ims)
            P_lat = np.stack([np.concatenate([res[b * 4 + q]["proj"][:SH] for q in range(4)], 0) for b in range(B)])
            P_ctx = np.stack([res[b * 4]["proj"][SH:] for b in range(B)])
            del res
            nc = _prog(("na", live), lambda: build_na(dict(rows=rows, with_ctx=live)))
            ims = []
            for r in range(8):
                b, g = r // 4, r % 4
                sl = slice(g * 256, (g + 1) * 256)
                fm = lambda a: a.reshape(-1, 4, 64).transpose(1, 2, 0)
                ql, kl, vl = P_lat[b][:, 0:1024][:, sl], P_lat[b][:, 1024:2048][:, sl], P_lat[b][:, 2048:3072][:, sl]
                qc, kc_, vc_ = P_ctx[b][:, 0:1024][:, sl], P_ctx[b][:, 1024:2048][:, sl], P_ctx[b][:, 2048:3072][:, sl]
                qq = np.concatenate([ql, qc], 0) if live else ql
                ims.append({"qT": fm(qq), "kT": fm(kl), "kcT": fm(kc_), "v": vl, "vc": vc_,
                            "mb": na_mask_bias(np.asarray(na_rpb[j][4 * g:4 * g + 4], f32), rows).reshape(25, 128, 512)})
            res = _run(nc, ims)
            mix_lat = np.zeros((B, S, 2 * D), f32)
            mix_ctx = np.zeros((B, NCT, 2 * D), f32)
            for r in range(8):
                b, g = r // 4, r % 4
                mix_lat[b, :, g * 256:(g + 1) * 256] = res[r]["attn"][:S]
                if live:
                    mix_ctx[b, :, g * 256:(g + 1) * 256] = res[r]["attn"][S:]
            del res
            nc = _prog("ssd", lambda: build_ssd(dict(nlat=S)))
            LP = 260 + S + 4
            ims = []
            for r in range(8):
                b, g = r // 4, r % 4
                ch = np.concatenate([np.arange(g * 256, (g + 1) * 256), 1024 + np.arange(g * 128, (g + 1) * 128),
                                     1536 + np.arange(g * 128, (g + 1) * 128)])
                xbcT = np.zeros((512, LP), f32)
                xbcT[:, 2:258] = P_ctx[b][:, 4096 + ch].T
                xbcT[:, 262:262 + S] = P_lat[b][:, 4096 + ch].T
                hd = np.arange(4 * g, 4 * g + 4)
                dcols = np.concatenate([6144 + hd, 6144 + 16 + hd])
                small = np.concatenate([ssm_dt_bias[j][0, hd], ssm_dt_bias[j][1, hd], ssm_a_log[j][0, hd], ssm_a_log[j][1, hd]])
                dn = np.stack([np.repeat(np.asarray(ssm_d[j], f32)[hd], 64), np.asarray(ssm_norm_w[j], f32)[g * 256:(g + 1) * 256]])
                ims.append({"xbcT": xbcT.reshape(4, 128, LP),
                            "cwT": np.asarray(ssm_conv_w[j], f32)[:, ch].T.reshape(4, 128, 5).transpose(1, 0, 2),
                            "cb": np.asarray(ssm_conv_b[j], f32)[ch].reshape(4, 128).T,
                            "dtr": np.concatenate([P_ctx[b][:, dcols], P_lat[b][:, dcols]], 0),
                            "small": _bc(small), "dn": _bc(dn),
                            "z": np.concatenate([P_ctx[b][:, 3072 + g * 256:3072 + (g + 1) * 256],
                                                 P_lat[b][:, 3072 + g * 256:3072 + (g + 1) * 256]], 0)})
            res = _run(nc, ims)
            for r in range(8):
                b, g = r // 4, r % 4
                mix_lat[b, :, D + g * 256:D + (g + 1) * 256] = res[r]["ssm"][NCT:]
                mix_ctx[b, :, D + g * 256:D + (g + 1) * 256] = res[r]["ssm"][:NCT]
            del res, P_lat, P_ctx
            nc = _prog(("tail_e", live), lambda: build_tail(dict(mixer="even", ne=1, f=2816, groups=tgroups, ntok=ntail, nmod=nms)))
            hs = shard(h_lat, h_ctx, live)
            ms = shard(mix_lat, mix_ctx, live)
            ims = []
            for r in range(8):
                cbc, cT = consts(i, r // 4, nms)
                ims.append({"h_in": hs[r], "mix": ms[r], "cbc": cbc, "cT": cT, "w_out": even_w_out[j],
                            "w1": ffn_w1[j][None], "w3": ffn_w3[j][None], "w2": ffn_w2[j][None]})
            res = _run(nc, ims)
        else:
            zc = [SH + 3, SH + 4 + NCT] if live else []
            nc = _prog(("tail_o", live), lambda: build_tail(dict(mixer="sc", ne=8, f=3584, groups=tgroups, ntok=ntail, nmod=nms,
                                                                 nv=SH + NCT + 6, zero_cols=zc, halo_cols=(0, SH + 1))))
            hs = shard(h_lat, h_ctx, live)
            ims = []
            for r in range(8):
                b, q = r // 4, r % 4
                cbc, cT = consts(i, b, nms)
                hal = np.zeros((2, D), f32)
                if q > 0:
                    hal[0] = h_lat[b, q * SH - 1]
                if q < 3:
                    hal[1] = h_lat[b, (q + 1) * SH]
                halv = _bc(np.array([1.0 if q > 0 else 0.0, 1.0 if q < 3 else 0.0], f32))
                ims.append({"h_in": hs[r], "cbc": cbc, "cT": cT, "hal": hal, "halv": halv, "sc_w_in": sc_w_in[j],
                            "sc_w_out": sc_w_out[j],
                            "sc_cw": np.asarray(sc_conv_w[j], f32).reshape(3, KC, 128).transpose(2, 1, 0),
                            "w_router": moe_router[j], "w1": moe_w1[j], "w3": moe_w3[j], "w2": moe_w2[j]})
            res = _run(nc, ims)
        new_lat = np.empty_like(h_lat)
        for r in range(8):
            b, q = r // 4, r % 4
            new_lat[b, q * SH:(q + 1) * SH] = res[r]["h_out"][:SH]
            if live and q == 0:
                h_ctx[b] = res[r]["h_out"][SH:]
        h_lat = new_lat
        del res
    return h_lat


def kernel_fused_experimental(x, c, ctx, c_ctx, ada_w, ada_b, ln_g, ln_b, even_w_in, na_rpb, ssm_conv_w, ssm_conv_b,
                              ssm_dt_bias, ssm_a_log, ssm_d, ssm_norm_w, even_w_out, ffn_w1, ffn_w3, ffn_w2, sc_w_in,
                              sc_conv_w, sc_w_out, moe_router, moe_w1, moe_w3, moe_w2):
    f32 = np.float32
    A = lambda a: np.asarray(a, f32)
    x = A(x)
    B, S, _ = x.shape
    SH = S // 4
    rows = S // GRID_W
    nc = _prog("fused", lambda: build_fused(S))
    cbc_ln = np.stack([np.stack([_bc(A(ln_g)[i, 0]), _bc(A(ln_b)[i, 0]), _bc(A(ln_g)[i, 1]), _bc(A(ln_b)[i, 1])], 1)
                       for i in range(4)])
    ada_b2 = np.repeat(A(ada_b)[:, None, :], 2, axis=1)
    shared = {"ada_w": A(ada_w), "ada_b2": ada_b2, "cbc_ln": cbc_ln,
              "ffn_w1": A(ffn_w1)[:, None], "ffn_w3": A(ffn_w3)[:, None], "ffn_w2": A(ffn_w2)[:, None],
              "sc_w_in": A(sc_w_in), "sc_w_out": A(sc_w_out),
              "sc_cw": A(sc_conv_w).reshape(2, 3, KC, 128).transpose(0, 3, 2, 1),
              "w_router": A(moe_router), "moe_w1": A(moe_w1), "moe_w3": A(moe_w3), "moe_w2": A(moe_w2)}
    shared = {kk: np.ascontiguousarray(vv, dtype=f32) for kk, vv in shared.items()}
    ims = []
    for r in range(8):
        b, g = r // 4, r % 4
        q = g
        hd = np.arange(4 * g, 4 * g + 4)
        cols = np.concatenate([np.arange(g * 256, (g + 1) * 256), 1024 + np.arange(g * 256, (g + 1) * 256),
                               4096 + np.arange(g * 256, (g + 1) * 256), 4096 + 1024 + np.arange(g * 128, (g + 1) * 128),
                               4096 + 1536 + np.arange(g * 128, (g + 1) * 128), 2048 + np.arange(g * 256, (g + 1) * 256),
                               3072 + np.arange(g * 256, (g + 1) * 256), 6144 + hd, 6144 + 16 + hd])
        ch = np.concatenate([np.arange(g * 256, (g + 1) * 256), 1024 + np.arange(g * 128, (g + 1) * 128),
                             1536 + np.arange(g * 128, (g + 1) * 128)])
        orow = np.concatenate([np.arange(g * 256, (g + 1) * 256), 1024 + np.arange(g * 256, (g + 1) * 256)])
        hs = np.zeros((2, 8), f32)
        if q > 0:
            hs[0, 2 * (q - 1) + 1] = 1.0
        if q < 3:
            hs[1, 2 * (q + 1)] = 1.0
        m = {"x_in": np.concatenate([x[b, q * SH:(q + 1) * SH], A(ctx)[b]], 0), "x_full": x[b],
             "cT2": np.stack([_colT(A(c)[b]), _colT(A(c_ctx))], -1),
             "w_bg": A(even_w_in)[:, :, cols], "wo_bg": A(even_w_out)[:, orow, :],
             "mb": np.stack([na_mask_bias(A(na_rpb)[j][hd], rows).reshape(25, 128, 512) for j in range(2)]),
             "cwT": np.stack([A(ssm_conv_w)[j][:, ch].T.reshape(4, 128, 5).transpose(1, 0, 2) for j in range(2)]),
             "cb": np.stack([A(ssm_conv_b)[j][ch].reshape(4, 128).T for j in range(2)]),
             "small": np.stack([_bc(np.concatenate([A(ssm_dt_bias)[j][0, hd], A(ssm_dt_bias)[j][1, hd],
                                                    A(ssm_a_log)[j][0, hd], A(ssm_a_log)[j][1, hd]])) for j in range(2)]),
             "dn": np.stack([_bc(np.stack([np.repeat(A(ssm_d)[j][hd], 64), A(ssm_norm_w)[j][g * 256:(g + 1) * 256]]))
                             for j in range(2)]),
             "halsel": np.broadcast_to(hs[None, :, None, :], (128, 2, KC, 8))}
        m = {kk: np.ascontiguousarray(vv, dtype=f32) for kk, vv in m.items()}
        m.update(shared)
        ims.append(m)
    res = run_bass_kernel_spmd(nc, ims, core_ids=list(range(8)))
    outp = np.empty((B, S, D), f32)
    for r in range(8):
        outp[r // 4, (r % 4) * SH:(r % 4 + 1) * SH] = res.results[r]["out"]
    return outp
```
